# Optimizing a Trainium2 kernel written in Bass

```python
import jax, jax.numpy as jnp
from jax import lax
import numpy as np

D_MODEL = 1024
BATCH = 8
SEQ = 4096
DEPTH = 2

HEAD_DIM = 64
GROUP_WIDTH = D_MODEL // 4
GROUP_HEADS = GROUP_WIDTH // HEAD_DIM
MIX_WIDTH = 4 * GROUP_WIDTH
ROPE_THETA = 10000.0
Q_BLOCK = 128

MOBA_BLOCK = 256
MOBA_TOPK = 3
RET_CHUNK = 128
RG_CONV = 4
RG_C = 8.0
RG_BLOCKS = GROUP_HEADS
RG_BLOCK_W = GROUP_WIDTH // RG_BLOCKS
IDX_HEADS = 8
IDX_DIM = HEAD_DIM
IDX_SCALE = (IDX_HEADS ** -0.5) * (IDX_DIM ** -0.5)
DSA_TOPK = 256

N_EXPERTS = 32
TOP_K = 4
D_FF = D_MODEL
SWIGLU_LIMIT = 7.0
SWIGLU_ALPHA = 1.702

ALPHA = (2 * DEPTH) ** 0.25
BETA = (8 * DEPTH) ** -0.25
LN_EPS = 1e-5

IN_SIZES = (GROUP_WIDTH,) * 3 + (GROUP_WIDTH,) * 4 + (GROUP_WIDTH,) * 2 + (GROUP_WIDTH,) * 3 + (IDX_HEADS * IDX_DIM, IDX_DIM, IDX_HEADS)
IN_WIDTH = 12 * GROUP_WIDTH + IDX_HEADS * IDX_DIM + IDX_DIM + IDX_HEADS

kernel_name = 'hybrid_moba_retnet_rglru_dsa_moe'

F32 = jnp.float32


def _layer_norm(x, g, b):
    xf = x.astype(F32)
    mu = jnp.mean(xf, axis=-1, keepdims=True)
    var = jnp.mean(jnp.square(xf - mu), axis=-1, keepdims=True)
    return ((xf - mu) * lax.rsqrt(var + LN_EPS) * g.astype(F32) + b.astype(F32)).astype(x.dtype)


def _rope_tables(T):
    inv = ROPE_THETA ** (-jnp.arange(0, HEAD_DIM, 2, dtype=F32) / HEAD_DIM)
    ang = jnp.arange(T, dtype=F32)[:, None] * inv[None, :]
    return jnp.cos(ang), jnp.sin(ang)


def _rope(x, cos, sin):
    xf = x.astype(F32)
    x1, x2 = jnp.split(xf, 2, axis=-1)
    c = cos[None, :, None, :]
    s = sin[None, :, None, :]
    return jnp.concatenate([x1 * c - x2 * s, x2 * c + x1 * s], axis=-1).astype(x.dtype)


def _moba_attention(q, k, v):
    B, T, H, d = q.shape
    nB = -(-T // MOBA_BLOCK)
    pad = nB * MOBA_BLOCK - T
    kp = jnp.pad(k, ((0, 0), (0, pad), (0, 0), (0, 0)))
    vp = jnp.pad(v, ((0, 0), (0, pad), (0, 0), (0, 0)))
    kb = kp.reshape(B, nB, MOBA_BLOCK, H, d).transpose(0, 3, 1, 2, 4)
    vb = vp.reshape(B, nB, MOBA_BLOCK, H, d).transpose(0, 3, 1, 2, 4)
    kmean = jnp.mean(kb.astype(F32), axis=3).astype(q.dtype)
    kt = min(MOBA_TOPK, nB)
    nQ = T // Q_BLOCK
    qc = q.reshape(B, nQ, Q_BLOCK, H, d)
    scale = d ** -0.5
    h_idx = jnp.arange(H)[None, :, None]

    def per_seq(args):
        qs, kps, vps, kbs, vbs, kms = args

        def per_chunk(cargs):
            qq, j = cargs
            start = j * Q_BLOCK
            own = start // MOBA_BLOCK
            pos_q = start + jnp.arange(Q_BLOCK)
            gate = jnp.einsum('qhd,hnd->qhn', qq, kms).astype(F32)
            gate = jnp.where(jnp.arange(nB) < own, gate, -jnp.inf)
            _, sel = lax.top_k(gate, kt)
            valid = sel < own
            kg = kbs[h_idx, sel]
            vg = vbs[h_idx, sel]
            s_sel = jnp.einsum('qhd,qhnkd->qhnk', qq, kg).astype(F32) * scale
            s_sel = jnp.where(valid[..., None], s_sel, -jnp.inf).reshape(Q_BLOCK, H, kt * MOBA_BLOCK)
            k_own = lax.dynamic_slice_in_dim(kps, own * MOBA_BLOCK, MOBA_BLOCK, axis=0)
            v_own = lax.dynamic_slice_in_dim(vps, own * MOBA_BLOCK, MOBA_BLOCK, axis=0)
            pos_k = own * MOBA_BLOCK + jnp.arange(MOBA_BLOCK)
            s_own = jnp.einsum('qhd,khd->qhk', qq, k_own).astype(F32) * scale
            s_own = jnp.where((pos_k[None, :] <= pos_q[:, None])[:, None, :], s_own, -jnp.inf)
            p = jax.nn.softmax(jnp.concatenate([s_sel, s_own], axis=-1), axis=-1).astype(vg.dtype)
            p_sel = p[..., :kt * MOBA_BLOCK].reshape(Q_BLOCK, H, kt, MOBA_BLOCK)
            p_own = p[..., kt * MOBA_BLOCK:]
            return jnp.einsum('qhnk,qhnkd->qhd', p_sel, vg) + jnp.einsum('qhk,khd->qhd', p_own, v_own)

        return lax.map(per_chunk, (qs, jnp.arange(nQ)))

    out = lax.map(per_seq, (qc, kp, vp, kb, vb, kmean))
    return out.reshape(B, T, H, d)


def _retention(q, k, v):
    B, T, H, d = q.shape
    dv = v.shape[-1]
    C = RET_CHUNK
    N = T // C
    log_g = jnp.log(1.0 - 2.0 ** (-5.0 - jnp.arange(H, dtype=F32)))
    n = jnp.arange(C, dtype=F32)
    diff = n[:, None] - n[None, :]
    d_mask = jnp.where(diff >= 0, jnp.exp(log_g[:, None, None] * jnp.maximum(diff, 0.0)), 0.0)
    xi = jnp.exp(log_g[:, None] * (n + 1.0))[None, :, :, None]
    zeta = jnp.exp(log_g[:, None] * (C - 1.0 - n))[None, :, :, None]
    g_chunk = jnp.exp(log_g * C)[None, :, None, None]
    to_chunks = lambda t: t.reshape(B, N, C, H, t.shape[-1]).transpose(1, 0, 3, 2, 4)

    def step(R, inp):
        qc, kc, vc = inp
        inner = jnp.einsum('bhnd,bhmd->bhnm', qc, kc) * d_mask
        o = jnp.einsum('bhnm,bhme->bhne', inner, vc) + jnp.einsum('bhnd,bhde->bhne', qc, R) * xi
        R = jnp.einsum('bhmd,bhme->bhde', kc * zeta, vc) + g_chunk * R
        return R, o

    R0 = jnp.zeros((B, H, d, dv), F32)
    _, o = lax.scan(step, R0, (to_chunks(q), to_chunks(k), to_chunks(v)))
    return o.transpose(1, 0, 3, 2, 4).reshape(B, T, H, dv)


def _rg_lru(xr, xg, conv_w, conv_b, wx, bx, wa, ba, lam):
    B, T, C = xr.shape
    xc = lax.conv_general_dilated(xr, conv_w[:, None, :], window_strides=(1,), padding=[(RG_CONV - 1, 0)],
                                  dimension_numbers=('NWC', 'WIO', 'NWC'), feature_group_count=C) + conv_b
    xb = xc.reshape(B, T, RG_BLOCKS, RG_BLOCK_W)
    gate_x = jax.nn.sigmoid((jnp.einsum('btnc,ncd->btnd', xb, wx).reshape(B, T, C) + bx).astype(F32))
    gate_a = jax.nn.sigmoid((jnp.einsum('btnc,ncd->btnd', xb, wa).reshape(B, T, C) + ba).astype(F32))
    log_a = -RG_C * gate_a * jax.nn.softplus(-lam.astype(F32))
    a = jnp.exp(log_a)
    b = jnp.sqrt(-jnp.expm1(2.0 * log_a)) * (gate_x * xc.astype(F32))

    def combine(left, right):
        a_l, b_l = left
        a_r, b_r = right
        return a_l * a_r, a_r * b_l + b_r

    _, h = lax.associative_scan(combine, (a, b), axis=1)
    return (h * jax.nn.gelu(xg.astype(F32))).astype(xr.dtype)


def _dsa_attention(q, k, v, qi, ki, wi):
    B, T, H, d = q.shape
    n_sel = min(DSA_TOPK, T // 4)
    nQ = T // Q_BLOCK
    scale = d ** -0.5
    qc = q.reshape(B, nQ, Q_BLOCK, H, d)
    qic = qi.reshape(B, nQ, Q_BLOCK, IDX_HEADS, IDX_DIM)
    wic = wi.reshape(B, nQ, Q_BLOCK, IDX_HEADS)
    pos_s = jnp.arange(T)

    def per_seq(args):
        qs, qis, wis, ks, vs, kis = args

        def per_chunk(cargs):
            qq, qiq, wq, j = cargs
            pos_q = j * Q_BLOCK + jnp.arange(Q_BLOCK)
            rel = jax.nn.relu(jnp.einsum('qhd,sd->qhs', qiq, kis).astype(F32))
            score = jnp.einsum('qh,qhs->qs', wq.astype(F32), rel) * IDX_SCALE
            score = jnp.where(pos_s[None, :] <= pos_q[:, None], score, -jnp.inf)
            _, idx = lax.top_k(score, n_sel)
            valid = idx <= pos_q[:, None]
            kg = ks[idx]
            vg = vs[idx]
            s = jnp.einsum('qhd,qkhd->qhk', qq, kg).astype(F32) * scale
            s = jnp.where(valid[:, None, :], s, -jnp.inf)
            p = jax.nn.softmax(s, axis=-1).astype(vg.dtype)
            return jnp.einsum('qhk,qkhd->qhd', p, vg)

        return lax.map(per_chunk, (qs, qis, wis, jnp.arange(nQ)))

    out = lax.map(per_seq, (qc, qic, wic, k, v, ki))
    return out.reshape(B, T, H, d)


def _moe(x, router_w, router_b, w1, b1, w2, b2):
    B, T, D = x.shape
    xf = x.reshape(B * T, D)
    logits = jnp.einsum('nd,de->ne', xf, router_w).astype(F32) + router_b.astype(F32)
    top_v, top_i = lax.top_k(logits, TOP_K)
    gates = jax.nn.softmax(top_v, axis=-1)
    comb = jnp.sum(jax.nn.one_hot(top_i, N_EXPERTS, dtype=F32) * gates[..., None], axis=1)

    def expert_step(acc, e):
        w1e, b1e, w2e, b2e, ce = e
        h = xf @ w1e + b1e
        glu_in = jnp.minimum(h[:, :D_FF], SWIGLU_LIMIT)
        up = jnp.clip(h[:, D_FF:], -SWIGLU_LIMIT, SWIGLU_LIMIT)
        glu = glu_in * jax.nn.sigmoid(SWIGLU_ALPHA * glu_in)
        y = ((up + 1.0) * glu) @ w2e + b2e
        return acc + ce[:, None] * y.astype(F32), None

    acc, _ = lax.scan(expert_step, jnp.zeros((B * T, D), F32), (w1, b1, w2, b2, comb.T))
    return acc.astype(x.dtype).reshape(B, T, D)


def _hybrid_layer(x, cos, sin, w_in, ret_gn_g, ret_gn_b, conv_w, conv_b, rg_wx, rg_bx, rg_wa, rg_ba,
                  rg_lambda, w_out, ln1_g, ln1_b, router_w, router_b, exp_w1, exp_b1, exp_w2, exp_b2,
                  ln2_g, ln2_b):
    B, T, _ = x.shape
    proj = jnp.einsum('btd,dp->btp', x, w_in)
    (a_q, a_k, a_v, r_q, r_k, r_v, r_g, c_x, c_g, d_q, d_k, d_v, d_qi, d_ki, d_w) = jnp.split(
        proj, np.cumsum(IN_SIZES)[:-1].tolist(), axis=-1)
    heads = lambda t: t.reshape(B, T, GROUP_HEADS, HEAD_DIM)

    o_a = _moba_attention(_rope(heads(a_q), cos, sin), _rope(heads(a_k), cos, sin), heads(a_v))

    rq = _rope(heads(r_q).astype(F32), cos, sin)
    rk = _rope(heads(r_k).astype(F32), cos, sin) * (HEAD_DIM ** -0.5)
    o_r = _retention(rq, rk, heads(r_v).astype(F32))
    o_r = _layer_norm(o_r, ret_gn_g.reshape(GROUP_HEADS, HEAD_DIM), ret_gn_b.reshape(GROUP_HEADS, HEAD_DIM))
    o_r = (o_r.reshape(B, T, GROUP_WIDTH) * jax.nn.silu(r_g.astype(F32))).astype(x.dtype)

    o_c = _rg_lru(c_x, c_g, conv_w, conv_b, rg_wx, rg_bx, rg_wa, rg_ba, rg_lambda)

    qi = _rope(d_qi.reshape(B, T, IDX_HEADS, IDX_DIM), cos, sin)
    ki = _rope(d_ki.reshape(B, T, 1, IDX_DIM), cos, sin)[:, :, 0]
    o_d = _dsa_attention(_rope(heads(d_q), cos, sin), _rope(heads(d_k), cos, sin), heads(d_v), qi, ki, d_w)

    mixed = jnp.concatenate([o_a.reshape(B, T, GROUP_WIDTH), o_r, o_c, o_d.reshape(B, T, GROUP_WIDTH)], axis=-1)
    x = _layer_norm(ALPHA * x + jnp.einsum('btm,md->btd', mixed, w_out), ln1_g, ln1_b)
    x = _layer_norm(ALPHA * x + _moe(x, router_w, router_b, exp_w1, exp_b1, exp_w2, exp_b2), ln2_g, ln2_b)
    return x


def setup_inputs(seed: int = 0) -> dict:
    key = jax.random.key(seed)
    ks = jax.random.split(key, 24)
    nrm = lambda k, shape, s: jax.random.normal(k, shape, F32) * s
    u = jax.random.uniform(ks[9], (DEPTH, GROUP_WIDTH), F32, 0.9, 0.999)
    a0 = u ** (1.0 / RG_C)
    return {
        'x': jax.random.normal(ks[0], (BATCH, SEQ, D_MODEL), F32),
        'w_in': nrm(ks[1], (DEPTH, D_MODEL, IN_WIDTH), D_MODEL ** -0.5),
        'ret_gn_g': 1.0 + nrm(ks[2], (DEPTH, GROUP_WIDTH), 0.02),
        'ret_gn_b': nrm(ks[3], (DEPTH, GROUP_WIDTH), 0.02),
        'conv_w': nrm(ks[4], (DEPTH, RG_CONV, GROUP_WIDTH), RG_CONV ** -0.5),
        'conv_b': nrm(ks[5], (DEPTH, GROUP_WIDTH), 0.02),
        'rg_wx': nrm(ks[6], (DEPTH, RG_BLOCKS, RG_BLOCK_W, RG_BLOCK_W), RG_BLOCK_W ** -0.5),
        'rg_bx': nrm(ks[7], (DEPTH, GROUP_WIDTH), 0.02),
        'rg_wa': nrm(ks[8], (DEPTH, RG_BLOCKS, RG_BLOCK_W, RG_BLOCK_W), RG_BLOCK_W ** -0.5),
        'rg_ba': nrm(ks[10], (DEPTH, GROUP_WIDTH), 0.02),
        'rg_lambda': jnp.log(a0) - jnp.log1p(-a0),
        'w_out': nrm(ks[11], (DEPTH, MIX_WIDTH, D_MODEL), BETA * MIX_WIDTH ** -0.5),
        'ln1_g': 1.0 + nrm(ks[12], (DEPTH, D_MODEL), 0.02),
        'ln1_b': nrm(ks[13], (DEPTH, D_MODEL), 0.02),
        'router_w': nrm(ks[14], (DEPTH, D_MODEL, N_EXPERTS), D_MODEL ** -0.5),
        'router_b': nrm(ks[15], (DEPTH, N_EXPERTS), 0.01),
        'exp_w1': nrm(ks[16], (DEPTH, N_EXPERTS, D_MODEL, 2 * D_FF), D_MODEL ** -0.5),
        'exp_b1': nrm(ks[17], (DEPTH, N_EXPERTS, 2 * D_FF), 0.02),
        'exp_w2': nrm(ks[18], (DEPTH, N_EXPERTS, D_FF, D_MODEL), BETA * D_FF ** -0.5),
        'exp_b2': nrm(ks[19], (DEPTH, N_EXPERTS, D_MODEL), 0.02),
        'ln2_g': 1.0 + nrm(ks[20], (DEPTH, D_MODEL), 0.02),
        'ln2_b': nrm(ks[21], (DEPTH, D_MODEL), 0.02),
    }


def reference(x, w_in, ret_gn_g, ret_gn_b, conv_w, conv_b, rg_wx, rg_bx, rg_wa, rg_ba, rg_lambda, w_out,
              ln1_g, ln1_b, router_w, router_b, exp_w1, exp_b1, exp_w2, exp_b2, ln2_g, ln2_b):
    cos, sin = _rope_tables(x.shape[1])
    for l in range(DEPTH):
        x = _hybrid_layer(x, cos, sin, w_in[l], ret_gn_g[l], ret_gn_b[l], conv_w[l], conv_b[l], rg_wx[l],
                          rg_bx[l], rg_wa[l], rg_ba[l], rg_lambda[l], w_out[l], ln1_g[l], ln1_b[l],
                          router_w[l], router_b[l], exp_w1[l], exp_b1[l], exp_w2[l], exp_b2[l],
                          ln2_g[l], ln2_b[l])
    return x
```

```python
import numpy as np
from contextlib import ExitStack
import concourse.bass as bass
import concourse.mybir as mybir
from concourse.bass_utils import run_bass_kernel_spmd

F32 = mybir.dt.float32
BF16 = mybir.dt.bfloat16
AF = mybir.ActivationFunctionType
ALU = mybir.AluOpType
AX = mybir.AxisListType

D = 1024
T = 4096
DEPTH = 2
NT = T // 128
NG = T // 512
NEG = -30000.0
ALPHA = (2 * DEPTH) ** 0.25
LN_EPS = 1e-5
N_EXPERTS = 32

_off = {}
_o = 0
for _n, _s in (("a_q", 256), ("a_k", 256), ("a_v", 256), ("r_q", 256), ("r_k", 256), ("r_v", 256),
               ("r_g", 256), ("c_x", 256), ("c_g", 256), ("d_q", 256), ("d_k", 256), ("d_v", 256),
               ("d_qi", 512), ("d_ki", 64), ("d_w", 8)):
    _off[_n] = (_o, _s)
    _o += _s
IN_WIDTH = _o


def _cols(name):
    o, s = _off[name]
    return np.arange(o, o + s)


def _swap(c):
    c = c.reshape(-1, 64)
    return np.concatenate([c[:, 32:], c[:, :32]], axis=1).reshape(-1)


ROPE_NAMES = ("a_q", "a_k", "r_q", "r_k", "d_q", "d_k", "d_qi", "d_ki")
FM_ROPE_COLS = np.concatenate([_cols(n) for n in ROPE_NAMES])
FM_ROPE_SW = np.concatenate([_swap(_cols(n)) for n in ROPE_NAMES])
FM_PLAIN_COLS = np.concatenate([_cols("c_x"), _cols("c_g")])
DW_COLS = _cols("d_w")
TM_COLS = np.concatenate([_cols("a_v"), _cols("r_v"), _cols("d_v"), _cols("r_g"), _cols("r_k"), _cols("d_w")])
NTM = TM_COLS.shape[0]
NFMC = 21
CH = dict(a_q=0, a_k=2, r_q=4, r_k=6, d_q=8, d_k=10, d_qi=12, d_ki=16, c_x=17, c_g=19)
TMO = dict(a_v=0, r_v=256, d_v=512, r_g=768, rkz=1024)
NTMO = 1280


class Tk:
    __slots__ = ("w", "r", "name", "excl")

    def __init__(self, name="", excl=False):
        self.w = {}
        self.r = {}
        self.name = name
        self.excl = excl


class Sched:
    ENG = ("pe", "act", "dve", "pool", "sp")

    def __init__(self, nc, es):
        self.nc = nc
        self.es = es
        self.eng = {"pe": nc.tensor, "act": nc.scalar, "dve": nc.vector,
                    "pool": nc.gpsimd, "sp": nc.sync}
        self.sem = {}
        self.cnt = {}
        for k in self.ENG:
            self.sem[k] = es.enter_context(nc.semaphore("s_" + k))
            self.cnt[k] = 0
        self.seen = {k: {} for k in self.ENG}
        self.ndsem = 0
        self.nwaits = 0
        self.free_dsems = []

    def dsem(self):
        if self.free_dsems:
            return self.free_dsems.pop()
        self.ndsem += 1
        key = "d%d" % self.ndsem
        self.sem[key] = self.es.enter_context(self.nc.semaphore(key))
        self.cnt[key] = 0
        return key

    def release_dsems(self, keys):
        self.free_dsems.extend(keys)

    def _wait(self, e, deps):
        seen = self.seen[e]
        for key, val in deps.items():
            if key == "pe" and e == "pe":
                continue
            if seen.get(key, 0) >= val:
                continue
            self.eng[e].wait_ge(self.sem[key], val)
            self.nwaits += 1
            seen[key] = val

    @staticmethod
    def _merge(d, s):
        for k, v in s.items():
            if d.get(k, 0) < v:
                d[k] = v

    def _deps(self, reads, writes):
        deps = {}
        for t in reads:
            self._merge(deps, t.w)
            if t.excl:
                self._merge(deps, t.r)
        for t in writes:
            self._merge(deps, t.w)
            self._merge(deps, t.r)
        return deps

    def op(self, e, fn, reads=(), writes=()):
        self._wait(e, self._deps(reads, writes))
        ins = fn(self.eng[e])
        self.cnt[e] += 1
        ins.then_inc(self.sem[e], 1)
        me = {e: self.cnt[e]}
        for t in reads:
            self._merge(t.r, me)
        for t in writes:
            t.w = dict(me)
            t.r = {}
        return ins

    def dma(self, q, dkey, out, in_, reads=(), writes=(), chain=False, **kw):
        deps = self._deps(reads, writes)
        if not chain and self.cnt[dkey] > 0:
            self._merge(deps, {dkey: self.cnt[dkey]})
        self._wait(q, deps)
        ins = self.eng[q].dma_start(out=out, in_=in_, **kw)
        self.cnt[dkey] += 16
        ins.then_inc(self.sem[dkey], 16)
        me = {dkey: self.cnt[dkey]}
        for t in reads:
            self._merge(t.r, me)
        for t in writes:
            if chain:
                self._merge(t.w, me)
            else:
                t.w = dict(me)
            t.r = {}
        return ins

    def wait_all(self, e, toks):
        deps = {}
        for t in toks:
            self._merge(deps, t.w)
            self._merge(deps, t.r)
        self._wait(e, deps)

    def barrier(self):
        deps = {k: v for k, v in self.cnt.items() if v > 0}
        for e in self.ENG:
            self._wait(e, deps)
        self.free_dsems = [k for k in self.cnt if k.startswith("d") and k[1:].isdigit()]


class Rot:
    def __init__(self, S, aps):
        self.S = S
        self.aps = list(aps)
        self.tk = [Tk() for _ in self.aps]
        self.dk = [None] * len(self.aps)
        self.i = -1

    def next(self):
        self.i = (self.i + 1) % len(self.aps)
        return self.aps[self.i], self.tk[self.i]

    def dkey(self):
        if self.dk[self.i] is None:
            self.dk[self.i] = self.S.dsem()
        return self.dk[self.i]


_SBN = [0]


def sb(nc, es, name, shape, dt):
    _SBN[0] += 1
    return es.enter_context(nc.sbuf_tensor("%s_%d" % (name, _SBN[0]), list(shape), dt))


class Ctx:
    pass


def bcast_row(C, es, name, src_row, N):
    nc, S = C.nc, C.S
    W = min(N, 512)
    row = sb(nc, es, name + "_row", [1, W], F32)
    out = sb(nc, es, name, [128, N], F32)
    t_row, t_out = Tk(), Tk()
    dk = S.dsem()
    for c0 in range(0, N, 512):
        w = min(512, N - c0)
        S.dma("sp", dk, row[0:1, 0:w], src_row[:, c0:c0 + w], writes=[t_row])
        ps, tp = C.psum.next()
        S.op("pe", lambda e: e.matmul(ps[:, 0:w], lhsT=C.ones_row[0:1, :], rhs=row[0:1, 0:w], start=True, stop=True),
             reads=[t_row, C.t_ones], writes=[tp])
        S.op("act", lambda e: e.copy(out=out[:, c0:c0 + w], in_=ps[:, 0:w]), reads=[tp], writes=[t_out])
    return out, t_out

def phase_A(C, l, x_src):
    nc, S = C.nc, C.S
    S.barrier()
    with ExitStack() as es:
        xT = sb(nc, es, "A_xT", [128, 8, T], BF16)
        t_xT = Tk()
        cosT = sb(nc, es, "A_cosT", [128, T], F32)
        sinT = sb(nc, es, "A_sinT", [128, T], F32)
        t_tab = Tk()
        dk_tab = S.dsem()
        S.dma("sp", dk_tab, cosT[:], C.cosT[:, :], writes=[t_tab])
        S.dma("sp", dk_tab, sinT[:], C.sinT[:, :], writes=[t_tab], chain=True)
        cosTM = sb(nc, es, "A_cosTM", [128, NT, 32], F32)
        sinTM = sb(nc, es, "A_sinTM", [128, NT, 32], F32)
        S.dma("sp", dk_tab, cosTM[:], C.cosTM.rearrange("(n p) c -> p n c", p=128), writes=[t_tab], chain=True)
        S.dma("sp", dk_tab, sinTM[:], C.sinTM.rearrange("(n p) c -> p n c", p=128), writes=[t_tab], chain=True)
        zt = sb(nc, es, "A_zeta", [128, 4], F32)
        S.dma("sp", dk_tab, zt[:], C.zeta8[:, :], writes=[t_tab], chain=True)
        sel2 = sb(nc, es, "A_sel2", [8, 512], BF16)
        t_sel = Tk()
        dk_sel = S.dsem()
        S.dma("pool", dk_sel, sel2[:], C.sel2[:, :], writes=[t_sel])

        xin = Rot(S, [sb(nc, es, f"A_xin{i}", [128, D], F32) for i in range(2)])
        for n in range(NT):
            xt_, tx = xin.next()
            S.dma("sp", xin.dkey(), xt_[:], x_src[n * 128:(n + 1) * 128, :], writes=[tx])
            for half in range(2):
                ps, tp = C.psum.next()
                for j in range(4):
                    k = half * 4 + j
                    S.op("pe", lambda e: e.transpose(ps[:, j * 128:(j + 1) * 128], xt_[:, k * 128:(k + 1) * 128], C.ident_f[:]),
                         reads=[tx, C.t_const], writes=[tp])
                dst = xT[:, half * 4:(half + 1) * 4, n * 128:(n + 1) * 128]
                src = ps[:].rearrange("p (k c) -> p k c", k=4)
                if half == 0:
                    S.op("act", lambda e: e.copy(out=dst, in_=src), reads=[tp], writes=[t_xT])
                else:
                    S.op("dve", lambda e: e.tensor_copy(out=dst, in_=src), reads=[tp], writes=[t_xT])

        wtm = sb(nc, es, "A_wtm", [128, 8, NTM], BF16)
        t_wtm = Tk()
        dk_wtm = S.dsem()
        S.dma("pool", dk_wtm, wtm[:], C.w_tm[l].rearrange("(k p) c -> p k c", p=128), writes=[t_wtm])
        tmo = Rot(S, [sb(nc, es, f"A_tmo{i}", [128, NTMO], BF16) for i in range(2)])
        rk32 = Rot(S, [sb(nc, es, f"A_rk{i}", [128, 4, 64], F32) for i in range(2)])
        rkt = Rot(S, [sb(nc, es, f"A_rkt{i}", [128, 4, 64], F32) for i in range(2)])
        rku = Rot(S, [sb(nc, es, f"A_rku{i}", [128, 4, 64], F32) for i in range(2)])
        for n in range(NT):
            ot, to = tmo.next()
            banks = []
            for (c0, cn) in ((0, 512), (512, 512), (1024, NTM - 1024)):
                ps, tp = C.psum.next()
                for k in range(8):
                    S.op("pe", lambda e: e.matmul(ps[:, 0:cn], lhsT=xT[:, k, n * 128:(n + 1) * 128], rhs=wtm[:, k, c0:c0 + cn],
                                                  start=(k == 0), stop=(k == 7)),
                         reads=[t_xT, t_wtm], writes=[tp])
                banks.append((ps, tp))
            S.op("act", lambda e: e.copy(out=ot[:, 0:512], in_=banks[0][0][:, 0:512]), reads=[banks[0][1]], writes=[to])
            S.op("act", lambda e: e.copy(out=ot[:, 512:1024], in_=banks[1][0][:, 0:512]), reads=[banks[1][1]], writes=[to])
            ps2, tp2 = banks[2]
            S.op("act", lambda e: e.activation(out=C.sgn[:, n, :], in_=ps2[:, 256:264], func=AF.Sign), reads=[tp2], writes=[C.t_sgn])
            r32, tr = rk32.next()
            S.op("dve", lambda e: e.tensor_copy(out=r32[:], in_=ps2[:, 0:256].rearrange("p (h d) -> p h d", h=4)), reads=[tp2], writes=[tr])
            cb = cosTM[:, n, :].unsqueeze(1).to_broadcast([128, 4, 32])
            sbb = sinTM[:, n, :].unsqueeze(1).to_broadcast([128, 4, 32])
            ra, tra = rkt.next()
            rb, trb = rku.next()
            x1 = r32[:, :, 0:32]
            x2 = r32[:, :, 32:64]
            S.op("dve", lambda e: e.tensor_tensor(out=ra[:, :, 0:32], in0=x1, in1=cb, op=ALU.mult), reads=[tr, t_tab], writes=[tra])
            S.op("dve", lambda e: e.tensor_tensor(out=ra[:, :, 32:64], in0=x2, in1=cb, op=ALU.mult), reads=[tr, t_tab], writes=[tra])
            S.op("dve", lambda e: e.tensor_tensor(out=rb[:, :, 0:32], in0=x2, in1=sbb, op=ALU.mult), reads=[tr, t_tab], writes=[trb])
            S.op("dve", lambda e: e.tensor_tensor(out=rb[:, :, 32:64], in0=x1, in1=sbb, op=ALU.mult), reads=[tr, t_tab], writes=[trb])
            S.op("dve", lambda e: e.tensor_tensor(out=ra[:, :, 0:32], in0=ra[:, :, 0:32], in1=rb[:, :, 0:32], op=ALU.subtract), reads=[tra, trb], writes=[tra])
            S.op("dve", lambda e: e.tensor_tensor(out=ra[:, :, 32:64], in0=ra[:, :, 32:64], in1=rb[:, :, 32:64], op=ALU.add), reads=[tra, trb], writes=[tra])
            S.op("dve", lambda e: e.tensor_tensor(out=ot[:, 1024:1280].rearrange("p (h d) -> p h d", h=4), in0=ra[:],
                                                  in1=zt[:].unsqueeze(2).to_broadcast([128, 4, 64]), op=ALU.mult),
                 reads=[tra, t_tab], writes=[to])
            S.dma("sp", tmo.dkey(), C.TMO[n * 128:(n + 1) * 128, :], ot[:], reads=[to], writes=[C.t_TMO], chain=True)

        absw = sb(nc, es, "A_absw", [8, T], BF16)
        t_absw = Tk()
        wdw = sb(nc, es, "A_wdw", [128, 8, 8], BF16)
        t_wdw = Tk()
        dk_wdw = S.dsem()
        S.dma("pool", dk_wdw, wdw[:], C.w_dw[l].rearrange("(k p) c -> p k c", p=128), writes=[t_wdw])
        for g in range(NG):
            ps, tp = C.psum.next()
            for k in range(8):
                S.op("pe", lambda e: e.matmul(ps[0:8, :], lhsT=wdw[:, k, :], rhs=xT[:, k, g * 512:(g + 1) * 512],
                                              start=(k == 0), stop=(k == 7)), reads=[t_xT, t_wdw], writes=[tp])
            S.op("act", lambda e: e.activation(out=absw[:, g * 512:(g + 1) * 512], in_=ps[0:8, :], func=AF.Abs), reads=[tp], writes=[t_absw])

        wb = Rot(S, [sb(nc, es, f"A_wb{i}", [128, 8, 512], BF16) for i in range(2)])
        ws = Rot(S, [sb(nc, es, f"A_ws{i}", [128, 8, 512], BF16) for i in range(2)])
        t1r = Rot(S, [sb(nc, es, f"A_t1{i}", [128, 512], F32) for i in range(2)])
        t2r = Rot(S, [sb(nc, es, f"A_t2{i}", [128, 512], F32) for i in range(2)])
        outr = Rot(S, [sb(nc, es, f"A_out{i}", [128, 512], BF16) for i in range(4)])
        groups = [(0, 4, True, 0), (4, 4, True, 512), (8, 4, True, 1024), (12, 4, True, 1536),
                  (16, 1, True, 2048), (17, 4, False, 0)]
        for (c0, ncn, rope, wc0) in groups:
            ncols = 64 if c0 == 16 else ncn * 128
            w_, tw = wb.next()
            src = (C.w_fmr if rope else C.w_fmp)[l]
            S.dma("pool", wb.dkey(), w_[:, :, 0:ncols], src[:, wc0:wc0 + ncols].rearrange("(k p) c -> p k c", p=128), writes=[tw])
            if rope:
                wsw, tws = ws.next()
                S.dma("pool", ws.dkey(), wsw[:, :, 0:ncols], C.w_fms[l][:, wc0:wc0 + ncols].rearrange("(k p) c -> p k c", p=128), writes=[tws])
            for g in range(NG):
                tsl = slice(g * 512, (g + 1) * 512)
                for ci in range(ncn):
                    c = c0 + ci
                    M = 64 if c == 16 else 128
                    wsl = slice(ci * 128, ci * 128 + M)
                    pX, tpX = C.psum.next()
                    for k in range(8):
                        S.op("pe", lambda e: e.matmul(pX[0:M, :], lhsT=w_[:, k, wsl], rhs=xT[:, k, tsl], start=(k == 0), stop=(k == 7)),
                             reads=[t_xT, tw], writes=[tpX])
                    o_, to_ = outr.next()
                    if not rope:
                        S.op("act", lambda e: e.copy(out=o_[0:M, :], in_=pX[0:M, :]), reads=[tpX], writes=[to_])
                    else:
                        pS, tpS = C.psum.next()
                        for k in range(8):
                            S.op("pe", lambda e: e.matmul(pS[0:M, :], lhsT=wsw[:, k, wsl], rhs=xT[:, k, tsl], start=(k == 0), stop=(k == 7)),
                                 reads=[t_xT, tws], writes=[tpS])
                        a1, ta1 = t1r.next()
                        a2, ta2 = t2r.next()
                        S.op("dve", lambda e: e.tensor_tensor(out=a1[0:M, :], in0=pX[0:M, :], in1=cosT[0:M, tsl], op=ALU.mult),
                             reads=[tpX, t_tab], writes=[ta1])
                        S.op("dve", lambda e: e.tensor_tensor(out=a2[0:M, :], in0=pS[0:M, :], in1=sinT[0:M, tsl], op=ALU.mult),
                             reads=[tpS, t_tab], writes=[ta2])
                        if 12 <= c < 16:
                            S.op("dve", lambda e: e.tensor_tensor(out=a1[:], in0=a1[:], in1=a2[:], op=ALU.add), reads=[ta1, ta2], writes=[ta1])
                            pB, tpB = C.psum.next()
                            S.op("pe", lambda e: e.matmul(pB[:], lhsT=sel2[:, (c - 12) * 128:(c - 11) * 128], rhs=absw[:, tsl], start=True, stop=True),
                                 reads=[t_sel, t_absw], writes=[tpB])
                            S.op("dve", lambda e: e.tensor_tensor(out=o_[:], in0=a1[:], in1=pB[:], op=ALU.mult), reads=[ta1, tpB], writes=[to_])
                        else:
                            S.op("dve", lambda e: e.tensor_tensor(out=o_[0:M, :], in0=a1[0:M, :], in1=a2[0:M, :], op=ALU.add),
                                 reads=[ta1, ta2], writes=[to_])
                    S.dma("sp", outr.dkey(), C.FMT[c * 128:c * 128 + M, tsl], o_[0:M, :], reads=[to_], writes=[C.t_FMT], chain=True)


def host_consts():
    inv = (10000.0 ** (-np.arange(0, 64, 2, dtype=np.float32) / 64)).astype(np.float32)
    ang = np.arange(T, dtype=np.float32)[:, None] * inv[None, :]
    cos = np.cos(ang).astype(np.float32)
    sin = np.sin(ang).astype(np.float32)
    p = np.arange(128)
    d = p % 64
    cosT = np.ascontiguousarray(cos[:, d % 32].T)
    sgn = np.where(d < 32, -1.0, 1.0).astype(np.float32)
    sinT = np.ascontiguousarray((sin[:, d % 32] * sgn[None, :]).T)
    log_g = np.log(1.0 - 2.0 ** (-5.0 - np.arange(4, dtype=np.float32))).astype(np.float32)
    n = np.arange(128, dtype=np.float32)
    zeta8 = (np.exp(log_g[None, :] * (127.0 - n[:, None])) * 0.125).astype(np.float32)
    sel2 = np.zeros((8, 512), np.float32)
    for c in range(4):
        sel2[2 * c, c * 128:c * 128 + 64] = 1.0
        sel2[2 * c + 1, c * 128 + 64:c * 128 + 128] = 1.0
    d_mask = np.where(n[:, None] - n[None, :] >= 0, np.exp(log_g[:, None, None] * np.maximum(n[:, None] - n[None, :], 0.0)), 0.0)
    dmaskT = np.ascontiguousarray((d_mask * 0.125).transpose(2, 0, 1)).astype(np.float32)
    xi = np.exp(log_g[:, None] * (n[None, :] + 1.0))
    hp = np.arange(128) // 64
    xiT = np.stack([xi[2 * c + hp] for c in range(2)], axis=1).astype(np.float32)
    gch = np.exp(log_g * 128.0)
    gvec = np.stack([gch[2 * c + hp] for c in range(2)], axis=1).astype(np.float32)
    q = np.arange(128)
    caus = np.where(q[None, :] <= q[:, None], 0.0, NEG).astype(np.float32)
    causF = np.where(q[None, :] <= q[:, None], 0.0, -1e30).astype(np.float32)
    return dict(cosT=cosT, sinT=sinT, cosTM=cos, sinTM=sin, zeta8=zeta8, sel2=sel2,
                ident=np.eye(128, dtype=np.float32), caus=caus, causF=causF,
                dmaskT=dmaskT, xiT=xiT, gvec=gvec)


def prep_weights(inp):
    w_in = np.asarray(inp["w_in"], dtype=np.float32)
    out = {}
    out["w_fmr"] = np.ascontiguousarray(w_in[:, :, FM_ROPE_COLS])
    out["w_fms"] = np.ascontiguousarray(w_in[:, :, FM_ROPE_SW])
    out["w_fmp"] = np.ascontiguousarray(w_in[:, :, FM_PLAIN_COLS])
    out["w_dw"] = np.ascontiguousarray(w_in[:, :, DW_COLS])
    out["w_tm"] = np.ascontiguousarray(w_in[:, :, TM_COLS])
    if "conv_w" in inp:
        L = w_in.shape[0]
        pc = np.zeros((L, 128, 16), np.float32)
        cwt = np.asarray(inp["conv_w"], np.float32)
        for c in range(2):
            for i in range(4):
                pc[:, :, c * 4 + i] = cwt[:, i, c * 128:(c + 1) * 128]
            pc[:, :, 8 + c] = np.asarray(inp["conv_b"])[:, c * 128:(c + 1) * 128]
            pc[:, :, 10 + c] = np.asarray(inp["rg_bx"])[:, c * 128:(c + 1) * 128]
            pc[:, :, 12 + c] = np.asarray(inp["rg_ba"])[:, c * 128:(c + 1) * 128]
            pc[:, :, 14 + c] = np.asarray(inp["rg_lambda"])[:, c * 128:(c + 1) * 128]
        out["rg_pc"] = pc
        wbd = np.zeros((L, 128, 4, 128), np.float32)
        for k, nm in enumerate(("rg_wx", "rg_wa")):
            w = np.asarray(inp[nm], np.float32)
            for c in range(2):
                for b in range(2):
                    wbd[:, b * 64:(b + 1) * 64, k * 2 + c, b * 64:(b + 1) * 64] = w[:, 2 * c + b]
        out["rg_wbd"] = wbd
    if "exp_b1" in inp:
        b1 = np.asarray(inp["exp_b1"], np.float32)
        out["exp_b1p"] = np.ascontiguousarray(b1.reshape(b1.shape[0], b1.shape[1], 16, 128).transpose(0, 1, 3, 2))
    return out


def build(cfg):
    nc = bass.Bass("TRN2", target_bir_lowering=False)
    C = Ctx()
    C.nc = nc
    C.cfg = cfg
    NLD = cfg.get("decl_depth", DEPTH)
    NLAY = cfg.get("layers", DEPTH)

    def din(name, shape, dt=F32):
        return nc.dram_tensor(name, list(shape), dt, kind="ExternalInput").ap()

    def dscr(name, shape, dt):
        kind = "ExternalOutput" if name in cfg.get("debug_out", ()) else ("ExternalInput" if name in cfg.get("debug_in", ()) else "Internal")
        return nc.dram_tensor(name, list(shape), dt, kind=kind).ap()

    x_in = din("x", [T, D])
    C.cosT = din("cosT", [128, T]); C.sinT = din("sinT", [128, T])
    C.cosTM = din("cosTM", [T, 32]); C.sinTM = din("sinTM", [T, 32])
    C.zeta8 = din("zeta8", [128, 4]); C.sel2 = din("sel2", [8, 512])
    ident_d = din("ident", [128, 128])
    C.w_fmr = din("w_fmr", [NLD, D, 2112]); C.w_fms = din("w_fms", [NLD, D, 2112])
    C.w_fmp = din("w_fmp", [NLD, D, 512]); C.w_dw = din("w_dw", [NLD, D, 8])
    C.w_tm = din("w_tm", [NLD, D, NTM])
    caus_d = din("caus", [128, 128]); causF_d = din("causF", [128, 128])
    C.dmaskT = din("dmaskT", [128, 4, 128]); C.xiT = din("xiT", [128, 2, 128]); C.gvec = din("gvec", [128, 2])
    C.ret_gn_g = din("ret_gn_g", [NLD, 256]); C.ret_gn_b = din("ret_gn_b", [NLD, 256])
    C.rg_pc = din("rg_pc", [NLD, 128, 16]); C.rg_wbd = din("rg_wbd", [NLD, 128, 4, 128])
    C.w_out = din("w_out", [NLD, D, D]); C.router_w = din("router_w", [NLD, D, 32]); C.router_b = din("router_b", [NLD, 32])
    C.ln1_g = din("ln1_g", [NLD, D]); C.ln1_b = din("ln1_b", [NLD, D])
    C.X1 = dscr("X1", [T, D], F32); C.X1T = dscr("X1T", [D, T], BF16)
    C.X2 = dscr("X2", [T, D], F32); C.t_X2 = Tk()
    C.OUT = nc.dram_tensor("out", [T, D], F32, kind="ExternalOutput").ap()
    C.exp_w1 = din("exp_w1", [NLD, N_EXPERTS, D, 2 * D]); C.exp_w2 = din("exp_w2", [NLD, N_EXPERTS, D, D])
    C.exp_b1p = din("exp_b1p", [NLD, N_EXPERTS, 128, 16]); C.exp_b2 = din("exp_b2", [NLD, N_EXPERTS, D])
    C.ln2_g = din("ln2_g", [NLD, D]); C.ln2_b = din("ln2_b", [NLD, D])
    C.t_X1 = Tk(); C.t_X1T = Tk(); C.t_xsrc = Tk(); C.t_comb = Tk()
    if "COMBD" in cfg.get("debug_out", ()):
        C.COMBD = nc.dram_tensor("COMBD", [T, 32], F32, kind="ExternalOutput").ap()
    C.MIXT = dscr("MIXT", [1024, T], BF16)
    if cfg.get("dbg_dsa") is not None:
        C.DBG1 = nc.dram_tensor("DBG1", [128, T], F32, kind="ExternalOutput").ap()
        C.DBG2 = nc.dram_tensor("DBG2", [128, NT * 8], F32, kind="ExternalOutput").ap()
        C.DBG3 = nc.dram_tensor("DBG3", [128, T], BF16, kind="ExternalOutput").ap()
        C.t_dbg = Tk()
    C.t_MIXT = Tk()
    C.FMT = dscr("FMT", [NFMC * 128, T], BF16)
    C.TMO = dscr("TMO", [T, NTMO], BF16)
    C.t_FMT = Tk(); C.t_TMO = Tk()

    with ExitStack() as es:
        S = Sched(nc, es)
        C.S = S
        banks = [es.enter_context(nc.psum_tensor(f"ps{i}", [128, 512], F32)) for i in range(8)]
        C.psum = Rot(S, banks[0:6])
        C.acc = Rot(S, banks[6:8])
        for t in C.psum.tk + C.acc.tk:
            t.excl = True
        C.ident_f = sb(nc, es, "ident_f", [128, 128], F32)
        C.ident_b = sb(nc, es, "ident_b", [128, 128], BF16)
        C.t_const = Tk()
        dk = S.dsem()
        S.dma("sp", dk, C.ident_f[:], ident_d[:, :], writes=[C.t_const])
        dk2 = S.dsem()
        S.dma("pool", dk2, C.ident_b[:], ident_d[:, :], writes=[C.t_const], chain=True)
        C.caus_b = sb(nc, es, "caus_b", [128, 128], BF16)
        C.caus_f = sb(nc, es, "caus_f", [128, 128], F32)
        C.ones_f = sb(nc, es, "ones_f", [128, 64], F32)
        S.dma("pool", dk2, C.caus_b[:], caus_d[:, :], writes=[C.t_const], chain=True)
        S.dma("sp", dk, C.caus_f[:], causF_d[:, :], writes=[C.t_const], chain=True)
        C.t_ones = Tk()
        S.op("pool", lambda e: e.memset(C.ones_f[:], 1.0), writes=[C.t_ones])
        C.ones_row = sb(nc, es, "ones_row", [1, 128], F32)
        S.op("pool", lambda e: e.memset(C.ones_row[:], 1.0), writes=[C.t_ones])
        C.sgn = sb(nc, es, "sgn", [128, NT, 8], F32)
        C.COMB = sb(nc, es, "COMB", [128, NT, 32], F32)
        C.t_sgn = Tk()

        for l in range(cfg.get("layers", DEPTH)):
            if "A" in cfg["phases"]:
                phase_A(C, l, x_in if l == 0 else C.X2)
            if "M" in cfg["phases"]:
                phase_moba(C)
            if "S" in cfg["phases"]:
                phase_dsa(C)
            if "R" in cfg["phases"]:
                phase_ret(C, l)
            if "G" in cfg["phases"]:
                phase_rglru(C, l)
            x_src = x_in if l == 0 else C.X2
            if "C" in cfg["phases"]:
                phase_C(C, l, x_src)
            if "D" in cfg["phases"]:
                phase_D(C, l, C.X2 if l < NLAY - 1 else C.OUT)
        S.barrier()
        C.stats = ({k: S.cnt[k] for k in S.ENG}, S.nwaits, S.ndsem)
    return nc


def attn_group(C, A, g, heads, bias_aps, bias_tks, out_row0):
    nc, S = C.nc, C.S
    tsl = slice(g * 512, (g + 1) * 512)
    nkt = 4 * g + 4
    for h in heads:
        c, pb = h // 2, (h % 2) * 64
        oT, toT = C.acc.next()
        for kt in range(nkt):
            ps, tp = C.psum.next()
            S.op("pe", lambda e: e.matmul(ps[:], lhsT=A.KT[pb:pb + 64, c, kt * 128:(kt + 1) * 128], rhs=A.QT[pb:pb + 64, c, tsl],
                                          start=True, stop=False), reads=[A.t_KT, A.t_QT], writes=[tp])
            for jj in range(4):
                S.op("pe", lambda e: e.matmul(ps[:, jj * 128:(jj + 1) * 128], lhsT=bias_aps[jj][:, kt * 128:(kt + 1) * 128], rhs=C.ident_b[:],
                                              start=False, stop=(jj == 3)), reads=[bias_tks[jj], C.t_const], writes=[tp])
            pT, tpT = A.pT.next()
            S.op("act", lambda e: e.activation(out=pT[:], in_=ps[:], func=AF.Exp, scale=0.125), reads=[tp], writes=[tpT])
            S.op("pe", lambda e: e.matmul(oT[0:65, :], lhsT=A.V[:, kt, h, :], rhs=pT[:], start=(kt == 0), stop=(kt == nkt - 1)),
                 reads=[tpT, A.t_V], writes=[toT])
        rec, trec = A.rec.next()
        S.op("dve", lambda e: e.reciprocal(out=rec[64:65, :], in_=oT[64:65, :]), reads=[toT], writes=[trec])
        osb, tosb = A.osb.next()
        S.op("act", lambda e: e.copy(out=osb[0:64, :], in_=oT[0:64, :]), reads=[toT], writes=[tosb])
        pb_, tpb_ = C.psum.next()
        S.op("pe", lambda e: e.matmul(pb_[0:64, :], lhsT=C.ones_f[64:65, 0:64], rhs=rec[64:65, :], start=True, stop=True),
             reads=[trec, C.t_ones], writes=[tpb_])
        om, tom = A.om.next()
        S.op("dve", lambda e: e.tensor_tensor(out=om[0:64, :], in0=osb[0:64, :], in1=pb_[0:64, :], op=ALU.mult), reads=[tosb, tpb_], writes=[tom])
        S.dma("sp", A.om.dkey(), C.MIXT[out_row0 + h * 64:out_row0 + (h + 1) * 64, tsl], om[0:64, :], reads=[tom], writes=[C.t_MIXT], chain=True)


def attn_alloc(C, es, qch, kch, vcol, pref):
    nc, S = C.nc, C.S
    A = Ctx()
    A.KT = sb(nc, es, pref + "KT", [128, 2, T], BF16)
    A.t_KT = Tk()
    dk = S.dsem()
    S.dma("sp", dk, A.KT[:], C.FMT[kch * 128:(kch + 2) * 128, :].rearrange("(c p) t -> p c t", p=128), reads=[C.t_FMT], writes=[A.t_KT])
    A.V = sb(nc, es, pref + "V", [128, NT, 4, 65], BF16)
    A.t_V = Tk()
    S.op("pool", lambda e: e.memset(A.V[:, :, :, 64:65], 1.0), writes=[A.t_V])
    dk2 = S.dsem()
    for h in range(4):
        S.dma("sp", dk2, A.V[:, :, h, 0:64], C.TMO[:, vcol + h * 64:vcol + (h + 1) * 64].rearrange("(n p) d -> p n d", p=128),
              reads=[C.t_TMO], writes=[A.t_V], chain=True)
    A.QTr = Rot(S, [sb(nc, es, pref + f"QT{i}", [128, 2, 512], BF16) for i in range(2)])
    A.qch = qch
    A.pT = Rot(S, [sb(nc, es, pref + f"pT{i}", [128, 512], BF16) for i in range(4)])
    A.rec = Rot(S, [sb(nc, es, pref + f"rec{i}", [128, 512], F32) for i in range(2)])
    A.osb = Rot(S, [sb(nc, es, pref + f"osb{i}", [128, 512], F32) for i in range(2)])
    A.om = Rot(S, [sb(nc, es, pref + f"om{i}", [128, 512], BF16) for i in range(2)])
    return A


def attn_load_q(C, A, g):
    S = C.S
    q_, tq = A.QTr.next()
    S.dma("sp", A.QTr.dkey(), q_[:], C.FMT[A.qch * 128:(A.qch + 2) * 128, g * 512:(g + 1) * 512].rearrange("(c p) t -> p c t", p=128),
          reads=[C.t_FMT], writes=[tq])
    A.QT = _Shift(q_, g * 512)
    A.t_QT = tq


class _Shift:
    def __init__(self, ap, off):
        self.ap = ap
        self.off = off

    def __getitem__(self, key):
        p, c, t = key
        t = slice(t.start - self.off, t.stop - self.off)
        return self.ap[p, c, t]


def phase_moba(C):
    nc, S = C.nc, C.S
    S.barrier()
    with ExitStack() as es:
        A = attn_alloc(C, es, CH["a_q"], CH["a_k"], TMO["a_v"], "M_")
        kms = sb(nc, es, "M_kms", [128, 2, 16], F32)
        kmb = sb(nc, es, "M_kmb", [128, 2, 16], BF16)
        t_km = Tk()
        for c in range(2):
            S.op("dve", lambda e: e.tensor_reduce(out=kms[:, c, :], in_=A.KT[:, c, :].rearrange("p (n k) -> p n k", k=256), axis=AX.X, op=ALU.add),
                 reads=[A.t_KT], writes=[t_km])
        S.op("dve", lambda e: e.tensor_scalar(out=kmb[:], in0=kms[:], scalar1=1.0 / 256.0, scalar2=None, op0=ALU.mult), reads=[t_km], writes=[t_km])
        bias = Rot(S, [sb(nc, es, f"M_bias{i}", [128, T], BF16) for i in range(8)])
        gsb = Rot(S, [sb(nc, es, f"M_g{i}", [128, 16], F32) for i in range(4)])
        m8 = Rot(S, [sb(nc, es, f"M_m8{i}", [128, 8], F32) for i in range(4)])
        sbi = Rot(S, [sb(nc, es, f"M_sb{i}", [128, 16], F32) for i in range(4)])
        for g in range(NG):
            attn_load_q(C, A, g)
            for h in range(4):
                c, pb = h // 2, (h % 2) * 64
                baps, btks = [], []
                for jj in range(4):
                    j = 4 * g + jj
                    own = j // 2
                    b_, tb = bias.next()
                    if own > 0:
                        pg, tpg = C.psum.next()
                        S.op("pe", lambda e: e.matmul(pg[:, 0:16], lhsT=A.QT[pb:pb + 64, c, slice(j * 128, (j + 1) * 128)], rhs=kmb[pb:pb + 64, c, :],
                                                      start=True, stop=True), reads=[A.t_QT, t_km], writes=[tpg])
                        g_, tg = gsb.next()
                        S.op("dve", lambda e: e.tensor_copy(out=g_[:], in_=pg[:, 0:16]), reads=[tpg], writes=[tg])
                        if own < 16:
                            S.op("dve", lambda e: e.memset(g_[:, own:16], -1e30), writes=[tg])
                        m_, tm = m8.next()
                        S.op("dve", lambda e: e.max(out=m_[:], in_=g_[:]), reads=[tg], writes=[tm])
                        S.op("dve", lambda e: e.tensor_scalar(out=m_[:, 2:3], in0=m_[:, 2:3], scalar1=-1e29, scalar2=None, op0=ALU.max), reads=[tm], writes=[tm])
                        s_, ts = sbi.next()
                        S.op("dve", lambda e: e.tensor_scalar(out=s_[:], in0=g_[:], scalar1=m_[:, 2:3], scalar2=NEG, op0=ALU.is_lt, op1=ALU.mult),
                             reads=[tg, tm], writes=[ts])
                        S.op("pool", lambda e: e.tensor_copy(out=b_[:, 0:own * 256].rearrange("p (n k) -> p n k", k=256),
                                                              in_=s_[:, 0:own].unsqueeze(2).to_broadcast([128, own, 256])), reads=[ts], writes=[tb])
                    if j % 2 == 1:
                        S.op("pool", lambda e: e.memset(b_[:, (j - 1) * 128:j * 128], 0.0), writes=[tb])
                    S.op("pool", lambda e: e.tensor_copy(out=b_[:, j * 128:(j + 1) * 128], in_=C.caus_b[:]), reads=[C.t_const], writes=[tb])
                    if jj < 3:
                        S.op("pool", lambda e: e.memset(b_[:, (j + 1) * 128:(4 * g + 4) * 128], NEG), writes=[tb])
                    baps.append(b_)
                    btks.append(tb)
                attn_group(C, A, g, [h], baps, btks, 0)


DSA_ITERS = 16


def phase_dsa(C):
    nc, S = C.nc, C.S
    S.barrier()
    with ExitStack() as es:
        A = attn_alloc(C, es, CH["d_q"], CH["d_k"], TMO["d_v"], "D_")
        KI = sb(nc, es, "D_KI", [128, T], BF16)
        t_KI = Tk()
        dk = S.dsem()
        r0 = CH["d_ki"] * 128
        S.dma("sp", dk, KI[0:64, :], C.FMT[r0:r0 + 64, :], reads=[C.t_FMT], writes=[t_KI])
        S.dma("sp", dk, KI[64:128, :], C.FMT[r0:r0 + 64, :], reads=[C.t_FMT], writes=[t_KI], chain=True)
        if C.cfg.get("dbg_dsa") is not None:
            dkd3 = S.dsem()
            S.dma("sp", dkd3, C.DBG3[:, :], KI[:], reads=[t_KI], writes=[C.t_dbg])
        QI = Rot(S, [sb(nc, es, f"D_QI{i}", [128, 4, 512], BF16) for i in range(2)])
        accr = Rot(S, [sb(nc, es, f"D_acc{i}", [128, T], F32) for i in range(2)])
        rel = Rot(S, [sb(nc, es, f"D_rel{i}", [128, 512], F32) for i in range(3)])
        bias = Rot(S, [sb(nc, es, f"D_bias{i}", [128, T], BF16) for i in range(8)])
        sm = Rot(S, [sb(nc, es, f"D_sm{i}", [128, 8], F32) for i in range(2)])
        stp = Rot(S, [sb(nc, es, f"D_stp{i}", [128, DSA_ITERS], F32) for i in range(2)])
        pw = sb(nc, es, "D_pw", [128, DSA_ITERS], F32)
        thr0 = sb(nc, es, "D_thr0", [128, 1], F32)
        t_pw = Tk()
        for it in range(DSA_ITERS):
            S.op("pool", lambda e: e.memset(pw[:, it:it + 1], 2.0 ** (-(it + 1))), writes=[t_pw])
        S.op("pool", lambda e: e.memset(thr0[:], -1e29), writes=[t_pw])
        for g in range(NG):
            attn_load_q(C, A, g)
            qi_, tqi = QI.next()
            S.dma("sp", QI.dkey(), qi_[:], C.FMT[CH["d_qi"] * 128:(CH["d_qi"] + 4) * 128, g * 512:(g + 1) * 512].rearrange("(c p) t -> p c t", p=128),
                  reads=[C.t_FMT], writes=[tqi])
            baps, btks = [], []
            for jj in range(4):
                j = 4 * g + jj
                L = (j + 1) * 128
                acc_, tacc = accr.next()
                for kc in range((L + 511) // 512):
                    w = min(512, L - kc * 512)
                    ksl = slice(kc * 512, kc * 512 + w)
                    for h in range(8):
                        c, pb = h // 2, (h % 2) * 64
                        ps, tp = C.psum.next()
                        S.op("pe", lambda e: e.matmul(ps[:, 0:w], lhsT=qi_[pb:pb + 64, c, jj * 128:(jj + 1) * 128], rhs=KI[pb:pb + 64, ksl],
                                                      start=True, stop=True), reads=[tqi, t_KI], writes=[tp])
                        r_, tr = rel.next()
                        S.op("act", lambda e: e.activation(out=r_[:, 0:w], in_=ps[:, 0:w], func=AF.Relu), reads=[tp], writes=[tr])
                        if h == 0:
                            S.op("dve", lambda e: e.tensor_scalar(out=acc_[:, ksl], in0=r_[:, 0:w], scalar1=C.sgn[:, j, 0:1], scalar2=None, op0=ALU.mult),
                                 reads=[tr, C.t_sgn], writes=[tacc])
                        else:
                            S.op("dve", lambda e: e.scalar_tensor_tensor(out=acc_[:, ksl], in0=r_[:, 0:w], scalar=C.sgn[:, j, h:h + 1], in1=acc_[:, ksl],
                                                                         op0=ALU.mult, op1=ALU.add), reads=[tr, C.t_sgn, tacc], writes=[tacc])
                b_, tb = bias.next()
                s_, ts = sm.next()
                if C.cfg.get("dbg_dsa") is not None and j == C.cfg["dbg_dsa"]:
                    dkd = S.dsem()
                    S.dma("sp", dkd, C.DBG1[:, :], acc_[:], reads=[tacc], writes=[C.t_dbg])
                    S.dma("sp", dkd, C.DBG2[:, :], C.sgn[:].rearrange("p n h -> p (n h)"), reads=[C.t_sgn], writes=[C.t_dbg], chain=True)
                if j >= 2:
                    S.op("dve", lambda e: e.tensor_reduce(out=s_[:, 0:1], in_=acc_[:, 0:L], axis=AX.X, op=ALU.max), reads=[tacc], writes=[ts])
                    S.op("dve", lambda e: e.tensor_reduce(out=s_[:, 1:2], in_=acc_[:, 0:L], axis=AX.X, op=ALU.min), reads=[tacc], writes=[ts])
                S.op("dve", lambda e: e.tensor_tensor(out=acc_[:, j * 128:L], in0=acc_[:, j * 128:L], in1=C.caus_f[:], op=ALU.add),
                     reads=[tacc, C.t_const], writes=[tacc])
                if j >= 2:
                    st_, tst = stp.next()
                    S.op("dve", lambda e: e.tensor_tensor(out=s_[:, 2:3], in0=s_[:, 0:1], in1=s_[:, 1:2], op=ALU.subtract), reads=[ts], writes=[ts])
                    S.op("dve", lambda e: e.tensor_scalar(out=s_[:, 2:3], in0=s_[:, 2:3], scalar1=1.0001, scalar2=1e-6, op0=ALU.mult, op1=ALU.add),
                         reads=[ts], writes=[ts])
                    S.op("dve", lambda e: e.tensor_tensor(out=st_[:], in0=pw[:], in1=s_[:, 2:3].to_broadcast([128, DSA_ITERS]), op=ALU.mult),
                         reads=[ts, t_pw], writes=[tst])
                    S.op("dve", lambda e: e.tensor_copy(out=s_[:, 3:4], in_=s_[:, 1:2]), reads=[ts], writes=[ts])
                    for it in range(DSA_ITERS):
                        S.op("dve", lambda e: e.tensor_tensor(out=s_[:, 4:5], in0=s_[:, 3:4], in1=st_[:, it:it + 1], op=ALU.add), reads=[ts, tst], writes=[ts])
                        S.op("dve", lambda e: e.tensor_scalar(out=b_[:, 0:L], in0=acc_[:, 0:L], scalar1=s_[:, 4:5], scalar2=None, op0=ALU.is_ge, op1=ALU.add,
                                                              accum_out=s_[:, 5:6]), reads=[ts, tacc], writes=[ts, tb])
                        S.op("dve", lambda e: e.scalar_tensor_tensor(out=s_[:, 6:7], in0=s_[:, 5:6], scalar=256.0, in1=st_[:, it:it + 1], op0=ALU.is_ge, op1=ALU.mult),
                             reads=[ts, tst], writes=[ts])
                        S.op("dve", lambda e: e.tensor_tensor(out=s_[:, 3:4], in0=s_[:, 3:4], in1=s_[:, 6:7], op=ALU.add), reads=[ts], writes=[ts])
                    thr = s_[:, 3:4]
                else:
                    thr = thr0[:]
                S.op("dve", lambda e: e.tensor_scalar(out=b_[:, 0:L], in0=acc_[:, 0:L], scalar1=thr, scalar2=NEG, op0=ALU.is_lt, op1=ALU.mult),
                     reads=[ts, tacc, t_pw], writes=[tb])
                if jj < 3:
                    S.op("pool", lambda e: e.memset(b_[:, L:(4 * g + 4) * 128], NEG), writes=[tb])
                baps.append(b_)
                btks.append(tb)
            attn_group(C, A, g, [0, 1, 2, 3], baps, btks, 768)


def phase_ret(C, l):
    nc, S = C.nc, C.S
    S.barrier()
    with ExitStack() as es:
        RG = sb(nc, es, "R_RG", [128, NT, 256], BF16)
        OF = sb(nc, es, "R_OF", [128, NT, 256], F32)
        t_of, t_rg = Tk(), Tk()
        dk0 = S.dsem()
        S.dma("sp", dk0, RG[:], C.TMO[:, TMO["r_g"]:TMO["r_g"] + 256].rearrange("(n p) c -> p n c", p=128), reads=[C.t_TMO], writes=[t_rg])
        gng, t_gng = bcast_row(C, es, "R_gng", C.ret_gn_g[l:l + 1, :], 256)
        gnb, t_gnb = bcast_row(C, es, "R_gnb", C.ret_gn_b[l:l + 1, :], 256)
        with ExitStack() as es1:
            RQ = sb(nc, es1, "R_RQ", [128, 2, T], BF16)
            RK = sb(nc, es1, "R_RK", [128, 2, T], BF16)
            RQX = sb(nc, es1, "R_RQX", [128, 2, T], BF16)
            V = sb(nc, es1, "R_V", [128, NT, 256], BF16)
            RKZ = sb(nc, es1, "R_RKZ", [128, NT, 256], BF16)
            dmT = sb(nc, es1, "R_dm", [128, 4, 128], F32)
            xiT = sb(nc, es1, "R_xi", [128, 2, 128], BF16)
            gv = sb(nc, es1, "R_gv", [128, 2], F32)
            Rf = sb(nc, es1, "R_Rf", [128, 2, 64], F32)
            Rb = Rot(S, [sb(nc, es1, f"R_Rb{i}", [128, 2, 64], BF16) for i in range(2)])
            t_in, t_c, t_rqx, t_Rf = Tk(), Tk(), Tk(), Tk()
            dk = S.dsem()
            S.dma("sp", dk, RQ[:], C.FMT[CH["r_q"] * 128:(CH["r_q"] + 2) * 128, :].rearrange("(c p) t -> p c t", p=128), reads=[C.t_FMT], writes=[t_in])
            S.dma("sp", dk, RK[:], C.FMT[CH["r_k"] * 128:(CH["r_k"] + 2) * 128, :].rearrange("(c p) t -> p c t", p=128), reads=[C.t_FMT], writes=[t_in], chain=True)
            S.dma("sp", dk, V[:], C.TMO[:, TMO["r_v"]:TMO["r_v"] + 256].rearrange("(n p) c -> p n c", p=128), reads=[C.t_TMO], writes=[t_in], chain=True)
            S.dma("sp", dk, RKZ[:], C.TMO[:, TMO["rkz"]:TMO["rkz"] + 256].rearrange("(n p) c -> p n c", p=128), reads=[C.t_TMO], writes=[t_in], chain=True)
            dk2 = S.dsem()
            S.dma("sp", dk2, dmT[:], C.dmaskT[:, :, :], writes=[t_c])
            S.dma("sp", dk2, gv[:], C.gvec[:, :], writes=[t_c], chain=True)
            dk3 = S.dsem()
            S.dma("pool", dk3, xiT[:], C.xiT[:, :, :], writes=[t_c], chain=True)
            cut = C.cfg.get("cut", 99)
            if cut <= 1:
                return
            for c in range(2):
                for n in range(NT):
                    csl = slice(n * 128, (n + 1) * 128)
                    eng = "dve" if n % 2 == 0 else "pool"
                    S.op(eng, lambda e: e.tensor_tensor(out=RQX[:, c, csl], in0=RQ[:, c, csl], in1=xiT[:, c, :], op=ALU.mult), reads=[t_in, t_c], writes=[])
            t_rqx.w = {"dve": S.cnt["dve"], "pool": S.cnt["pool"]}
            S.op("dve", lambda e: e.memset(Rf[:], 0.0), writes=[t_Rf])
            if cut <= 2:
                return
            inr = Rot(S, [sb(nc, es1, f"R_in{i}", [128, 4, 128], BF16) for i in range(2)])
            rb_prev, trb_prev = None, None
            lsub = C.cfg.get("lsub", 9)
            for n in range(C.cfg.get("nchunks", NT)):
                csl = slice(n * 128, (n + 1) * 128)
                pIa, tpIa = C.psum.next()
                pIb, tpIb = C.psum.next()
                in_, tin = inr.next()
                for (pI, tpI, hs) in ((pIa, tpIa, (0, 2)), (pIb, tpIb, (1, 3))):
                    for i, h in enumerate(hs):
                        c, pb = h // 2, (h % 2) * 64
                        S.op("pe", lambda e: e.matmul(pI[:, i * 128:(i + 1) * 128], lhsT=RK[pb:pb + 64, c, csl], rhs=RQ[pb:pb + 64, c, csl], start=True, stop=True),
                             reads=[t_in], writes=[tpI])
                for (pI, tpI, hs) in ((pIa, tpIa, (0, 2)), (pIb, tpIb, (1, 3))):
                    for i, h in enumerate(hs):
                        S.op("dve", lambda e: e.tensor_tensor(out=in_[:, h, :], in0=pI[:, i * 128:(i + 1) * 128], in1=dmT[:, h, :], op=ALU.mult), reads=[tpI, t_c], writes=[tin])
                if lsub <= 1:
                    continue
                pO, tpO = C.psum.next()
                for h in range(4):
                    c, pb = h // 2, (h % 2) * 64
                    S.op("pe", lambda e: e.matmul(pO[:, h * 64:(h + 1) * 64], lhsT=in_[:, h, :], rhs=V[:, n, h * 64:(h + 1) * 64], start=True, stop=(n == 0)),
                         reads=[tin, t_in], writes=[tpO])
                    if n > 0:
                        S.op("pe", lambda e: e.matmul(pO[:, h * 64:(h + 1) * 64], lhsT=RQX[pb:pb + 64, c, csl], rhs=rb_prev[pb:pb + 64, c, :], start=False, stop=True),
                             reads=[t_rqx, trb_prev], writes=[tpO])
                S.op("act", lambda e: e.copy(out=OF[:, n, :], in_=pO[:, 0:256]), reads=[tpO], writes=[t_of])
                if lsub <= 2:
                    continue
                if n < NT - 1:
                    rb_, trb = Rb.next()
                    for c in range(2):
                        pK, tpK = C.psum.next()
                        S.op("pe", lambda e: e.matmul(pK[:, 0:128], lhsT=RKZ[:, n, c * 128:(c + 1) * 128], rhs=V[:, n, c * 128:(c + 1) * 128], start=True, stop=True),
                             reads=[t_in], writes=[tpK])
                        for hh in range(2):
                            ps_ = slice(hh * 64, (hh + 1) * 64)
                            S.op("dve", lambda e: e.scalar_tensor_tensor(out=Rf[ps_, c, :], in0=Rf[ps_, c, :], scalar=gv[ps_, c:c + 1], in1=pK[ps_, hh * 64:(hh + 1) * 64],
                                                                         op0=ALU.mult, op1=ALU.add), reads=[tpK, t_Rf, t_c], writes=[t_Rf])
                    S.op("act", lambda e: e.copy(out=rb_[:], in_=Rf[:]), reads=[t_Rf], writes=[trb])
                    rb_prev, trb_prev = rb_, trb
            S.barrier()
        if cut <= 3:
            return
        SQ = sb(nc, es, "R_SQ", [128, NT, 256], BF16)
        SG = sb(nc, es, "R_SG", [128, NT, 256], F32)
        mu = sb(nc, es, "R_mu", [128, NT * 4], F32)
        ss = sb(nc, es, "R_ss", [128, NT * 4], F32)
        t_mu, t_sq, t_sg = Tk(), Tk(), Tk()
        S.op("act", lambda e: e.activation(out=SG[:], in_=RG[:], func=AF.Silu), reads=[t_rg], writes=[t_sg])
        for n in range(NT):
            O3 = OF[:, n, :].rearrange("p (h d) -> p h d", d=64)
            S3 = SQ[:, n, :].rearrange("p (h d) -> p h d", d=64)
            m_ = mu[:, n * 4:(n + 1) * 4]
            s_ = ss[:, n * 4:(n + 1) * 4]
            S.op("dve", lambda e: e.tensor_reduce(out=m_, in_=O3, axis=AX.X, op=ALU.add), reads=[t_of], writes=[t_mu])
            S.op("dve", lambda e: e.tensor_scalar(out=m_, in0=m_, scalar1=-1.0 / 64.0, scalar2=None, op0=ALU.mult), reads=[t_mu], writes=[t_mu])
            S.op("dve", lambda e: e.tensor_tensor(out=O3, in0=O3, in1=m_.unsqueeze(2).to_broadcast([128, 4, 64]), op=ALU.add), reads=[t_of, t_mu], writes=[t_of])
            S.op("act", lambda e: e.activation(out=SQ[:, n, :], in_=OF[:, n, :], func=AF.Square), reads=[t_of], writes=[t_sq])
            S.op("dve", lambda e: e.tensor_reduce(out=s_, in_=S3, axis=AX.X, op=ALU.add), reads=[t_sq], writes=[t_mu])
            S.op("dve", lambda e: e.tensor_scalar(out=s_, in0=s_, scalar1=1.0 / 64.0, scalar2=LN_EPS, op0=ALU.mult, op1=ALU.add), reads=[t_mu], writes=[t_mu])
        S.op("act", lambda e: e.activation(out=ss[:], in_=ss[:], func=AF.Sqrt), reads=[t_mu], writes=[t_mu])
        S.op("dve", lambda e: e.reciprocal(out=ss[:], in_=ss[:]), reads=[t_mu], writes=[t_mu])
        for n in range(NT):
            O3 = OF[:, n, :].rearrange("p (h d) -> p h d", d=64)
            s_ = ss[:, n * 4:(n + 1) * 4]
            S.op("dve", lambda e: e.tensor_tensor(out=O3, in0=O3, in1=s_.unsqueeze(2).to_broadcast([128, 4, 64]), op=ALU.mult), reads=[t_of, t_mu], writes=[t_of])
            S.op("dve", lambda e: e.tensor_tensor(out=OF[:, n, :], in0=OF[:, n, :], in1=gng[:], op=ALU.mult), reads=[t_of, t_gng], writes=[t_of])
            S.op("dve", lambda e: e.tensor_tensor(out=OF[:, n, :], in0=OF[:, n, :], in1=gnb[:], op=ALU.add), reads=[t_of, t_gnb], writes=[t_of])
            S.op("dve", lambda e: e.tensor_tensor(out=SQ[:, n, :], in0=OF[:, n, :], in1=SG[:, n, :], op=ALU.mult), reads=[t_of, t_sg, t_sq], writes=[t_sq])
        if cut <= 4:
            return
        stg = Rot(S, [sb(nc, es, f"R_stg{i}", [128, 2, 512], BF16) for i in range(2)])
        for g in range(NG):
            st_, tst = stg.next()
            for c in range(2):
                pT, tpT = C.psum.next()
                pTb = pT[:].bitcast(BF16)
                for jj in range(4):
                    n = 4 * g + jj
                    S.op("pe", lambda e: e.transpose(pTb[:, jj * 128:(jj + 1) * 128], SQ[:, n, c * 128:(c + 1) * 128], C.ident_b[:]), reads=[t_sq, C.t_const], writes=[tpT])
                S.op("act", lambda e: e.copy(out=st_[:, c, :], in_=pTb[:, 0:512]), reads=[tpT], writes=[tst])
            S.dma("sp", stg.dkey(), C.MIXT[256:512, g * 512:(g + 1) * 512].rearrange("(c p) t -> p c t", p=128), st_[:], reads=[tst], writes=[C.t_MIXT], chain=True)


def phase_rglru(C, l):
    nc, S = C.nc, C.S
    S.barrier()
    with ExitStack() as es:
        pc = sb(nc, es, "G_pc", [128, 16], F32)
        wbd = sb(nc, es, "G_wbd", [128, 4, 128], BF16)
        t_pc, t_w = Tk(), Tk()
        dk = S.dsem()
        S.dma("sp", dk, pc[:], C.rg_pc[l], writes=[t_pc])
        dkw = S.dsem()
        S.dma("pool", dkw, wbd[:], C.rg_wbd[l], writes=[t_w])
        sc = sb(nc, es, "G_sc", [128, 4], F32)
        S.op("act", lambda e: e.activation(out=sc[:, 0:2], in_=pc[:, 14:16], func=AF.Exp, scale=-1.0), reads=[t_pc], writes=[t_pc])
        S.op("dve", lambda e: e.tensor_scalar(out=sc[:, 0:2], in0=sc[:, 0:2], scalar1=1.0, scalar2=None, op0=ALU.add), reads=[t_pc], writes=[t_pc])
        S.op("act", lambda e: e.activation(out=sc[:, 0:2], in_=sc[:, 0:2], func=AF.Ln), reads=[t_pc], writes=[t_pc])
        S.op("dve", lambda e: e.tensor_scalar(out=sc[:, 2:4], in0=sc[:, 0:2], scalar1=-16.0, scalar2=None, op0=ALU.mult), reads=[t_pc], writes=[t_pc])
        S.op("dve", lambda e: e.tensor_scalar(out=sc[:, 0:2], in0=sc[:, 0:2], scalar1=-8.0, scalar2=None, op0=ALU.mult), reads=[t_pc], writes=[t_pc])
        CX = sb(nc, es, "G_CX", [128, T], BF16)
        CG = sb(nc, es, "G_CG", [128, T], BF16)
        xc = sb(nc, es, "G_xc", [128, T], F32)
        xcb = sb(nc, es, "G_xcb", [128, T], BF16)
        gx = sb(nc, es, "G_gx", [128, T], F32)
        ga = sb(nc, es, "G_ga", [128, T], F32)
        a2 = sb(nc, es, "G_a2", [128, T], F32)
        gl = sb(nc, es, "G_gl", [128, T], F32)
        hh = sb(nc, es, "G_h", [128, T], F32)
        ob = sb(nc, es, "G_ob", [128, T], BF16)
        t_cx, t_cg, t_xc, t_xcb, t_gx, t_ga, t_a2, t_gl, t_h, t_ob = (Tk() for _ in range(10))
        dkx, dkg, dko = S.dsem(), S.dsem(), S.dsem()
        for c in range(2):
            S.dma("sp", dkx, CX[:], C.FMT[(CH["c_x"] + c) * 128:(CH["c_x"] + c + 1) * 128, :], reads=[C.t_FMT], writes=[t_cx])
            S.dma("sp", dkg, CG[:], C.FMT[(CH["c_g"] + c) * 128:(CH["c_g"] + c + 1) * 128, :], reads=[C.t_FMT], writes=[t_cg])
            cw = lambda i: pc[:, c * 4 + i:c * 4 + i + 1]
            S.op("dve", lambda e: e.tensor_scalar(out=xc[:], in0=CX[:], scalar1=cw(3), scalar2=pc[:, 8 + c:9 + c], op0=ALU.mult, op1=ALU.add),
                 reads=[t_cx, t_pc], writes=[t_xc])
            for sh in (1, 2, 3):
                S.op("dve", lambda e: e.scalar_tensor_tensor(out=xc[:, sh:T], in0=CX[:, 0:T - sh], scalar=cw(3 - sh), in1=xc[:, sh:T], op0=ALU.mult, op1=ALU.add),
                     reads=[t_cx, t_pc, t_xc], writes=[t_xc])
            S.op("act", lambda e: e.copy(out=xcb[:], in_=xc[:]), reads=[t_xc], writes=[t_xcb])
            for g in range(NG):
                tsl = slice(g * 512, (g + 1) * 512)
                pX, tpX = C.psum.next()
                S.op("pe", lambda e: e.matmul(pX[:], lhsT=wbd[:, c, :], rhs=xcb[:, tsl], start=True, stop=True), reads=[t_w, t_xcb], writes=[tpX])
                S.op("act", lambda e: e.activation(out=gx[:, tsl], in_=pX[:], func=AF.Sigmoid, bias=pc[:, 10 + c:11 + c]), reads=[tpX, t_pc], writes=[t_gx])
                pA, tpA = C.psum.next()
                S.op("pe", lambda e: e.matmul(pA[:], lhsT=wbd[:, 2 + c, :], rhs=xcb[:, tsl], start=True, stop=True), reads=[t_w, t_xcb], writes=[tpA])
                S.op("act", lambda e: e.activation(out=ga[:, tsl], in_=pA[:], func=AF.Sigmoid, bias=pc[:, 12 + c:13 + c]), reads=[tpA, t_pc], writes=[t_ga])
            S.op("act", lambda e: e.copy(out=gl[:], in_=CG[:]), reads=[t_cg], writes=[t_gl])
            S.op("pool", lambda e: e.tensor_tensor(out=hh[:], in0=gl[:], in1=gl[:], op=ALU.mult), reads=[t_gl], writes=[t_h])
            S.op("dve", lambda e: e.tensor_scalar(out=hh[:], in0=hh[:], scalar1=0.044715, scalar2=1.0, op0=ALU.mult, op1=ALU.add), reads=[t_h], writes=[t_h])
            S.op("dve", lambda e: e.tensor_tensor(out=hh[:], in0=hh[:], in1=gl[:], op=ALU.mult), reads=[t_h, t_gl], writes=[t_h])
            S.op("act", lambda e: e.activation(out=hh[:], in_=hh[:], func=AF.Sigmoid, scale=1.5957691216), reads=[t_h], writes=[t_h])
            S.op("dve", lambda e: e.tensor_tensor(out=gl[:], in0=gl[:], in1=hh[:], op=ALU.mult), reads=[t_h, t_gl], writes=[t_gl])
            S.op("act", lambda e: e.activation(out=a2[:], in_=ga[:], func=AF.Exp, scale=sc[:, 2 + c:3 + c]), reads=[t_ga, t_pc], writes=[t_a2])
            S.op("act", lambda e: e.activation(out=ga[:], in_=ga[:], func=AF.Exp, scale=sc[:, c:c + 1]), reads=[t_ga, t_pc], writes=[t_ga])
            S.op("dve", lambda e: e.tensor_scalar(out=a2[:], in0=a2[:], scalar1=-1.0, scalar2=1.0, op0=ALU.mult, op1=ALU.add), reads=[t_a2], writes=[t_a2])
            S.op("act", lambda e: e.activation(out=a2[:], in_=a2[:], func=AF.Sqrt), reads=[t_a2], writes=[t_a2])
            S.op("dve", lambda e: e.tensor_tensor(out=gx[:], in0=gx[:], in1=xc[:], op=ALU.mult), reads=[t_gx, t_xc], writes=[t_gx])
            S.op("dve", lambda e: e.tensor_tensor(out=gx[:], in0=gx[:], in1=a2[:], op=ALU.mult), reads=[t_gx, t_a2], writes=[t_gx])
            S.op("dve", lambda e: e.tensor_tensor_scan(out=hh[:], data0=ga[:], data1=gx[:], initial=0.0, op0=ALU.mult, op1=ALU.add), reads=[t_ga, t_gx, t_h], writes=[t_h])
            S.op("dve", lambda e: e.tensor_tensor(out=ob[:], in0=hh[:], in1=gl[:], op=ALU.mult), reads=[t_h, t_gl], writes=[t_ob])
            S.dma("sp", dko, C.MIXT[512 + c * 128:512 + (c + 1) * 128, :], ob[:], reads=[t_ob], writes=[C.t_MIXT], chain=True)


def ln_tile(C, L, y, ty, gbc, t_g, bbc, t_b):
    S = C.S
    st, tst = L.st.next()
    jk, tjk = L.jk.next()
    S.op("dve", lambda e: e.tensor_reduce(out=st[:, 0:1], in_=y[:], axis=AX.X, op=ALU.add), reads=[ty], writes=[tst])
    S.op("act", lambda e: e.activation(out=jk[:], in_=y[:], func=AF.Square), reads=[ty], writes=[tjk])
    S.op("dve", lambda e: e.tensor_reduce(out=st[:, 1:2], in_=jk[:], axis=AX.X, op=ALU.add), reads=[tjk, tst], writes=[tst])
    S.op("dve", lambda e: e.tensor_scalar(out=st[:, 0:2], in0=st[:, 0:2], scalar1=1.0 / D, scalar2=None, op0=ALU.mult), reads=[tst], writes=[tst])
    S.op("dve", lambda e: e.tensor_tensor(out=st[:, 2:3], in0=st[:, 0:1], in1=st[:, 0:1], op=ALU.mult), reads=[tst], writes=[tst])
    S.op("dve", lambda e: e.tensor_tensor(out=st[:, 1:2], in0=st[:, 1:2], in1=st[:, 2:3], op=ALU.subtract), reads=[tst], writes=[tst])
    S.op("dve", lambda e: e.tensor_scalar(out=st[:, 1:2], in0=st[:, 1:2], scalar1=LN_EPS, scalar2=None, op0=ALU.add), reads=[tst], writes=[tst])
    S.op("act", lambda e: e.activation(out=st[:, 1:2], in_=st[:, 1:2], func=AF.Sqrt), reads=[tst], writes=[tst])
    S.op("dve", lambda e: e.reciprocal(out=st[:, 1:2], in_=st[:, 1:2]), reads=[tst], writes=[tst])
    S.op("dve", lambda e: e.tensor_scalar(out=y[:], in0=y[:], scalar1=st[:, 0:1], scalar2=st[:, 1:2], op0=ALU.subtract, op1=ALU.mult), reads=[tst, ty], writes=[ty])
    S.op("dve", lambda e: e.tensor_tensor(out=y[:], in0=y[:], in1=gbc[:], op=ALU.mult), reads=[ty, t_g], writes=[ty])
    S.op("dve", lambda e: e.tensor_tensor(out=y[:], in0=y[:], in1=bbc[:], op=ALU.add), reads=[ty, t_b], writes=[ty])


def ln_alloc(C, es, pref):
    nc, S = C.nc, C.S
    L = Ctx()
    L.st = Rot(S, [sb(nc, es, pref + f"st{i}", [128, 4], F32) for i in range(2)])
    L.jk = Rot(S, [sb(nc, es, pref + f"jk{i}", [128, D], BF16) for i in range(2)])
    return L


def phase_C(C, l, x_src):
    nc, S = C.nc, C.S
    S.barrier()
    with ExitStack() as es:
        Wo = sb(nc, es, "C_Wo", [128, 8, D], BF16)
        t_wo = Tk()
        dkw = S.dsem()
        for hf in range(2):
            S.dma("pool", dkw, Wo[:, hf * 4:(hf + 1) * 4, :], C.w_out[l][hf * 512:(hf + 1) * 512, :].rearrange("(k p) d -> p k d", p=128), writes=[t_wo], chain=True)
        RW = sb(nc, es, "C_RW", [128, 8, 32], F32)
        RWh = sb(nc, es, "C_RWh", [128, 8, 32], BF16)
        RWl = sb(nc, es, "C_RWl", [128, 8, 32], BF16)
        t_rw = Tk()
        dkr = S.dsem()
        S.dma("sp", dkr, RW[:], C.router_w[l].rearrange("(k p) e -> p k e", p=128), writes=[t_rw])
        S.op("act", lambda e: e.copy(out=RWh[:], in_=RW[:]), reads=[t_rw], writes=[t_rw])
        S.op("dve", lambda e: e.tensor_tensor(out=RWl[:], in0=RW[:], in1=RWh[:], op=ALU.subtract), reads=[t_rw], writes=[t_rw])
        g1, t_g1 = bcast_row(C, es, "C_g1", C.ln1_g[l:l + 1, :], D)
        b1, t_b1 = bcast_row(C, es, "C_b1", C.ln1_b[l:l + 1, :], D)
        rb, t_rb = bcast_row(C, es, "C_rb", C.router_b[l:l + 1, :], 32)
        L = ln_alloc(C, es, "C_")
        MT = Rot(S, [sb(nc, es, f"C_MT{i}", [128, 8, 512], BF16) for i in range(2)])
        xin = Rot(S, [sb(nc, es, f"C_x{i}", [128, D], F32) for i in range(2)])
        yr = Rot(S, [sb(nc, es, f"C_y{i}", [128, D], F32) for i in range(2)])
        xtb = Rot(S, [sb(nc, es, f"C_xtb{i}", [128, 8, 128], BF16) for i in range(2)])
        xtf = Rot(S, [sb(nc, es, f"C_xtf{i}", [128, 8, 128], BF16) for i in range(2)])
        sm = Rot(S, [sb(nc, es, f"C_sm{i}", [128, 128], F32) for i in range(2)])
        for g in range(NG):
            mt, tmt = MT.next()
            S.dma("sp", MT.dkey(), mt[:], C.MIXT[:, g * 512:(g + 1) * 512].rearrange("(k p) t -> p k t", p=128), reads=[C.t_MIXT], writes=[tmt])
            for jj in range(4):
                n = 4 * g + jj
                x_, tx = xin.next()
                S.dma("sp", xin.dkey(), x_[:], x_src[n * 128:(n + 1) * 128, :], reads=[C.t_xsrc], writes=[tx])
                y_, ty = yr.next()
                for hf in range(2):
                    ps, tp = C.psum.next()
                    for k in range(8):
                        S.op("pe", lambda e: e.matmul(ps[:], lhsT=mt[:, k, jj * 128:(jj + 1) * 128], rhs=Wo[:, k, hf * 512:(hf + 1) * 512], start=(k == 0), stop=(k == 7)),
                             reads=[tmt, t_wo], writes=[tp])
                    S.op("dve", lambda e: e.scalar_tensor_tensor(out=y_[:, hf * 512:(hf + 1) * 512], in0=x_[:, hf * 512:(hf + 1) * 512], scalar=ALPHA, in1=ps[:],
                                                                 op0=ALU.mult, op1=ALU.add), reads=[tx, tp], writes=[ty])
                ln_tile(C, L, y_, ty, g1, t_g1, b1, t_b1)
                S.dma("sp", yr.dkey(), C.X1[n * 128:(n + 1) * 128, :], y_[:], reads=[ty], writes=[C.t_X1], chain=True)
                tb_, ttb = xtb.next()
                tf_, ttf = xtf.next()
                for hf in range(2):
                    ps, tp = C.psum.next()
                    for j in range(4):
                        k = hf * 4 + j
                        S.op("pe", lambda e: e.transpose(ps[:, j * 128:(j + 1) * 128], y_[:, k * 128:(k + 1) * 128], C.ident_f[:]), reads=[ty, C.t_const], writes=[tp])
                    src = ps[:].rearrange("p (k c) -> p k c", k=4)
                    S.op("act", lambda e: e.copy(out=tb_[:, hf * 4:(hf + 1) * 4, :], in_=src), reads=[tp], writes=[ttb])
                    S.op("dve", lambda e: e.tensor_tensor(out=tf_[:, hf * 4:(hf + 1) * 4, :], in0=src, in1=tb_[:, hf * 4:(hf + 1) * 4, :], op=ALU.subtract), reads=[tp, ttb], writes=[ttf])
                S.dma("sp", xtb.dkey(), C.X1T[:, n * 128:(n + 1) * 128].rearrange("(k p) t -> p k t", p=128), tb_[:], reads=[ttb], writes=[C.t_X1T], chain=True)
                pr, tpr = C.psum.next()
                i3 = 0
                for (xa, wa) in ((tb_, RWh), (tf_, RWh), (tb_, RWl)):
                    for k in range(8):
                        S.op("pe", lambda e: e.matmul(pr[:, 0:32], lhsT=xa[:, k, :], rhs=wa[:, k, :], start=(i3 == 0), stop=(i3 == 23)), reads=[ttf, ttb, t_rw], writes=[tpr])
                        i3 += 1
                s_, ts = sm.next()
                lg, ex, m8 = s_[:, 0:32], s_[:, 32:64], s_[:, 64:72]
                S.op("dve", lambda e: e.tensor_tensor(out=lg, in0=pr[:, 0:32], in1=rb[:], op=ALU.add), reads=[tpr, t_rb], writes=[ts])
                S.op("dve", lambda e: e.max(out=m8, in_=lg), reads=[ts], writes=[ts])
                S.op("dve", lambda e: e.tensor_scalar(out=s_[:, 72:73], in0=s_[:, 64:65], scalar1=-1.0, scalar2=None, op0=ALU.mult), reads=[ts], writes=[ts])
                S.op("act", lambda e: e.activation(out=ex, in_=lg, func=AF.Exp, bias=s_[:, 72:73]), reads=[ts], writes=[ts])
                S.op("dve", lambda e: e.scalar_tensor_tensor(out=ex, in0=lg, scalar=s_[:, 67:68], in1=ex, op0=ALU.is_ge, op1=ALU.mult), reads=[ts], writes=[ts])
                S.op("dve", lambda e: e.tensor_reduce(out=s_[:, 73:74], in_=ex, axis=AX.X, op=ALU.add), reads=[ts], writes=[ts])
                S.op("dve", lambda e: e.reciprocal(out=s_[:, 73:74], in_=s_[:, 73:74]), reads=[ts], writes=[ts])
                S.op("dve", lambda e: e.tensor_scalar(out=C.COMB[:, n, :], in0=ex, scalar1=s_[:, 73:74], scalar2=None, op0=ALU.mult), reads=[ts], writes=[C.t_comb])
        if "COMBD" in C.cfg.get("debug_out", ()):
            dkc = S.dsem()
            S.dma("sp", dkc, C.COMBD.rearrange("(n p) e -> p n e", p=128), C.COMB[:], reads=[C.t_comb], writes=[Tk()])


SIGMAX = float(1.0 / (1.0 + np.exp(-1.702 * 7.0)))
QT_ = 1024


def phase_D(C, l, dst):
    nc, S = C.nc, C.S
    S.barrier()
    NE = C.cfg.get("n_experts", N_EXPERTS)
    with ExitStack() as es:
        g2, t_g2 = bcast_row(C, es, "D_g2", C.ln2_g[l:l + 1, :], D)
        b2l, t_b2l = bcast_row(C, es, "D_b2l", C.ln2_b[l:l + 1, :], D)
        L = ln_alloc(C, es, "D_")
        B2 = sb(nc, es, "D_B2", [32, D], BF16)
        t_B2 = Tk()
        dkb = S.dsem()
        S.dma("pool", dkb, B2[:], C.exp_b2[l], writes=[t_B2])
        W1 = Rot(S, [sb(nc, es, f"D_W1_{i}", [128, 8, 2048], BF16) for i in range(2)])
        W2 = [sb(nc, es, f"D_W2_{i}", [128, 8, 1024], BF16) for i in range(2)]
        bt = Rot(S, [sb(nc, es, f"D_bt{i}", [128, 16], F32) for i in range(2)])
        bs = Rot(S, [sb(nc, es, f"D_bs{i}", [128, 16], F32) for i in range(2)])
        XT = sb(nc, es, "D_XT", [128, 8, QT_], BF16)
        acc = sb(nc, es, "D_acc", [128, QT_ // 128, D], F32)
        gT = Rot(S, [sb(nc, es, f"D_gT{i}", [128, 8, 512], BF16) for i in range(2)])
        sg = Rot(S, [sb(nc, es, f"D_sg{i}", [128, 512], F32) for i in range(2)])
        gl = Rot(S, [sb(nc, es, f"D_gl{i}", [128, 512], F32) for i in range(2)])
        ub = Rot(S, [sb(nc, es, f"D_ub{i}", [128, 512], F32) for i in range(2)])
        yb = Rot(S, [sb(nc, es, f"D_yb{i}", [128, 512], F32) for i in range(2)])
        cT = Rot(S, [sb(nc, es, f"D_cT{i}", [32, 128], BF16) for i in range(2)])
        xres = Rot(S, [sb(nc, es, f"D_xr{i}", [128, D], F32) for i in range(1)])
        t_XT, t_acc = Tk(), [Tk() for _ in range(QT_ // 128)]
        dkx = S.dsem()
        for q in range(T // QT_):
            q0 = q * QT_
            S.dma("sp", dkx, XT[:], C.X1T[:, q0:q0 + QT_].rearrange("(k p) t -> p k t", p=128), reads=[C.t_X1T], writes=[t_XT])
            for i in range(QT_ // 128):
                n = q * (QT_ // 128) + i
                pc_, tpc = C.psum.next()
                S.op("pe", lambda e: e.transpose(pc_[0:32, 0:128], C.COMB[:, n, :], C.ident_f[:]), reads=[C.t_comb, C.t_const], writes=[tpc])
                c_, tc = cT.next()
                S.op("act", lambda e: e.copy(out=c_[:], in_=pc_[0:32, 0:128]), reads=[tpc], writes=[tc])
                for hf in range(2):
                    pb_, tpb = C.psum.next()
                    S.op("pe", lambda e: e.matmul(pb_[:], lhsT=c_[:], rhs=B2[:, hf * 512:(hf + 1) * 512], start=True, stop=True), reads=[tc, t_B2], writes=[tpb])
                    S.op("act", lambda e: e.copy(out=acc[:, i, hf * 512:(hf + 1) * 512], in_=pb_[:]), reads=[tpb], writes=[t_acc[i]])
            for ex in range(NE):
                w1, tw1 = W1.next()
                w2 = W2[W1.i]
                dkw = W1.dkey()
                for kh in range(2):
                    S.dma("pool", dkw, w1[:, kh * 4:(kh + 1) * 4, :], C.exp_w1[l, ex, kh * 512:(kh + 1) * 512, :].rearrange("(k p) f -> p k f", p=128),
                          writes=[tw1], chain=(kh > 0))
                for kh in range(2):
                    S.dma("pool", dkw, w2[:, kh * 4:(kh + 1) * 4, :], C.exp_w2[l, ex, kh * 512:(kh + 1) * 512, :].rearrange("(k p) d -> p k d", p=128),
                          writes=[tw1], chain=True)
                bt_, tbt = bt.next()
                S.dma("sp", bt.dkey(), bt_[:], C.exp_b1p[l, ex], writes=[tbt])
                bs_, tbs = bs.next()
                S.op("dve", lambda e: e.tensor_scalar(out=bs_[:, 0:8], in0=bt_[:, 0:8], scalar1=1.702, scalar2=None, op0=ALU.mult), reads=[tbt], writes=[tbs])
                S.op("dve", lambda e: e.tensor_scalar(out=bs_[:, 8:16], in0=bt_[:, 8:16], scalar1=1.0, scalar2=None, op0=ALU.add), reads=[tbt, tbs], writes=[tbs])
                dsub = C.cfg.get("dsub", 9)
                for g in range(QT_ // 512 if dsub >= 2 else 0):
                    tsl = slice(g * 512, (g + 1) * 512)
                    g_, tg = gT.next()
                    for j in range(8):
                        pG, tpG = C.psum.next()
                        for k in range(8):
                            S.op("pe", lambda e: e.matmul(pG[:], lhsT=w1[:, k, j * 128:(j + 1) * 128], rhs=XT[:, k, tsl], start=(k == 0), stop=(k == 7)),
                                 reads=[tw1, t_XT], writes=[tpG])
                        pU, tpU = C.psum.next()
                        for k in range(8):
                            S.op("pe", lambda e: e.matmul(pU[:], lhsT=w1[:, k, 1024 + j * 128:1024 + (j + 1) * 128], rhs=XT[:, k, tsl], start=(k == 0), stop=(k == 7)),
                                 reads=[tw1, t_XT], writes=[tpU])
                        z_, tz = gl.next()
                        u_, tu = ub.next()
                        s_, ts = sg.next()
                        S.op("act", lambda e: e.activation(out=z_[:], in_=pG[:], func=AF.Identity, bias=bt_[:, j:j + 1]), reads=[tpG, tbt], writes=[tz])
                        S.op("act", lambda e: e.activation(out=u_[:], in_=pU[:], func=AF.Identity, bias=bs_[:, 8 + j:9 + j]), reads=[tpU, tbs], writes=[tu])
                        S.op("dve", lambda e: e.tensor_scalar(out=z_[:], in0=z_[:], scalar1=7.0, scalar2=None, op0=ALU.min), reads=[tz], writes=[tz])
                        S.op("act", lambda e: e.activation(out=s_[:], in_=z_[:], func=AF.Sigmoid, scale=1.702), reads=[tz], writes=[ts])
                        S.op("dve", lambda e: e.tensor_scalar(out=u_[:], in0=u_[:], scalar1=8.0, scalar2=-6.0, op0=ALU.min, op1=ALU.max), reads=[tu], writes=[tu])
                        S.op("dve", lambda e: e.tensor_tensor(out=z_[:], in0=z_[:], in1=s_[:], op=ALU.mult), reads=[tz, ts], writes=[tz])
                        S.op("dve", lambda e: e.tensor_tensor(out=g_[:, j, :], in0=u_[:], in1=z_[:], op=ALU.mult), reads=[tu, tz], writes=[tg])
                    for tt in range(4 if dsub >= 3 else 0):
                        i = g * 4 + tt
                        n = q * (QT_ // 128) + i
                        for hf in range(2):
                            pY, tpY = C.psum.next()
                            for j in range(8):
                                S.op("pe", lambda e: e.matmul(pY[:], lhsT=g_[:, j, tt * 128:(tt + 1) * 128], rhs=w2[:, j, hf * 512:(hf + 1) * 512], start=(j == 0), stop=(j == 7)),
                                     reads=[tg, tw1], writes=[tpY])
                            y_, tyb = yb.next()
                            S.op("act", lambda e: e.activation(out=y_[:], in_=pY[:], func=AF.Copy, scale=C.COMB[:, n, ex:ex + 1]), reads=[tpY, C.t_comb], writes=[tyb])
                            S.op("pool", lambda e: e.tensor_tensor(out=acc[:, i, hf * 512:(hf + 1) * 512], in0=acc[:, i, hf * 512:(hf + 1) * 512], in1=y_[:], op=ALU.add),
                                 reads=[tyb, t_acc[i]], writes=[t_acc[i]])
            for i in range(QT_ // 128):
                n = q * (QT_ // 128) + i
                x_, tx = xres.next()
                S.dma("sp", xres.dkey(), x_[:], C.X1[n * 128:(n + 1) * 128, :], reads=[C.t_X1], writes=[tx])
                y = acc[:, i, :]
                S.op("dve", lambda e: e.scalar_tensor_tensor(out=y, in0=x_[:], scalar=ALPHA, in1=y, op0=ALU.mult, op1=ALU.add), reads=[tx, t_acc[i]], writes=[t_acc[i]])
                ln_tile(C, L, y, t_acc[i], g2, t_g2, b2l, t_b2l)
                S.dma("sp", dkx, dst[n * 128:(n + 1) * 128, :], y, reads=[t_acc[i]], writes=[C.t_X2], chain=True)


SMALL_INPUTS = ("ret_gn_g", "ret_gn_b", "w_out", "router_w", "router_b", "ln1_g", "ln1_b", "exp_w1", "exp_w2", "exp_b2", "ln2_g", "ln2_b")


WEIGHT_KEYS = ("w_in", "conv_w", "conv_b", "rg_bx", "rg_ba", "rg_lambda", "rg_wx", "rg_wa", "exp_b1")
_CACHE = {}


def _shared_inputs(inp):
    shared = dict(host_consts())
    shared.update(prep_weights({k: inp[k] for k in WEIGHT_KEYS}))
    for k in SMALL_INPUTS:
        shared[k] = np.ascontiguousarray(inp[k], dtype=np.float32)
    return shared


def _run(inputs, n_cores=8, trace=False, first=0):
    inp = {k: np.asarray(v) for k, v in inputs.items()}
    if "nc" not in _CACHE:
        _CACHE["nc"] = build(dict(phases="AMSRGCD", layers=DEPTH))
    nc = _CACHE["nc"]
    shared = _shared_inputs(inp)
    x = np.ascontiguousarray(inp["x"], dtype=np.float32)
    in_maps = []
    for c in range(n_cores):
        m = dict(shared)
        m["x"] = np.ascontiguousarray(x[first + c])
        in_maps.append(m)
    res = run_bass_kernel_spmd(nc, in_maps, core_ids=list(range(n_cores)), trace=trace)
    out = np.stack([np.asarray(r["out"], dtype=np.float32) for r in res.results], axis=0)
    return out, res


LAYER_KEYS = ("w_fmr", "w_fms", "w_fmp", "w_dw", "w_tm", "rg_pc", "rg_wbd", "exp_b1p") + SMALL_INPUTS


def _run_layers(inputs, n_cores=8, trace=False):
    inp = {k: np.asarray(v) for k, v in inputs.items()}
    if "nc1" not in _CACHE:
        _CACHE["nc1"] = build(dict(phases="AMSRGCD", layers=1, decl_depth=1))
    nc = _CACHE["nc1"]
    shared = _shared_inputs(inp)
    x = np.ascontiguousarray(inp["x"], dtype=np.float32)
    results = []
    for l in range(DEPTH):
        sh = dict(shared)
        for k in LAYER_KEYS:
            sh[k] = np.ascontiguousarray(shared[k][l:l + 1])
        in_maps = []
        for c in range(n_cores):
            m = dict(sh)
            m["x"] = np.ascontiguousarray(x[c])
            in_maps.append(m)
        res = run_bass_kernel_spmd(nc, in_maps, core_ids=list(range(n_cores)), trace=trace)
        results.append(res)
        x = np.stack([np.asarray(r["out"], dtype=np.float32) for r in res.results], axis=0)
    return x, results


def kernel(**inputs):
    out, _ = _run(inputs, 8)
    return out
```

```python
import numpy as np
from contextlib import ExitStack
import concourse.bass as bass
import concourse.mybir as mybir
from concourse.bass_utils import run_bass_kernel_spmd

F32 = mybir.dt.float32
BF16 = mybir.dt.bfloat16
AF = mybir.ActivationFunctionType
ALU = mybir.AluOpType
AX = mybir.AxisListType

D = 1024
T = 4096
DEPTH = 2
NT = T // 128
NG = T // 512
NEG = -30000.0
ALPHA = (2 * DEPTH) ** 0.25
LN_EPS = 1e-5
N_EXPERTS = 32

_off = {}
_o = 0
for _n, _s in (("a_q", 256), ("a_k", 256), ("a_v", 256), ("r_q", 256), ("r_k", 256), ("r_v", 256),
               ("r_g", 256), ("c_x", 256), ("c_g", 256), ("d_q", 256), ("d_k", 256), ("d_v", 256),
               ("d_qi", 512), ("d_ki", 64), ("d_w", 8)):
    _off[_n] = (_o, _s)
    _o += _s
IN_WIDTH = _o


def _cols(name):
    o, s = _off[name]
    return np.arange(o, o + s)


def _swap(c):
    c = c.reshape(-1, 64)
    return np.concatenate([c[:, 32:], c[:, :32]], axis=1).reshape(-1)


ROPE_NAMES = ("a_q", "a_k", "r_q", "r_k", "d_q", "d_k", "d_qi", "d_ki")
FM_ROPE_COLS = np.concatenate([_cols(n) for n in ROPE_NAMES])
FM_ROPE_SW = np.concatenate([_swap(_cols(n)) for n in ROPE_NAMES])
FM_PLAIN_COLS = np.concatenate([_cols("c_x"), _cols("c_g")])
DW_COLS = _cols("d_w")
TM_COLS = np.concatenate([_cols("a_v"), _cols("r_v"), _cols("d_v"), _cols("r_g"), _cols("r_k"), _cols("d_w")])
NTM = TM_COLS.shape[0]
NFMC = 21
CH = dict(a_q=0, a_k=2, r_q=4, r_k=6, d_q=8, d_k=10, d_qi=12, d_ki=16, c_x=17, c_g=19)
TMO = dict(a_v=0, r_v=256, d_v=512, r_g=768, rkz=1024)
NTMO = 1280


class Tk:
    __slots__ = ("w", "r", "name", "excl")

    def __init__(self, name="", excl=False):
        self.w = {}
        self.r = {}
        self.name = name
        self.excl = excl


class Sched:
    ENG = ("pe", "act", "dve", "pool", "sp")

    def __init__(self, nc, es):
        self.nc = nc
        self.es = es
        self.eng = {"pe": nc.tensor, "act": nc.scalar, "dve": nc.vector,
                    "pool": nc.gpsimd, "sp": nc.sync}
        self.sem = {}
        self.cnt = {}
        for k in self.ENG:
            self.sem[k] = es.enter_context(nc.semaphore("s_" + k))
            self.cnt[k] = 0
        self.seen = {k: {} for k in self.ENG}
        self.ndsem = 0
        self.nwaits = 0
        self.free_dsems = []

    def dsem(self):
        if self.free_dsems:
            return self.free_dsems.pop()
        self.ndsem += 1
        key = "d%d" % self.ndsem
        self.sem[key] = self.es.enter_context(self.nc.semaphore(key))
        self.cnt[key] = 0
        return key

    def release_dsems(self, keys):
        self.free_dsems.extend(keys)

    def _wait(self, e, deps):
        seen = self.seen[e]
        for key, val in deps.items():
            if key == "pe" and e == "pe":
                continue
            if seen.get(key, 0) >= val:
                continue
            self.eng[e].wait_ge(self.sem[key], val)
            self.nwaits += 1
            seen[key] = val

    @staticmethod
    def _merge(d, s):
        for k, v in s.items():
            if d.get(k, 0) < v:
                d[k] = v

    def _deps(self, reads, writes):
        deps = {}
        for t in reads:
            self._merge(deps, t.w)
            if t.excl:
                self._merge(deps, t.r)
        for t in writes:
            self._merge(deps, t.w)
            self._merge(deps, t.r)
        return deps

    def op(self, e, fn, reads=(), writes=()):
        self._wait(e, self._deps(reads, writes))
        ins = fn(self.eng[e])
        self.cnt[e] += 1
        ins.then_inc(self.sem[e], 1)
        me = {e: self.cnt[e]}
        for t in reads:
            self._merge(t.r, me)
        for t in writes:
            t.w = dict(me)
            t.r = {}
        return ins

    def dma(self, q, dkey, out, in_, reads=(), writes=(), chain=False, **kw):
        deps = self._deps(reads, writes)
        if not chain and self.cnt[dkey] > 0:
            self._merge(deps, {dkey: self.cnt[dkey]})
        self._wait(q, deps)
        ins = self.eng[q].dma_start(out=out, in_=in_, **kw)
        self.cnt[dkey] += 16
        ins.then_inc(self.sem[dkey], 16)
        me = {dkey: self.cnt[dkey]}
        for t in reads:
            self._merge(t.r, me)
        for t in writes:
            if chain:
                self._merge(t.w, me)
            else:
                t.w = dict(me)
            t.r = {}
        return ins

    def wait_all(self, e, toks):
        deps = {}
        for t in toks:
            self._merge(deps, t.w)
            self._merge(deps, t.r)
        self._wait(e, deps)

    def barrier(self):
        deps = {k: v for k, v in self.cnt.items() if v > 0}
        for e in self.ENG:
            self._wait(e, deps)
        self.free_dsems = [k for k in self.cnt if k.startswith("d") and k[1:].isdigit()]


class Rot:
    def __init__(self, S, aps):
        self.S = S
        self.aps = list(aps)
        self.tk = [Tk() for _ in self.aps]
        self.dk = [None] * len(self.aps)
        self.i = -1

    def next(self):
        self.i = (self.i + 1) % len(self.aps)
        return self.aps[self.i], self.tk[self.i]

    def dkey(self):
        if self.dk[self.i] is None:
            self.dk[self.i] = self.S.dsem()
        return self.dk[self.i]


_SBN = [0]


def sb(nc, es, name, shape, dt):
    _SBN[0] += 1
    return es.enter_context(nc.sbuf_tensor("%s_%d" % (name, _SBN[0]), list(shape), dt))


class Ctx:
    pass


def bcast_row(C, es, name, src_row, N):
    nc, S = C.nc, C.S
    W = min(N, 512)
    row = sb(nc, es, name + "_row", [1, W], F32)
    out = sb(nc, es, name, [128, N], F32)
    t_row, t_out = Tk(), Tk()
    dk = S.dsem()
    for c0 in range(0, N, 512):
        w = min(512, N - c0)
        S.dma("sp", dk, row[0:1, 0:w], src_row[:, c0:c0 + w], writes=[t_row])
        ps, tp = C.psum.next()
        S.op("pe", lambda e: e.matmul(ps[:, 0:w], lhsT=C.ones_row[0:1, :], rhs=row[0:1, 0:w], start=True, stop=True),
             reads=[t_row, C.t_ones], writes=[tp])
        S.op("act", lambda e: e.copy(out=out[:, c0:c0 + w], in_=ps[:, 0:w]), reads=[tp], writes=[t_out])
    return out, t_out

def phase_A(C, l, x_src):
    nc, S = C.nc, C.S
    S.barrier()
    with ExitStack() as es:
        xT = sb(nc, es, "A_xT", [128, 8, T], BF16)
        t_xT = Tk()
        cosT = sb(nc, es, "A_cosT", [128, T], F32)
        sinT = sb(nc, es, "A_sinT", [128, T], F32)
        t_tab = Tk()
        dk_tab = S.dsem()
        S.dma("sp", dk_tab, cosT[:], C.cosT[:, :], writes=[t_tab])
        S.dma("sp", dk_tab, sinT[:], C.sinT[:, :], writes=[t_tab], chain=True)
        cosTM = sb(nc, es, "A_cosTM", [128, NT, 32], F32)
        sinTM = sb(nc, es, "A_sinTM", [128, NT, 32], F32)
        S.dma("sp", dk_tab, cosTM[:], C.cosTM.rearrange("(n p) c -> p n c", p=128), writes=[t_tab], chain=True)
        S.dma("sp", dk_tab, sinTM[:], C.sinTM.rearrange("(n p) c -> p n c", p=128), writes=[t_tab], chain=True)
        zt = sb(nc, es, "A_zeta", [128, 4], F32)
        S.dma("sp", dk_tab, zt[:], C.zeta8[:, :], writes=[t_tab], chain=True)
        sel2 = sb(nc, es, "A_sel2", [8, 512], BF16)
        t_sel = Tk()
        dk_sel = S.dsem()
        S.dma("pool", dk_sel, sel2[:], C.sel2[:, :], writes=[t_sel])

        xin = Rot(S, [sb(nc, es, f"A_xin{i}", [128, D], F32) for i in range(2)])
        for n in range(NT):
            xt_, tx = xin.next()
            S.dma("sp", xin.dkey(), xt_[:], x_src[n * 128:(n + 1) * 128, :], writes=[tx])
            for half in range(2):
                ps, tp = C.psum.next()
                for j in range(4):
                    k = half * 4 + j
                    S.op("pe", lambda e: e.transpose(ps[:, j * 128:(j + 1) * 128], xt_[:, k * 128:(k + 1) * 128], C.ident_f[:]),
                         reads=[tx, C.t_const], writes=[tp])
                dst = xT[:, half * 4:(half + 1) * 4, n * 128:(n + 1) * 128]
                src = ps[:].rearrange("p (k c) -> p k c", k=4)
                if half == 0:
                    S.op("act", lambda e: e.copy(out=dst, in_=src), reads=[tp], writes=[t_xT])
                else:
                    S.op("dve", lambda e: e.tensor_copy(out=dst, in_=src), reads=[tp], writes=[t_xT])

        wtm = sb(nc, es, "A_wtm", [128, 8, NTM], BF16)
        t_wtm = Tk()
        dk_wtm = S.dsem()
        S.dma("pool", dk_wtm, wtm[:], C.w_tm[l].rearrange("(k p) c -> p k c", p=128), writes=[t_wtm])
        tmo = Rot(S, [sb(nc, es, f"A_tmo{i}", [128, NTMO], BF16) for i in range(2)])
        rk32 = Rot(S, [sb(nc, es, f"A_rk{i}", [128, 4, 64], F32) for i in range(2)])
        rkt = Rot(S, [sb(nc, es, f"A_rkt{i}", [128, 4, 64], F32) for i in range(2)])
        rku = Rot(S, [sb(nc, es, f"A_rku{i}", [128, 4, 64], F32) for i in range(2)])
        for n in range(NT):
            ot, to = tmo.next()
            banks = []
            for (c0, cn) in ((0, 512), (512, 512), (1024, NTM - 1024)):
                ps, tp = C.psum.next()
                for k in range(8):
                    S.op("pe", lambda e: e.matmul(ps[:, 0:cn], lhsT=xT[:, k, n * 128:(n + 1) * 128], rhs=wtm[:, k, c0:c0 + cn],
                                                  start=(k == 0), stop=(k == 7)),
                         reads=[t_xT, t_wtm], writes=[tp])
                banks.append((ps, tp))
            S.op("act", lambda e: e.copy(out=ot[:, 0:512], in_=banks[0][0][:, 0:512]), reads=[banks[0][1]], writes=[to])
            S.op("act", lambda e: e.copy(out=ot[:, 512:1024], in_=banks[1][0][:, 0:512]), reads=[banks[1][1]], writes=[to])
            ps2, tp2 = banks[2]
            S.op("act", lambda e: e.activation(out=C.sgn[:, n, :], in_=ps2[:, 256:264], func=AF.Sign), reads=[tp2], writes=[C.t_sgn])
            r32, tr = rk32.next()
            S.op("dve", lambda e: e.tensor_copy(out=r32[:], in_=ps2[:, 0:256].rearrange("p (h d) -> p h d", h=4)), reads=[tp2], writes=[tr])
            cb = cosTM[:, n, :].unsqueeze(1).to_broadcast([128, 4, 32])
            sbb = sinTM[:, n, :].unsqueeze(1).to_broadcast([128, 4, 32])
            ra, tra = rkt.next()
            rb, trb = rku.next()
            x1 = r32[:, :, 0:32]
            x2 = r32[:, :, 32:64]
            S.op("dve", lambda e: e.tensor_tensor(out=ra[:, :, 0:32], in0=x1, in1=cb, op=ALU.mult), reads=[tr, t_tab], writes=[tra])
            S.op("dve", lambda e: e.tensor_tensor(out=ra[:, :, 32:64], in0=x2, in1=cb, op=ALU.mult), reads=[tr, t_tab], writes=[tra])
            S.op("dve", lambda e: e.tensor_tensor(out=rb[:, :, 0:32], in0=x2, in1=sbb, op=ALU.mult), reads=[tr, t_tab], writes=[trb])
            S.op("dve", lambda e: e.tensor_tensor(out=rb[:, :, 32:64], in0=x1, in1=sbb, op=ALU.mult), reads=[tr, t_tab], writes=[trb])
            S.op("dve", lambda e: e.tensor_tensor(out=ra[:, :, 0:32], in0=ra[:, :, 0:32], in1=rb[:, :, 0:32], op=ALU.subtract), reads=[tra, trb], writes=[tra])
            S.op("dve", lambda e: e.tensor_tensor(out=ra[:, :, 32:64], in0=ra[:, :, 32:64], in1=rb[:, :, 32:64], op=ALU.add), reads=[tra, trb], writes=[tra])
            S.op("dve", lambda e: e.tensor_tensor(out=ot[:, 1024:1280].rearrange("p (h d) -> p h d", h=4), in0=ra[:],
                                                  in1=zt[:].unsqueeze(2).to_broadcast([128, 4, 64]), op=ALU.mult),
                 reads=[tra, t_tab], writes=[to])
            S.dma("sp", tmo.dkey(), C.TMO[n * 128:(n + 1) * 128, :], ot[:], reads=[to], writes=[C.t_TMO], chain=True)

        absw = sb(nc, es, "A_absw", [8, T], BF16)
        t_absw = Tk()
        wdw = sb(nc, es, "A_wdw", [128, 8, 8], BF16)
        t_wdw = Tk()
        dk_wdw = S.dsem()
        S.dma("pool", dk_wdw, wdw[:], C.w_dw[l].rearrange("(k p) c -> p k c", p=128), writes=[t_wdw])
        for g in range(NG):
            ps, tp = C.psum.next()
            for k in range(8):
                S.op("pe", lambda e: e.matmul(ps[0:8, :], lhsT=wdw[:, k, :], rhs=xT[:, k, g * 512:(g + 1) * 512],
                                              start=(k == 0), stop=(k == 7)), reads=[t_xT, t_wdw], writes=[tp])
            S.op("act", lambda e: e.activation(out=absw[:, g * 512:(g + 1) * 512], in_=ps[0:8, :], func=AF.Abs), reads=[tp], writes=[t_absw])

        wb = Rot(S, [sb(nc, es, f"A_wb{i}", [128, 8, 512], BF16) for i in range(2)])
        ws = Rot(S, [sb(nc, es, f"A_ws{i}", [128, 8, 512], BF16) for i in range(2)])
        t1r = Rot(S, [sb(nc, es, f"A_t1{i}", [128, 512], F32) for i in range(2)])
        t2r = Rot(S, [sb(nc, es, f"A_t2{i}", [128, 512], F32) for i in range(2)])
        outr = Rot(S, [sb(nc, es, f"A_out{i}", [128, 512], BF16) for i in range(4)])
        groups = [(0, 4, True, 0), (4, 4, True, 512), (8, 4, True, 1024), (12, 4, True, 1536),
                  (16, 1, True, 2048), (17, 4, False, 0)]
        for (c0, ncn, rope, wc0) in groups:
            ncols = 64 if c0 == 16 else ncn * 128
            w_, tw = wb.next()
            src = (C.w_fmr if rope else C.w_fmp)[l]
            S.dma("pool", wb.dkey(), w_[:, :, 0:ncols], src[:, wc0:wc0 + ncols].rearrange("(k p) c -> p k c", p=128), writes=[tw])
            if rope:
                wsw, tws = ws.next()
                S.dma("pool", ws.dkey(), wsw[:, :, 0:ncols], C.w_fms[l][:, wc0:wc0 + ncols].rearrange("(k p) c -> p k c", p=128), writes=[tws])
            for g in range(NG):
                tsl = slice(g * 512, (g + 1) * 512)
                for ci in range(ncn):
                    c = c0 + ci
                    M = 64 if c == 16 else 128
                    wsl = slice(ci * 128, ci * 128 + M)
                    pX, tpX = C.psum.next()
                    for k in range(8):
                        S.op("pe", lambda e: e.matmul(pX[0:M, :], lhsT=w_[:, k, wsl], rhs=xT[:, k, tsl], start=(k == 0), stop=(k == 7)),
                             reads=[t_xT, tw], writes=[tpX])
                    o_, to_ = outr.next()
                    if not rope:
                        S.op("act", lambda e: e.copy(out=o_[0:M, :], in_=pX[0:M, :]), reads=[tpX], writes=[to_])
                    else:
                        pS, tpS = C.psum.next()
                        for k in range(8):
                            S.op("pe", lambda e: e.matmul(pS[0:M, :], lhsT=wsw[:, k, wsl], rhs=xT[:, k, tsl], start=(k == 0), stop=(k == 7)),
                                 reads=[t_xT, tws], writes=[tpS])
                        a1, ta1 = t1r.next()
                        a2, ta2 = t2r.next()
                        S.op("dve", lambda e: e.tensor_tensor(out=a1[0:M, :], in0=pX[0:M, :], in1=cosT[0:M, tsl], op=ALU.mult),
                             reads=[tpX, t_tab], writes=[ta1])
                        S.op("dve", lambda e: e.tensor_tensor(out=a2[0:M, :], in0=pS[0:M, :], in1=sinT[0:M, tsl], op=ALU.mult),
                             reads=[tpS, t_tab], writes=[ta2])
                        if 12 <= c < 16:
                            S.op("dve", lambda e: e.tensor_tensor(out=a1[:], in0=a1[:], in1=a2[:], op=ALU.add), reads=[ta1, ta2], writes=[ta1])
                            pB, tpB = C.psum.next()
                            S.op("pe", lambda e: e.matmul(pB[:], lhsT=sel2[:, (c - 12) * 128:(c - 11) * 128], rhs=absw[:, tsl], start=True, stop=True),
                                 reads=[t_sel, t_absw], writes=[tpB])
                            S.op("dve", lambda e: e.tensor_tensor(out=o_[:], in0=a1[:], in1=pB[:], op=ALU.mult), reads=[ta1, tpB], writes=[to_])
                        else:
                            S.op("dve", lambda e: e.tensor_tensor(out=o_[0:M, :], in0=a1[0:M, :], in1=a2[0:M, :], op=ALU.add),
                                 reads=[ta1, ta2], writes=[to_])
                    S.dma("sp", outr.dkey(), C.FMT[c * 128:c * 128 + M, tsl], o_[0:M, :], reads=[to_], writes=[C.t_FMT], chain=True)


def host_consts():
    inv = (10000.0 ** (-np.arange(0, 64, 2, dtype=np.float32) / 64)).astype(np.float32)
    ang = np.arange(T, dtype=np.float32)[:, None] * inv[None, :]
    cos = np.cos(ang).astype(np.float32)
    sin = np.sin(ang).astype(np.float32)
    p = np.arange(128)
    d = p % 64
    cosT = np.ascontiguousarray(cos[:, d % 32].T)
    sgn = np.where(d < 32, -1.0, 1.0).astype(np.float32)
    sinT = np.ascontiguousarray((sin[:, d % 32] * sgn[None, :]).T)
    log_g = np.log(1.0 - 2.0 ** (-5.0 - np.arange(4, dtype=np.float32))).astype(np.float32)
    n = np.arange(128, dtype=np.float32)
    zeta8 = (np.exp(log_g[None, :] * (127.0 - n[:, None])) * 0.125).astype(np.float32)
    sel2 = np.zeros((8, 512), np.float32)
    for c in range(4):
        sel2[2 * c, c * 128:c * 128 + 64] = 1.0
        sel2[2 * c + 1, c * 128 + 64:c * 128 + 128] = 1.0
    d_mask = np.where(n[:, None] - n[None, :] >= 0, np.exp(log_g[:, None, None] * np.maximum(n[:, None] - n[None, :], 0.0)), 0.0)
    dmaskT = np.ascontiguousarray((d_mask * 0.125).transpose(2, 0, 1)).astype(np.float32)
    xi = np.exp(log_g[:, None] * (n[None, :] + 1.0))
    hp = np.arange(128) // 64
    xiT = np.stack([xi[2 * c + hp] for c in range(2)], axis=1).astype(np.float32)
    gch = np.exp(log_g * 128.0)
    gvec = np.stack([gch[2 * c + hp] for c in range(2)], axis=1).astype(np.float32)
    q = np.arange(128)
    caus = np.where(q[None, :] <= q[:, None], 0.0, NEG).astype(np.float32)
    causF = np.where(q[None, :] <= q[:, None], 0.0, -1e30).astype(np.float32)
    return dict(cosT=cosT, sinT=sinT, cosTM=cos, sinTM=sin, zeta8=zeta8, sel2=sel2,
                ident=np.eye(128, dtype=np.float32), caus=caus, causF=causF,
                dmaskT=dmaskT, xiT=xiT, gvec=gvec)


def prep_weights(inp):
    w_in = np.asarray(inp["w_in"], dtype=np.float32)
    out = {}
    out["w_fmr"] = np.ascontiguousarray(w_in[:, :, FM_ROPE_COLS])
    out["w_fms"] = np.ascontiguousarray(w_in[:, :, FM_ROPE_SW])
    out["w_fmp"] = np.ascontiguousarray(w_in[:, :, FM_PLAIN_COLS])
    out["w_dw"] = np.ascontiguousarray(w_in[:, :, DW_COLS])
    out["w_tm"] = np.ascontiguousarray(w_in[:, :, TM_COLS])
    if "conv_w" in inp:
        L = w_in.shape[0]
        pc = np.zeros((L, 128, 16), np.float32)
        cwt = np.asarray(inp["conv_w"], np.float32)
        for c in range(2):
            for i in range(4):
                pc[:, :, c * 4 + i] = cwt[:, i, c * 128:(c + 1) * 128]
            pc[:, :, 8 + c] = np.asarray(inp["conv_b"])[:, c * 128:(c + 1) * 128]
            pc[:, :, 10 + c] = np.asarray(inp["rg_bx"])[:, c * 128:(c + 1) * 128]
            pc[:, :, 12 + c] = np.asarray(inp["rg_ba"])[:, c * 128:(c + 1) * 128]
            pc[:, :, 14 + c] = np.asarray(inp["rg_lambda"])[:, c * 128:(c + 1) * 128]
        out["rg_pc"] = pc
        wbd = np.zeros((L, 128, 4, 128), np.float32)
        for k, nm in enumerate(("rg_wx", "rg_wa")):
            w = np.asarray(inp[nm], np.float32)
            for c in range(2):
                for b in range(2):
                    wbd[:, b * 64:(b + 1) * 64, k * 2 + c, b * 64:(b + 1) * 64] = w[:, 2 * c + b]
        out["rg_wbd"] = wbd
    if "exp_b1" in inp:
        b1 = np.asarray(inp["exp_b1"], np.float32)
        out["exp_b1p"] = np.ascontiguousarray(b1.reshape(b1.shape[0], b1.shape[1], 16, 128).transpose(0, 1, 3, 2))
    return out


def build(cfg):
    nc = bass.Bass("TRN2", target_bir_lowering=False)
    C = Ctx()
    C.nc = nc
    C.cfg = cfg
    NLD = cfg.get("decl_depth", DEPTH)
    NLAY = cfg.get("layers", DEPTH)

    def din(name, shape, dt=F32):
        return nc.dram_tensor(name, list(shape), dt, kind="ExternalInput").ap()

    def dscr(name, shape, dt):
        kind = "ExternalOutput" if name in cfg.get("debug_out", ()) else ("ExternalInput" if name in cfg.get("debug_in", ()) else "Internal")
        return nc.dram_tensor(name, list(shape), dt, kind=kind).ap()

    x_in = din("x", [T, D])
    C.cosT = din("cosT", [128, T]); C.sinT = din("sinT", [128, T])
    C.cosTM = din("cosTM", [T, 32]); C.sinTM = din("sinTM", [T, 32])
    C.zeta8 = din("zeta8", [128, 4]); C.sel2 = din("sel2", [8, 512])
    ident_d = din("ident", [128, 128])
    C.w_fmr = din("w_fmr", [NLD, D, 2112]); C.w_fms = din("w_fms", [NLD, D, 2112])
    C.w_fmp = din("w_fmp", [NLD, D, 512]); C.w_dw = din("w_dw", [NLD, D, 8])
    C.w_tm = din("w_tm", [NLD, D, NTM])
    caus_d = din("caus", [128, 128]); causF_d = din("causF", [128, 128])
    C.dmaskT = din("dmaskT", [128, 4, 128]); C.xiT = din("xiT", [128, 2, 128]); C.gvec = din("gvec", [128, 2])
    C.ret_gn_g = din("ret_gn_g", [NLD, 256]); C.ret_gn_b = din("ret_gn_b", [NLD, 256])
    C.rg_pc = din("rg_pc", [NLD, 128, 16]); C.rg_wbd = din("rg_wbd", [NLD, 128, 4, 128])
    C.w_out = din("w_out", [NLD, D, D]); C.router_w = din("router_w", [NLD, D, 32]); C.router_b = din("router_b", [NLD, 32])
    C.ln1_g = din("ln1_g", [NLD, D]); C.ln1_b = din("ln1_b", [NLD, D])
    C.X1 = dscr("X1", [T, D], F32); C.X1T = dscr("X1T", [D, T], BF16)
    C.X2 = dscr("X2", [T, D], F32); C.t_X2 = Tk()
    C.W1B = dscr("W1B", [N_EXPERTS, D, 2 * D], BF16); C.W2B = dscr("W2B", [N_EXPERTS, D, D], BF16); C.t_WB = Tk()
    C.OUT = nc.dram_tensor("out", [T, D], F32, kind="ExternalOutput").ap()
    C.exp_w1 = din("exp_w1", [NLD, N_EXPERTS, D, 2 * D]); C.exp_w2 = din("exp_w2", [NLD, N_EXPERTS, D, D])
    C.exp_b1p = din("exp_b1p", [NLD, N_EXPERTS, 128, 16]); C.exp_b2 = din("exp_b2", [NLD, N_EXPERTS, D])
    C.ln2_g = din("ln2_g", [NLD, D]); C.ln2_b = din("ln2_b", [NLD, D])
    C.t_X1 = Tk(); C.t_X1T = Tk(); C.t_xsrc = Tk(); C.t_comb = Tk()
    if "COMBD" in cfg.get("debug_out", ()):
        C.COMBD = nc.dram_tensor("COMBD", [T, 32], F32, kind="ExternalOutput").ap()
    C.MIXT = dscr("MIXT", [1024, T], BF16)
    if cfg.get("dbg_dsa") is not None:
        C.DBG1 = nc.dram_tensor("DBG1", [128, T], F32, kind="ExternalOutput").ap()
        C.DBG2 = nc.dram_tensor("DBG2", [128, NT * 8], F32, kind="ExternalOutput").ap()
        C.DBG3 = nc.dram_tensor("DBG3", [128, T], BF16, kind="ExternalOutput").ap()
        C.t_dbg = Tk()
    C.t_MIXT = Tk()
    C.FMT = dscr("FMT", [NFMC * 128, T], BF16)
    C.TMO = dscr("TMO", [T, NTMO], BF16)
    C.t_FMT = Tk(); C.t_TMO = Tk()

    with ExitStack() as es:
        S = Sched(nc, es)
        C.S = S
        banks = [es.enter_context(nc.psum_tensor(f"ps{i}", [128, 512], F32)) for i in range(8)]
        C.psum = Rot(S, banks[0:6])
        C.acc = Rot(S, banks[6:8])
        for t in C.psum.tk + C.acc.tk:
            t.excl = True
        C.ident_f = sb(nc, es, "ident_f", [128, 128], F32)
        C.ident_b = sb(nc, es, "ident_b", [128, 128], BF16)
        C.t_const = Tk()
        dk = S.dsem()
        S.dma("sp", dk, C.ident_f[:], ident_d[:, :], writes=[C.t_const])
        dk2 = S.dsem()
        S.dma("pool", dk2, C.ident_b[:], ident_d[:, :], writes=[C.t_const], chain=True)
        C.caus_b = sb(nc, es, "caus_b", [128, 128], BF16)
        C.caus_f = sb(nc, es, "caus_f", [128, 128], F32)
        C.ones_f = sb(nc, es, "ones_f", [128, 64], F32)
        S.dma("pool", dk2, C.caus_b[:], caus_d[:, :], writes=[C.t_const], chain=True)
        S.dma("sp", dk, C.caus_f[:], causF_d[:, :], writes=[C.t_const], chain=True)
        C.t_ones = Tk()
        S.op("pool", lambda e: e.memset(C.ones_f[:], 1.0), writes=[C.t_ones])
        C.ones_row = sb(nc, es, "ones_row", [1, 128], F32)
        S.op("pool", lambda e: e.memset(C.ones_row[:], 1.0), writes=[C.t_ones])
        C.sgn = sb(nc, es, "sgn", [128, NT, 8], F32)
        C.COMB = sb(nc, es, "COMB", [128, NT, 32], F32)
        C.t_sgn = Tk()

        for l in range(cfg.get("layers", DEPTH)):
            if "A" in cfg["phases"]:
                phase_A(C, l, x_in if l == 0 else C.X2)
            if "M" in cfg["phases"]:
                phase_moba(C)
            if "S" in cfg["phases"]:
                phase_dsa(C)
            if "R" in cfg["phases"]:
                phase_ret(C, l)
            if "G" in cfg["phases"]:
                phase_rglru(C, l)
            x_src = x_in if l == 0 else C.X2
            if "C" in cfg["phases"]:
                phase_C(C, l, x_src)
            if "W" in cfg["phases"]:
                phase_W(C, l)
            if "D" in cfg["phases"]:
                phase_D(C, l, C.X2 if l < NLAY - 1 else C.OUT)
        S.barrier()
        C.stats = ({k: S.cnt[k] for k in S.ENG}, S.nwaits, S.ndsem)
    return nc


def attn_group(C, A, g, heads, bias_aps, bias_tks, out_row0):
    nc, S = C.nc, C.S
    tsl = slice(g * 512, (g + 1) * 512)
    nkt = 4 * g + 4
    for h in heads:
        c, pb = h // 2, (h % 2) * 64
        oT, toT = C.acc.next()
        for kt in range(nkt):
            ps, tp = C.psum.next()
            S.op("pe", lambda e: e.matmul(ps[:], lhsT=A.KT[pb:pb + 64, c, kt * 128:(kt + 1) * 128], rhs=A.QT[pb:pb + 64, c, tsl],
                                          start=True, stop=False), reads=[A.t_KT, A.t_QT], writes=[tp])
            for jj in range(4):
                S.op("pe", lambda e: e.matmul(ps[:, jj * 128:(jj + 1) * 128], lhsT=bias_aps[jj][:, kt * 128:(kt + 1) * 128], rhs=C.ident_b[:],
                                              start=False, stop=(jj == 3)), reads=[bias_tks[jj], C.t_const], writes=[tp])
            pT, tpT = A.pT.next()
            S.op("act", lambda e: e.activation(out=pT[:], in_=ps[:], func=AF.Exp, scale=0.125), reads=[tp], writes=[tpT])
            S.op("pe", lambda e: e.matmul(oT[0:65, :], lhsT=A.V[:, kt, h, :], rhs=pT[:], start=(kt == 0), stop=(kt == nkt - 1)),
                 reads=[tpT, A.t_V], writes=[toT])
        rec, trec = A.rec.next()
        S.op("dve", lambda e: e.reciprocal(out=rec[64:65, :], in_=oT[64:65, :]), reads=[toT], writes=[trec])
        osb, tosb = A.osb.next()
        S.op("act", lambda e: e.copy(out=osb[0:64, :], in_=oT[0:64, :]), reads=[toT], writes=[tosb])
        pb_, tpb_ = C.psum.next()
        S.op("pe", lambda e: e.matmul(pb_[0:64, :], lhsT=C.ones_f[64:65, 0:64], rhs=rec[64:65, :], start=True, stop=True),
             reads=[trec, C.t_ones], writes=[tpb_])
        om, tom = A.om.next()
        S.op("dve", lambda e: e.tensor_tensor(out=om[0:64, :], in0=osb[0:64, :], in1=pb_[0:64, :], op=ALU.mult), reads=[tosb, tpb_], writes=[tom])
        S.dma("sp", A.om.dkey(), C.MIXT[out_row0 + h * 64:out_row0 + (h + 1) * 64, tsl], om[0:64, :], reads=[tom], writes=[C.t_MIXT], chain=True)


def attn_alloc(C, es, qch, kch, vcol, pref):
    nc, S = C.nc, C.S
    A = Ctx()
    A.KT = sb(nc, es, pref + "KT", [128, 2, T], BF16)
    A.t_KT = Tk()
    dk = S.dsem()
    S.dma("sp", dk, A.KT[:], C.FMT[kch * 128:(kch + 2) * 128, :].rearrange("(c p) t -> p c t", p=128), reads=[C.t_FMT], writes=[A.t_KT])
    A.V = sb(nc, es, pref + "V", [128, NT, 4, 65], BF16)
    A.t_V = Tk()
    S.op("pool", lambda e: e.memset(A.V[:, :, :, 64:65], 1.0), writes=[A.t_V])
    dk2 = S.dsem()
    for h in range(4):
        S.dma("sp", dk2, A.V[:, :, h, 0:64], C.TMO[:, vcol + h * 64:vcol + (h + 1) * 64].rearrange("(n p) d -> p n d", p=128),
              reads=[C.t_TMO], writes=[A.t_V], chain=True)
    A.QTr = Rot(S, [sb(nc, es, pref + f"QT{i}", [128, 2, 512], BF16) for i in range(2)])
    A.qch = qch
    A.pT = Rot(S, [sb(nc, es, pref + f"pT{i}", [128, 512], BF16) for i in range(4)])
    A.rec = Rot(S, [sb(nc, es, pref + f"rec{i}", [128, 512], F32) for i in range(2)])
    A.osb = Rot(S, [sb(nc, es, pref + f"osb{i}", [128, 512], F32) for i in range(2)])
    A.om = Rot(S, [sb(nc, es, pref + f"om{i}", [128, 512], BF16) for i in range(2)])
    return A


def attn_load_q(C, A, g):
    S = C.S
    q_, tq = A.QTr.next()
    S.dma("sp", A.QTr.dkey(), q_[:], C.FMT[A.qch * 128:(A.qch + 2) * 128, g * 512:(g + 1) * 512].rearrange("(c p) t -> p c t", p=128),
          reads=[C.t_FMT], writes=[tq])
    A.QT = _Shift(q_, g * 512)
    A.t_QT = tq


class _Shift:
    def __init__(self, ap, off):
        self.ap = ap
        self.off = off

    def __getitem__(self, key):
        p, c, t = key
        t = slice(t.start - self.off, t.stop - self.off)
        return self.ap[p, c, t]


def phase_moba(C):
    nc, S = C.nc, C.S
    S.barrier()
    with ExitStack() as es:
        A = attn_alloc(C, es, CH["a_q"], CH["a_k"], TMO["a_v"], "M_")
        kms = sb(nc, es, "M_kms", [128, 2, 16], F32)
        kmb = sb(nc, es, "M_kmb", [128, 2, 16], BF16)
        t_km = Tk()
        for c in range(2):
            S.op("dve", lambda e: e.tensor_reduce(out=kms[:, c, :], in_=A.KT[:, c, :].rearrange("p (n k) -> p n k", k=256), axis=AX.X, op=ALU.add),
                 reads=[A.t_KT], writes=[t_km])
        S.op("dve", lambda e: e.tensor_scalar(out=kmb[:], in0=kms[:], scalar1=1.0 / 256.0, scalar2=None, op0=ALU.mult), reads=[t_km], writes=[t_km])
        bias = Rot(S, [sb(nc, es, f"M_bias{i}", [128, T], BF16) for i in range(8)])
        gsb = Rot(S, [sb(nc, es, f"M_g{i}", [128, 16], F32) for i in range(4)])
        m8 = Rot(S, [sb(nc, es, f"M_m8{i}", [128, 8], F32) for i in range(4)])
        sbi = Rot(S, [sb(nc, es, f"M_sb{i}", [128, 16], F32) for i in range(4)])
        for g in range(NG):
            attn_load_q(C, A, g)
            for h in range(4):
                c, pb = h // 2, (h % 2) * 64
                baps, btks = [], []
                for jj in range(4):
                    j = 4 * g + jj
                    own = j // 2
                    b_, tb = bias.next()
                    if own > 0:
                        pg, tpg = C.psum.next()
                        S.op("pe", lambda e: e.matmul(pg[:, 0:16], lhsT=A.QT[pb:pb + 64, c, slice(j * 128, (j + 1) * 128)], rhs=kmb[pb:pb + 64, c, :],
                                                      start=True, stop=True), reads=[A.t_QT, t_km], writes=[tpg])
                        g_, tg = gsb.next()
                        S.op("dve", lambda e: e.tensor_copy(out=g_[:], in_=pg[:, 0:16]), reads=[tpg], writes=[tg])
                        if own < 16:
                            S.op("dve", lambda e: e.memset(g_[:, own:16], -1e30), writes=[tg])
                        m_, tm = m8.next()
                        S.op("dve", lambda e: e.max(out=m_[:], in_=g_[:]), reads=[tg], writes=[tm])
                        S.op("dve", lambda e: e.tensor_scalar(out=m_[:, 2:3], in0=m_[:, 2:3], scalar1=-1e29, scalar2=None, op0=ALU.max), reads=[tm], writes=[tm])
                        s_, ts = sbi.next()
                        S.op("dve", lambda e: e.tensor_scalar(out=s_[:], in0=g_[:], scalar1=m_[:, 2:3], scalar2=NEG, op0=ALU.is_lt, op1=ALU.mult),
                             reads=[tg, tm], writes=[ts])
                        S.op("pool", lambda e: e.tensor_copy(out=b_[:, 0:own * 256].rearrange("p (n k) -> p n k", k=256),
                                                              in_=s_[:, 0:own].unsqueeze(2).to_broadcast([128, own, 256])), reads=[ts], writes=[tb])
                    if j % 2 == 1:
                        S.op("pool", lambda e: e.memset(b_[:, (j - 1) * 128:j * 128], 0.0), writes=[tb])
                    S.op("pool", lambda e: e.tensor_copy(out=b_[:, j * 128:(j + 1) * 128], in_=C.caus_b[:]), reads=[C.t_const], writes=[tb])
                    if jj < 3:
                        S.op("pool", lambda e: e.memset(b_[:, (j + 1) * 128:(4 * g + 4) * 128], NEG), writes=[tb])
                    baps.append(b_)
                    btks.append(tb)
                attn_group(C, A, g, [h], baps, btks, 0)


DSA_ITERS = 16


def phase_dsa(C):
    nc, S = C.nc, C.S
    S.barrier()
    with ExitStack() as es:
        A = attn_alloc(C, es, CH["d_q"], CH["d_k"], TMO["d_v"], "D_")
        KI = sb(nc, es, "D_KI", [128, T], BF16)
        t_KI = Tk()
        dk = S.dsem()
        r0 = CH["d_ki"] * 128
        S.dma("sp", dk, KI[0:64, :], C.FMT[r0:r0 + 64, :], reads=[C.t_FMT], writes=[t_KI])
        S.dma("sp", dk, KI[64:128, :], C.FMT[r0:r0 + 64, :], reads=[C.t_FMT], writes=[t_KI], chain=True)
        if C.cfg.get("dbg_dsa") is not None:
            dkd3 = S.dsem()
            S.dma("sp", dkd3, C.DBG3[:, :], KI[:], reads=[t_KI], writes=[C.t_dbg])
        QI = Rot(S, [sb(nc, es, f"D_QI{i}", [128, 4, 512], BF16) for i in range(2)])
        accr = Rot(S, [sb(nc, es, f"D_acc{i}", [128, T], F32) for i in range(2)])
        rel = Rot(S, [sb(nc, es, f"D_rel{i}", [128, 512], F32) for i in range(3)])
        bias = Rot(S, [sb(nc, es, f"D_bias{i}", [128, T], BF16) for i in range(8)])
        sm = Rot(S, [sb(nc, es, f"D_sm{i}", [128, 8], F32) for i in range(2)])
        stp = Rot(S, [sb(nc, es, f"D_stp{i}", [128, DSA_ITERS], F32) for i in range(2)])
        pw = sb(nc, es, "D_pw", [128, DSA_ITERS], F32)
        thr0 = sb(nc, es, "D_thr0", [128, 1], F32)
        t_pw = Tk()
        for it in range(DSA_ITERS):
            S.op("pool", lambda e: e.memset(pw[:, it:it + 1], 2.0 ** (-(it + 1))), writes=[t_pw])
        S.op("pool", lambda e: e.memset(thr0[:], -1e29), writes=[t_pw])
        for g in range(NG):
            attn_load_q(C, A, g)
            qi_, tqi = QI.next()
            S.dma("sp", QI.dkey(), qi_[:], C.FMT[CH["d_qi"] * 128:(CH["d_qi"] + 4) * 128, g * 512:(g + 1) * 512].rearrange("(c p) t -> p c t", p=128),
                  reads=[C.t_FMT], writes=[tqi])
            baps, btks = [], []
            for jj in range(4):
                j = 4 * g + jj
                L = (j + 1) * 128
                acc_, tacc = accr.next()
                for kc in range((L + 511) // 512):
                    w = min(512, L - kc * 512)
                    ksl = slice(kc * 512, kc * 512 + w)
                    for h in range(8):
                        c, pb = h // 2, (h % 2) * 64
                        ps, tp = C.psum.next()
                        S.op("pe", lambda e: e.matmul(ps[:, 0:w], lhsT=qi_[pb:pb + 64, c, jj * 128:(jj + 1) * 128], rhs=KI[pb:pb + 64, ksl],
                                                      start=True, stop=True), reads=[tqi, t_KI], writes=[tp])
                        r_, tr = rel.next()
                        S.op("act", lambda e: e.activation(out=r_[:, 0:w], in_=ps[:, 0:w], func=AF.Relu), reads=[tp], writes=[tr])
                        if h == 0:
                            S.op("dve", lambda e: e.tensor_scalar(out=acc_[:, ksl], in0=r_[:, 0:w], scalar1=C.sgn[:, j, 0:1], scalar2=None, op0=ALU.mult),
                                 reads=[tr, C.t_sgn], writes=[tacc])
                        else:
                            S.op("dve", lambda e: e.scalar_tensor_tensor(out=acc_[:, ksl], in0=r_[:, 0:w], scalar=C.sgn[:, j, h:h + 1], in1=acc_[:, ksl],
                                                                         op0=ALU.mult, op1=ALU.add), reads=[tr, C.t_sgn, tacc], writes=[tacc])
                b_, tb = bias.next()
                s_, ts = sm.next()
                if C.cfg.get("dbg_dsa") is not None and j == C.cfg["dbg_dsa"]:
                    dkd = S.dsem()
                    S.dma("sp", dkd, C.DBG1[:, :], acc_[:], reads=[tacc], writes=[C.t_dbg])
                    S.dma("sp", dkd, C.DBG2[:, :], C.sgn[:].rearrange("p n h -> p (n h)"), reads=[C.t_sgn], writes=[C.t_dbg], chain=True)
                if j >= 2:
                    S.op("dve", lambda e: e.tensor_reduce(out=s_[:, 0:1], in_=acc_[:, 0:L], axis=AX.X, op=ALU.max), reads=[tacc], writes=[ts])
                    S.op("dve", lambda e: e.tensor_reduce(out=s_[:, 1:2], in_=acc_[:, 0:L], axis=AX.X, op=ALU.min), reads=[tacc], writes=[ts])
                S.op("dve", lambda e: e.tensor_tensor(out=acc_[:, j * 128:L], in0=acc_[:, j * 128:L], in1=C.caus_f[:], op=ALU.add),
                     reads=[tacc, C.t_const], writes=[tacc])
                if j >= 2:
                    st_, tst = stp.next()
                    S.op("dve", lambda e: e.tensor_tensor(out=s_[:, 2:3], in0=s_[:, 0:1], in1=s_[:, 1:2], op=ALU.subtract), reads=[ts], writes=[ts])
                    S.op("dve", lambda e: e.tensor_scalar(out=s_[:, 2:3], in0=s_[:, 2:3], scalar1=1.0001, scalar2=1e-6, op0=ALU.mult, op1=ALU.add),
                         reads=[ts], writes=[ts])
                    S.op("dve", lambda e: e.tensor_tensor(out=st_[:], in0=pw[:], in1=s_[:, 2:3].to_broadcast([128, DSA_ITERS]), op=ALU.mult),
                         reads=[ts, t_pw], writes=[tst])
                    S.op("dve", lambda e: e.tensor_copy(out=s_[:, 3:4], in_=s_[:, 1:2]), reads=[ts], writes=[ts])
                    for it in range(DSA_ITERS):
                        S.op("dve", lambda e: e.tensor_tensor(out=s_[:, 4:5], in0=s_[:, 3:4], in1=st_[:, it:it + 1], op=ALU.add), reads=[ts, tst], writes=[ts])
                        S.op("dve", lambda e: e.tensor_scalar(out=b_[:, 0:L], in0=acc_[:, 0:L], scalar1=s_[:, 4:5], scalar2=None, op0=ALU.is_ge, op1=ALU.add,
                                                              accum_out=s_[:, 5:6]), reads=[ts, tacc], writes=[ts, tb])
                        S.op("dve", lambda e: e.scalar_tensor_tensor(out=s_[:, 6:7], in0=s_[:, 5:6], scalar=256.0, in1=st_[:, it:it + 1], op0=ALU.is_ge, op1=ALU.mult),
                             reads=[ts, tst], writes=[ts])
                        S.op("dve", lambda e: e.tensor_tensor(out=s_[:, 3:4], in0=s_[:, 3:4], in1=s_[:, 6:7], op=ALU.add), reads=[ts], writes=[ts])
                    thr = s_[:, 3:4]
                else:
                    thr = thr0[:]
                S.op("dve", lambda e: e.tensor_scalar(out=b_[:, 0:L], in0=acc_[:, 0:L], scalar1=thr, scalar2=NEG, op0=ALU.is_lt, op1=ALU.mult),
                     reads=[ts, tacc, t_pw], writes=[tb])
                if jj < 3:
                    S.op("pool", lambda e: e.memset(b_[:, L:(4 * g + 4) * 128], NEG), writes=[tb])
                baps.append(b_)
                btks.append(tb)
            attn_group(C, A, g, [0, 1, 2, 3], baps, btks, 768)


def phase_ret(C, l):
    nc, S = C.nc, C.S
    S.barrier()
    with ExitStack() as es:
        RG = sb(nc, es, "R_RG", [128, NT, 256], BF16)
        OF = sb(nc, es, "R_OF", [128, NT, 256], F32)
        t_of, t_rg = Tk(), Tk()
        dk0 = S.dsem()
        S.dma("sp", dk0, RG[:], C.TMO[:, TMO["r_g"]:TMO["r_g"] + 256].rearrange("(n p) c -> p n c", p=128), reads=[C.t_TMO], writes=[t_rg])
        gng, t_gng = bcast_row(C, es, "R_gng", C.ret_gn_g[l:l + 1, :], 256)
        gnb, t_gnb = bcast_row(C, es, "R_gnb", C.ret_gn_b[l:l + 1, :], 256)
        with ExitStack() as es1:
            RQ = sb(nc, es1, "R_RQ", [128, 2, T], BF16)
            RK = sb(nc, es1, "R_RK", [128, 2, T], BF16)
            RQX = sb(nc, es1, "R_RQX", [128, 2, T], BF16)
            V = sb(nc, es1, "R_V", [128, NT, 256], BF16)
            RKZ = sb(nc, es1, "R_RKZ", [128, NT, 256], BF16)
            dmT = sb(nc, es1, "R_dm", [128, 4, 128], F32)
            xiT = sb(nc, es1, "R_xi", [128, 2, 128], BF16)
            gv = sb(nc, es1, "R_gv", [128, 2], F32)
            Rf = sb(nc, es1, "R_Rf", [128, 2, 64], F32)
            Rb = Rot(S, [sb(nc, es1, f"R_Rb{i}", [128, 2, 64], BF16) for i in range(2)])
            t_in, t_c, t_rqx, t_Rf = Tk(), Tk(), Tk(), Tk()
            dk = S.dsem()
            S.dma("sp", dk, RQ[:], C.FMT[CH["r_q"] * 128:(CH["r_q"] + 2) * 128, :].rearrange("(c p) t -> p c t", p=128), reads=[C.t_FMT], writes=[t_in])
            S.dma("sp", dk, RK[:], C.FMT[CH["r_k"] * 128:(CH["r_k"] + 2) * 128, :].rearrange("(c p) t -> p c t", p=128), reads=[C.t_FMT], writes=[t_in], chain=True)
            S.dma("sp", dk, V[:], C.TMO[:, TMO["r_v"]:TMO["r_v"] + 256].rearrange("(n p) c -> p n c", p=128), reads=[C.t_TMO], writes=[t_in], chain=True)
            S.dma("sp", dk, RKZ[:], C.TMO[:, TMO["rkz"]:TMO["rkz"] + 256].rearrange("(n p) c -> p n c", p=128), reads=[C.t_TMO], writes=[t_in], chain=True)
            dk2 = S.dsem()
            S.dma("sp", dk2, dmT[:], C.dmaskT[:, :, :], writes=[t_c])
            S.dma("sp", dk2, gv[:], C.gvec[:, :], writes=[t_c], chain=True)
            dk3 = S.dsem()
            S.dma("pool", dk3, xiT[:], C.xiT[:, :, :], writes=[t_c], chain=True)
            cut = C.cfg.get("cut", 99)
            if cut <= 1:
                return
            for c in range(2):
                for n in range(NT):
                    csl = slice(n * 128, (n + 1) * 128)
                    eng = "dve" if n % 2 == 0 else "pool"
                    S.op(eng, lambda e: e.tensor_tensor(out=RQX[:, c, csl], in0=RQ[:, c, csl], in1=xiT[:, c, :], op=ALU.mult), reads=[t_in, t_c], writes=[])
            t_rqx.w = {"dve": S.cnt["dve"], "pool": S.cnt["pool"]}
            S.op("dve", lambda e: e.memset(Rf[:], 0.0), writes=[t_Rf])
            if cut <= 2:
                return
            inr = Rot(S, [sb(nc, es1, f"R_in{i}", [128, 4, 128], BF16) for i in range(2)])
            rb_prev, trb_prev = None, None
            lsub = C.cfg.get("lsub", 9)
            for n in range(C.cfg.get("nchunks", NT)):
                csl = slice(n * 128, (n + 1) * 128)
                pIa, tpIa = C.psum.next()
                pIb, tpIb = C.psum.next()
                in_, tin = inr.next()
                for (pI, tpI, hs) in ((pIa, tpIa, (0, 2)), (pIb, tpIb, (1, 3))):
                    for i, h in enumerate(hs):
                        c, pb = h // 2, (h % 2) * 64
                        S.op("pe", lambda e: e.matmul(pI[:, i * 128:(i + 1) * 128], lhsT=RK[pb:pb + 64, c, csl], rhs=RQ[pb:pb + 64, c, csl], start=True, stop=True),
                             reads=[t_in], writes=[tpI])
                for (pI, tpI, hs) in ((pIa, tpIa, (0, 2)), (pIb, tpIb, (1, 3))):
                    for i, h in enumerate(hs):
                        S.op("dve", lambda e: e.tensor_tensor(out=in_[:, h, :], in0=pI[:, i * 128:(i + 1) * 128], in1=dmT[:, h, :], op=ALU.mult), reads=[tpI, t_c], writes=[tin])
                if lsub <= 1:
                    continue
                pO, tpO = C.psum.next()
                for h in range(4):
                    c, pb = h // 2, (h % 2) * 64
                    S.op("pe", lambda e: e.matmul(pO[:, h * 64:(h + 1) * 64], lhsT=in_[:, h, :], rhs=V[:, n, h * 64:(h + 1) * 64], start=True, stop=(n == 0)),
                         reads=[tin, t_in], writes=[tpO])
                    if n > 0:
                        S.op("pe", lambda e: e.matmul(pO[:, h * 64:(h + 1) * 64], lhsT=RQX[pb:pb + 64, c, csl], rhs=rb_prev[pb:pb + 64, c, :], start=False, stop=True),
                             reads=[t_rqx, trb_prev], writes=[tpO])
                S.op("act", lambda e: e.copy(out=OF[:, n, :], in_=pO[:, 0:256]), reads=[tpO], writes=[t_of])
                if lsub <= 2:
                    continue
                if n < NT - 1:
                    rb_, trb = Rb.next()
                    for c in range(2):
                        pK, tpK = C.psum.next()
                        S.op("pe", lambda e: e.matmul(pK[:, 0:128], lhsT=RKZ[:, n, c * 128:(c + 1) * 128], rhs=V[:, n, c * 128:(c + 1) * 128], start=True, stop=True),
                             reads=[t_in], writes=[tpK])
                        for hh in range(2):
                            ps_ = slice(hh * 64, (hh + 1) * 64)
                            S.op("dve", lambda e: e.scalar_tensor_tensor(out=Rf[ps_, c, :], in0=Rf[ps_, c, :], scalar=gv[ps_, c:c + 1], in1=pK[ps_, hh * 64:(hh + 1) * 64],
                                                                         op0=ALU.mult, op1=ALU.add), reads=[tpK, t_Rf, t_c], writes=[t_Rf])
                    S.op("act", lambda e: e.copy(out=rb_[:], in_=Rf[:]), reads=[t_Rf], writes=[trb])
                    rb_prev, trb_prev = rb_, trb
            S.barrier()
        if cut <= 3:
            return
        SQ = sb(nc, es, "R_SQ", [128, NT, 256], BF16)
        SG = sb(nc, es, "R_SG", [128, NT, 256], F32)
        mu = sb(nc, es, "R_mu", [128, NT * 4], F32)
        ss = sb(nc, es, "R_ss", [128, NT * 4], F32)
        t_mu, t_sq, t_sg = Tk(), Tk(), Tk()
        S.op("act", lambda e: e.activation(out=SG[:], in_=RG[:], func=AF.Silu), reads=[t_rg], writes=[t_sg])
        for n in range(NT):
            O3 = OF[:, n, :].rearrange("p (h d) -> p h d", d=64)
            S3 = SQ[:, n, :].rearrange("p (h d) -> p h d", d=64)
            m_ = mu[:, n * 4:(n + 1) * 4]
            s_ = ss[:, n * 4:(n + 1) * 4]
            S.op("dve", lambda e: e.tensor_reduce(out=m_, in_=O3, axis=AX.X, op=ALU.add), reads=[t_of], writes=[t_mu])
            S.op("dve", lambda e: e.tensor_scalar(out=m_, in0=m_, scalar1=-1.0 / 64.0, scalar2=None, op0=ALU.mult), reads=[t_mu], writes=[t_mu])
            S.op("dve", lambda e: e.tensor_tensor(out=O3, in0=O3, in1=m_.unsqueeze(2).to_broadcast([128, 4, 64]), op=ALU.add), reads=[t_of, t_mu], writes=[t_of])
            S.op("act", lambda e: e.activation(out=SQ[:, n, :], in_=OF[:, n, :], func=AF.Square), reads=[t_of], writes=[t_sq])
            S.op("dve", lambda e: e.tensor_reduce(out=s_, in_=S3, axis=AX.X, op=ALU.add), reads=[t_sq], writes=[t_mu])
            S.op("dve", lambda e: e.tensor_scalar(out=s_, in0=s_, scalar1=1.0 / 64.0, scalar2=LN_EPS, op0=ALU.mult, op1=ALU.add), reads=[t_mu], writes=[t_mu])
        S.op("act", lambda e: e.activation(out=ss[:], in_=ss[:], func=AF.Sqrt), reads=[t_mu], writes=[t_mu])
        S.op("dve", lambda e: e.reciprocal(out=ss[:], in_=ss[:]), reads=[t_mu], writes=[t_mu])
        for n in range(NT):
            O3 = OF[:, n, :].rearrange("p (h d) -> p h d", d=64)
            s_ = ss[:, n * 4:(n + 1) * 4]
            S.op("dve", lambda e: e.tensor_tensor(out=O3, in0=O3, in1=s_.unsqueeze(2).to_broadcast([128, 4, 64]), op=ALU.mult), reads=[t_of, t_mu], writes=[t_of])
            S.op("dve", lambda e: e.tensor_tensor(out=OF[:, n, :], in0=OF[:, n, :], in1=gng[:], op=ALU.mult), reads=[t_of, t_gng], writes=[t_of])
            S.op("dve", lambda e: e.tensor_tensor(out=OF[:, n, :], in0=OF[:, n, :], in1=gnb[:], op=ALU.add), reads=[t_of, t_gnb], writes=[t_of])
            S.op("dve", lambda e: e.tensor_tensor(out=SQ[:, n, :], in0=OF[:, n, :], in1=SG[:, n, :], op=ALU.mult), reads=[t_of, t_sg, t_sq], writes=[t_sq])
        if cut <= 4:
            return
        stg = Rot(S, [sb(nc, es, f"R_stg{i}", [128, 2, 512], BF16) for i in range(2)])
        for g in range(NG):
            st_, tst = stg.next()
            for c in range(2):
                pT, tpT = C.psum.next()
                pTb = pT[:].bitcast(BF16)
                for jj in range(4):
                    n = 4 * g + jj
                    S.op("pe", lambda e: e.transpose(pTb[:, jj * 128:(jj + 1) * 128], SQ[:, n, c * 128:(c + 1) * 128], C.ident_b[:]), reads=[t_sq, C.t_const], writes=[tpT])
                S.op("act", lambda e: e.copy(out=st_[:, c, :], in_=pTb[:, 0:512]), reads=[tpT], writes=[tst])
            S.dma("sp", stg.dkey(), C.MIXT[256:512, g * 512:(g + 1) * 512].rearrange("(c p) t -> p c t", p=128), st_[:], reads=[tst], writes=[C.t_MIXT], chain=True)


def phase_rglru(C, l):
    nc, S = C.nc, C.S
    S.barrier()
    with ExitStack() as es:
        pc = sb(nc, es, "G_pc", [128, 16], F32)
        wbd = sb(nc, es, "G_wbd", [128, 4, 128], BF16)
        t_pc, t_w = Tk(), Tk()
        dk = S.dsem()
        S.dma("sp", dk, pc[:], C.rg_pc[l], writes=[t_pc])
        dkw = S.dsem()
        S.dma("pool", dkw, wbd[:], C.rg_wbd[l], writes=[t_w])
        sc = sb(nc, es, "G_sc", [128, 4], F32)
        S.op("act", lambda e: e.activation(out=sc[:, 0:2], in_=pc[:, 14:16], func=AF.Exp, scale=-1.0), reads=[t_pc], writes=[t_pc])
        S.op("dve", lambda e: e.tensor_scalar(out=sc[:, 0:2], in0=sc[:, 0:2], scalar1=1.0, scalar2=None, op0=ALU.add), reads=[t_pc], writes=[t_pc])
        S.op("act", lambda e: e.activation(out=sc[:, 0:2], in_=sc[:, 0:2], func=AF.Ln), reads=[t_pc], writes=[t_pc])
        S.op("dve", lambda e: e.tensor_scalar(out=sc[:, 2:4], in0=sc[:, 0:2], scalar1=-16.0, scalar2=None, op0=ALU.mult), reads=[t_pc], writes=[t_pc])
        S.op("dve", lambda e: e.tensor_scalar(out=sc[:, 0:2], in0=sc[:, 0:2], scalar1=-8.0, scalar2=None, op0=ALU.mult), reads=[t_pc], writes=[t_pc])
        CX = sb(nc, es, "G_CX", [128, T], BF16)
        CG = sb(nc, es, "G_CG", [128, T], BF16)
        xc = sb(nc, es, "G_xc", [128, T], F32)
        xcb = sb(nc, es, "G_xcb", [128, T], BF16)
        gx = sb(nc, es, "G_gx", [128, T], F32)
        ga = sb(nc, es, "G_ga", [128, T], F32)
        a2 = sb(nc, es, "G_a2", [128, T], F32)
        gl = sb(nc, es, "G_gl", [128, T], F32)
        hh = sb(nc, es, "G_h", [128, T], F32)
        ob = sb(nc, es, "G_ob", [128, T], BF16)
        t_cx, t_cg, t_xc, t_xcb, t_gx, t_ga, t_a2, t_gl, t_h, t_ob = (Tk() for _ in range(10))
        dkx, dkg, dko = S.dsem(), S.dsem(), S.dsem()
        for c in range(2):
            S.dma("sp", dkx, CX[:], C.FMT[(CH["c_x"] + c) * 128:(CH["c_x"] + c + 1) * 128, :], reads=[C.t_FMT], writes=[t_cx])
            S.dma("sp", dkg, CG[:], C.FMT[(CH["c_g"] + c) * 128:(CH["c_g"] + c + 1) * 128, :], reads=[C.t_FMT], writes=[t_cg])
            cw = lambda i: pc[:, c * 4 + i:c * 4 + i + 1]
            S.op("dve", lambda e: e.tensor_scalar(out=xc[:], in0=CX[:], scalar1=cw(3), scalar2=pc[:, 8 + c:9 + c], op0=ALU.mult, op1=ALU.add),
                 reads=[t_cx, t_pc], writes=[t_xc])
            for sh in (1, 2, 3):
                S.op("dve", lambda e: e.scalar_tensor_tensor(out=xc[:, sh:T], in0=CX[:, 0:T - sh], scalar=cw(3 - sh), in1=xc[:, sh:T], op0=ALU.mult, op1=ALU.add),
                     reads=[t_cx, t_pc, t_xc], writes=[t_xc])
            S.op("act", lambda e: e.copy(out=xcb[:], in_=xc[:]), reads=[t_xc], writes=[t_xcb])
            for g in range(NG):
                tsl = slice(g * 512, (g + 1) * 512)
                pX, tpX = C.psum.next()
                S.op("pe", lambda e: e.matmul(pX[:], lhsT=wbd[:, c, :], rhs=xcb[:, tsl], start=True, stop=True), reads=[t_w, t_xcb], writes=[tpX])
                S.op("act", lambda e: e.activation(out=gx[:, tsl], in_=pX[:], func=AF.Sigmoid, bias=pc[:, 10 + c:11 + c]), reads=[tpX, t_pc], writes=[t_gx])
                pA, tpA = C.psum.next()
                S.op("pe", lambda e: e.matmul(pA[:], lhsT=wbd[:, 2 + c, :], rhs=xcb[:, tsl], start=True, stop=True), reads=[t_w, t_xcb], writes=[tpA])
                S.op("act", lambda e: e.activation(out=ga[:, tsl], in_=pA[:], func=AF.Sigmoid, bias=pc[:, 12 + c:13 + c]), reads=[tpA, t_pc], writes=[t_ga])
            S.op("act", lambda e: e.copy(out=gl[:], in_=CG[:]), reads=[t_cg], writes=[t_gl])
            S.op("pool", lambda e: e.tensor_tensor(out=hh[:], in0=gl[:], in1=gl[:], op=ALU.mult), reads=[t_gl], writes=[t_h])
            S.op("dve", lambda e: e.tensor_scalar(out=hh[:], in0=hh[:], scalar1=0.044715, scalar2=1.0, op0=ALU.mult, op1=ALU.add), reads=[t_h], writes=[t_h])
            S.op("dve", lambda e: e.tensor_tensor(out=hh[:], in0=hh[:], in1=gl[:], op=ALU.mult), reads=[t_h, t_gl], writes=[t_h])
            S.op("act", lambda e: e.activation(out=hh[:], in_=hh[:], func=AF.Sigmoid, scale=1.5957691216), reads=[t_h], writes=[t_h])
            S.op("dve", lambda e: e.tensor_tensor(out=gl[:], in0=gl[:], in1=hh[:], op=ALU.mult), reads=[t_h, t_gl], writes=[t_gl])
            S.op("act", lambda e: e.activation(out=a2[:], in_=ga[:], func=AF.Exp, scale=sc[:, 2 + c:3 + c]), reads=[t_ga, t_pc], writes=[t_a2])
            S.op("act", lambda e: e.activation(out=ga[:], in_=ga[:], func=AF.Exp, scale=sc[:, c:c + 1]), reads=[t_ga, t_pc], writes=[t_ga])
            S.op("dve", lambda e: e.tensor_scalar(out=a2[:], in0=a2[:], scalar1=-1.0, scalar2=1.0, op0=ALU.mult, op1=ALU.add), reads=[t_a2], writes=[t_a2])
            S.op("act", lambda e: e.activation(out=a2[:], in_=a2[:], func=AF.Sqrt), reads=[t_a2], writes=[t_a2])
            S.op("dve", lambda e: e.tensor_tensor(out=gx[:], in0=gx[:], in1=xc[:], op=ALU.mult), reads=[t_gx, t_xc], writes=[t_gx])
            S.op("dve", lambda e: e.tensor_tensor(out=gx[:], in0=gx[:], in1=a2[:], op=ALU.mult), reads=[t_gx, t_a2], writes=[t_gx])
            S.op("dve", lambda e: e.tensor_tensor_scan(out=hh[:], data0=ga[:], data1=gx[:], initial=0.0, op0=ALU.mult, op1=ALU.add), reads=[t_ga, t_gx, t_h], writes=[t_h])
            S.op("dve", lambda e: e.tensor_tensor(out=ob[:], in0=hh[:], in1=gl[:], op=ALU.mult), reads=[t_h, t_gl], writes=[t_ob])
            S.dma("sp", dko, C.MIXT[512 + c * 128:512 + (c + 1) * 128, :], ob[:], reads=[t_ob], writes=[C.t_MIXT], chain=True)


def ln_tile(C, L, y, ty, gbc, t_g, bbc, t_b):
    S = C.S
    st, tst = L.st.next()
    jk, tjk = L.jk.next()
    S.op("dve", lambda e: e.tensor_reduce(out=st[:, 0:1], in_=y[:], axis=AX.X, op=ALU.add), reads=[ty], writes=[tst])
    S.op("act", lambda e: e.activation(out=jk[:], in_=y[:], func=AF.Square), reads=[ty], writes=[tjk])
    S.op("dve", lambda e: e.tensor_reduce(out=st[:, 1:2], in_=jk[:], axis=AX.X, op=ALU.add), reads=[tjk, tst], writes=[tst])
    S.op("dve", lambda e: e.tensor_scalar(out=st[:, 0:2], in0=st[:, 0:2], scalar1=1.0 / D, scalar2=None, op0=ALU.mult), reads=[tst], writes=[tst])
    S.op("dve", lambda e: e.tensor_tensor(out=st[:, 2:3], in0=st[:, 0:1], in1=st[:, 0:1], op=ALU.mult), reads=[tst], writes=[tst])
    S.op("dve", lambda e: e.tensor_tensor(out=st[:, 1:2], in0=st[:, 1:2], in1=st[:, 2:3], op=ALU.subtract), reads=[tst], writes=[tst])
    S.op("dve", lambda e: e.tensor_scalar(out=st[:, 1:2], in0=st[:, 1:2], scalar1=LN_EPS, scalar2=None, op0=ALU.add), reads=[tst], writes=[tst])
    S.op("act", lambda e: e.activation(out=st[:, 1:2], in_=st[:, 1:2], func=AF.Sqrt), reads=[tst], writes=[tst])
    S.op("dve", lambda e: e.reciprocal(out=st[:, 1:2], in_=st[:, 1:2]), reads=[tst], writes=[tst])
    S.op("dve", lambda e: e.tensor_scalar(out=y[:], in0=y[:], scalar1=st[:, 0:1], scalar2=st[:, 1:2], op0=ALU.subtract, op1=ALU.mult), reads=[tst, ty], writes=[ty])
    S.op("dve", lambda e: e.tensor_tensor(out=y[:], in0=y[:], in1=gbc[:], op=ALU.mult), reads=[ty, t_g], writes=[ty])
    S.op("dve", lambda e: e.tensor_tensor(out=y[:], in0=y[:], in1=bbc[:], op=ALU.add), reads=[ty, t_b], writes=[ty])


def ln_alloc(C, es, pref):
    nc, S = C.nc, C.S
    L = Ctx()
    L.st = Rot(S, [sb(nc, es, pref + f"st{i}", [128, 4], F32) for i in range(2)])
    L.jk = Rot(S, [sb(nc, es, pref + f"jk{i}", [128, D], BF16) for i in range(2)])
    return L


def phase_C(C, l, x_src):
    nc, S = C.nc, C.S
    S.barrier()
    with ExitStack() as es:
        Wo = sb(nc, es, "C_Wo", [128, 8, D], BF16)
        t_wo = Tk()
        dkw = S.dsem()
        for hf in range(2):
            S.dma("pool", dkw, Wo[:, hf * 4:(hf + 1) * 4, :], C.w_out[l][hf * 512:(hf + 1) * 512, :].rearrange("(k p) d -> p k d", p=128), writes=[t_wo], chain=True)
        RW = sb(nc, es, "C_RW", [128, 8, 32], F32)
        RWh = sb(nc, es, "C_RWh", [128, 8, 32], BF16)
        RWl = sb(nc, es, "C_RWl", [128, 8, 32], BF16)
        t_rw = Tk()
        dkr = S.dsem()
        S.dma("sp", dkr, RW[:], C.router_w[l].rearrange("(k p) e -> p k e", p=128), writes=[t_rw])
        S.op("act", lambda e: e.copy(out=RWh[:], in_=RW[:]), reads=[t_rw], writes=[t_rw])
        S.op("dve", lambda e: e.tensor_tensor(out=RWl[:], in0=RW[:], in1=RWh[:], op=ALU.subtract), reads=[t_rw], writes=[t_rw])
        g1, t_g1 = bcast_row(C, es, "C_g1", C.ln1_g[l:l + 1, :], D)
        b1, t_b1 = bcast_row(C, es, "C_b1", C.ln1_b[l:l + 1, :], D)
        rb, t_rb = bcast_row(C, es, "C_rb", C.router_b[l:l + 1, :], 32)
        L = ln_alloc(C, es, "C_")
        MT = Rot(S, [sb(nc, es, f"C_MT{i}", [128, 8, 512], BF16) for i in range(2)])
        xin = Rot(S, [sb(nc, es, f"C_x{i}", [128, D], F32) for i in range(2)])
        yr = Rot(S, [sb(nc, es, f"C_y{i}", [128, D], F32) for i in range(2)])
        xtb = Rot(S, [sb(nc, es, f"C_xtb{i}", [128, 8, 128], BF16) for i in range(2)])
        xtf = Rot(S, [sb(nc, es, f"C_xtf{i}", [128, 8, 128], BF16) for i in range(2)])
        sm = Rot(S, [sb(nc, es, f"C_sm{i}", [128, 128], F32) for i in range(2)])
        for g in range(NG):
            mt, tmt = MT.next()
            S.dma("sp", MT.dkey(), mt[:], C.MIXT[:, g * 512:(g + 1) * 512].rearrange("(k p) t -> p k t", p=128), reads=[C.t_MIXT], writes=[tmt])
            for jj in range(4):
                n = 4 * g + jj
                x_, tx = xin.next()
                S.dma("sp", xin.dkey(), x_[:], x_src[n * 128:(n + 1) * 128, :], reads=[C.t_xsrc], writes=[tx])
                y_, ty = yr.next()
                for hf in range(2):
                    ps, tp = C.psum.next()
                    for k in range(8):
                        S.op("pe", lambda e: e.matmul(ps[:], lhsT=mt[:, k, jj * 128:(jj + 1) * 128], rhs=Wo[:, k, hf * 512:(hf + 1) * 512], start=(k == 0), stop=(k == 7)),
                             reads=[tmt, t_wo], writes=[tp])
                    S.op("dve", lambda e: e.scalar_tensor_tensor(out=y_[:, hf * 512:(hf + 1) * 512], in0=x_[:, hf * 512:(hf + 1) * 512], scalar=ALPHA, in1=ps[:],
                                                                 op0=ALU.mult, op1=ALU.add), reads=[tx, tp], writes=[ty])
                ln_tile(C, L, y_, ty, g1, t_g1, b1, t_b1)
                S.dma("sp", yr.dkey(), C.X1[n * 128:(n + 1) * 128, :], y_[:], reads=[ty], writes=[C.t_X1], chain=True)
                tb_, ttb = xtb.next()
                tf_, ttf = xtf.next()
                for hf in range(2):
                    ps, tp = C.psum.next()
                    for j in range(4):
                        k = hf * 4 + j
                        S.op("pe", lambda e: e.transpose(ps[:, j * 128:(j + 1) * 128], y_[:, k * 128:(k + 1) * 128], C.ident_f[:]), reads=[ty, C.t_const], writes=[tp])
                    src = ps[:].rearrange("p (k c) -> p k c", k=4)
                    S.op("act", lambda e: e.copy(out=tb_[:, hf * 4:(hf + 1) * 4, :], in_=src), reads=[tp], writes=[ttb])
                    S.op("dve", lambda e: e.tensor_tensor(out=tf_[:, hf * 4:(hf + 1) * 4, :], in0=src, in1=tb_[:, hf * 4:(hf + 1) * 4, :], op=ALU.subtract), reads=[tp, ttb], writes=[ttf])
                S.dma("sp", xtb.dkey(), C.X1T[:, n * 128:(n + 1) * 128].rearrange("(k p) t -> p k t", p=128), tb_[:], reads=[ttb], writes=[C.t_X1T], chain=True)
                pr, tpr = C.psum.next()
                i3 = 0
                for (xa, wa) in ((tb_, RWh), (tf_, RWh), (tb_, RWl)):
                    for k in range(8):
                        S.op("pe", lambda e: e.matmul(pr[:, 0:32], lhsT=xa[:, k, :], rhs=wa[:, k, :], start=(i3 == 0), stop=(i3 == 23)), reads=[ttf, ttb, t_rw], writes=[tpr])
                        i3 += 1
                s_, ts = sm.next()
                lg, ex, m8 = s_[:, 0:32], s_[:, 32:64], s_[:, 64:72]
                S.op("dve", lambda e: e.tensor_tensor(out=lg, in0=pr[:, 0:32], in1=rb[:], op=ALU.add), reads=[tpr, t_rb], writes=[ts])
                S.op("dve", lambda e: e.max(out=m8, in_=lg), reads=[ts], writes=[ts])
                S.op("dve", lambda e: e.tensor_scalar(out=s_[:, 72:73], in0=s_[:, 64:65], scalar1=-1.0, scalar2=None, op0=ALU.mult), reads=[ts], writes=[ts])
                S.op("act", lambda e: e.activation(out=ex, in_=lg, func=AF.Exp, bias=s_[:, 72:73]), reads=[ts], writes=[ts])
                S.op("dve", lambda e: e.scalar_tensor_tensor(out=ex, in0=lg, scalar=s_[:, 67:68], in1=ex, op0=ALU.is_ge, op1=ALU.mult), reads=[ts], writes=[ts])
                S.op("dve", lambda e: e.tensor_reduce(out=s_[:, 73:74], in_=ex, axis=AX.X, op=ALU.add), reads=[ts], writes=[ts])
                S.op("dve", lambda e: e.reciprocal(out=s_[:, 73:74], in_=s_[:, 73:74]), reads=[ts], writes=[ts])
                S.op("dve", lambda e: e.tensor_scalar(out=C.COMB[:, n, :], in0=ex, scalar1=s_[:, 73:74], scalar2=None, op0=ALU.mult), reads=[ts], writes=[C.t_comb])
        if "COMBD" in C.cfg.get("debug_out", ()):
            dkc = S.dsem()
            S.dma("sp", dkc, C.COMBD.rearrange("(n p) e -> p n e", p=128), C.COMB[:], reads=[C.t_comb], writes=[Tk()])


def phase_W(C, l):
    nc, S = C.nc, C.S
    S.barrier()
    NE = C.cfg.get("n_experts", N_EXPERTS)
    with ExitStack() as es:
        stg = Rot(S, [sb(nc, es, f"W_stg{i}", [128, 8192], F32) for i in range(2)])
        ob = Rot(S, [sb(nc, es, f"W_ob{i}", [128, 8192], BF16) for i in range(2)])
        for ex in range(NE):
            pieces = [(C.exp_w1[l, ex, 0:512, :], C.W1B[ex, 0:512, :], 4, 2048), (C.exp_w1[l, ex, 512:1024, :], C.W1B[ex, 512:1024, :], 4, 2048),
                      (C.exp_w2[l, ex], C.W2B[ex], 8, 1024)]
            for (src, dst, kk, ff) in pieces:
                st_, tst = stg.next()
                S.dma("sp", stg.dkey(), st_[:].rearrange("p (k f) -> p k f", k=kk), src.rearrange("(k p) f -> p k f", p=128), writes=[tst])
                o_, to = ob.next()
                S.op("act", lambda e: e.copy(out=o_[:, 0:2048], in_=st_[:, 0:2048]), reads=[tst], writes=[to])
                S.op("dve", lambda e: e.tensor_copy(out=o_[:, 2048:6144], in_=st_[:, 2048:6144]), reads=[tst], writes=[to])
                S.op("pool", lambda e: e.tensor_copy(out=o_[:, 6144:8192], in_=st_[:, 6144:8192]), reads=[tst], writes=[to])
                to.w = {"act": S.cnt["act"], "dve": S.cnt["dve"], "pool": S.cnt["pool"]}
                S.dma("sp", ob.dkey(), dst.rearrange("(k p) f -> p k f", p=128), o_[:].rearrange("p (k f) -> p k f", k=kk), reads=[to], writes=[C.t_WB], chain=True)


SIGMAX = float(1.0 / (1.0 + np.exp(-1.702 * 7.0)))
QT_ = 1024


def phase_D(C, l, dst):
    nc, S = C.nc, C.S
    S.barrier()
    NE = C.cfg.get("n_experts", N_EXPERTS)
    with ExitStack() as es:
        g2, t_g2 = bcast_row(C, es, "D_g2", C.ln2_g[l:l + 1, :], D)
        b2l, t_b2l = bcast_row(C, es, "D_b2l", C.ln2_b[l:l + 1, :], D)
        L = ln_alloc(C, es, "D_")
        B2 = sb(nc, es, "D_B2", [32, D], BF16)
        t_B2 = Tk()
        dkb = S.dsem()
        S.dma("pool", dkb, B2[:], C.exp_b2[l], writes=[t_B2])
        W1 = Rot(S, [sb(nc, es, f"D_W1_{i}", [128, 8, 2048], BF16) for i in range(2)])
        W2 = [sb(nc, es, f"D_W2_{i}", [128, 8, 1024], BF16) for i in range(2)]
        bt = Rot(S, [sb(nc, es, f"D_bt{i}", [128, 16], F32) for i in range(2)])
        bs = Rot(S, [sb(nc, es, f"D_bs{i}", [128, 16], F32) for i in range(2)])
        XT = sb(nc, es, "D_XT", [128, 8, QT_], BF16)
        acc = sb(nc, es, "D_acc", [128, QT_ // 128, D], F32)
        gT = Rot(S, [sb(nc, es, f"D_gT{i}", [128, 8, 512], BF16) for i in range(2)])
        sg = Rot(S, [sb(nc, es, f"D_sg{i}", [128, 512], F32) for i in range(2)])
        gl = Rot(S, [sb(nc, es, f"D_gl{i}", [128, 512], F32) for i in range(2)])
        ub = Rot(S, [sb(nc, es, f"D_ub{i}", [128, 512], F32) for i in range(2)])
        yb = Rot(S, [sb(nc, es, f"D_yb{i}", [128, 512], F32) for i in range(2)])
        cT = Rot(S, [sb(nc, es, f"D_cT{i}", [32, 128], BF16) for i in range(2)])
        xres = Rot(S, [sb(nc, es, f"D_xr{i}", [128, D], F32) for i in range(1)])
        t_XT, t_acc = Tk(), [Tk() for _ in range(QT_ // 128)]
        dkx = S.dsem()
        for q in range(T // QT_):
            q0 = q * QT_
            S.dma("sp", dkx, XT[:], C.X1T[:, q0:q0 + QT_].rearrange("(k p) t -> p k t", p=128), reads=[C.t_X1T], writes=[t_XT])
            for i in range(QT_ // 128):
                n = q * (QT_ // 128) + i
                pc_, tpc = C.psum.next()
                S.op("pe", lambda e: e.transpose(pc_[0:32, 0:128], C.COMB[:, n, :], C.ident_f[:]), reads=[C.t_comb, C.t_const], writes=[tpc])
                c_, tc = cT.next()
                S.op("act", lambda e: e.copy(out=c_[:], in_=pc_[0:32, 0:128]), reads=[tpc], writes=[tc])
                for hf in range(2):
                    pb_, tpb = C.psum.next()
                    S.op("pe", lambda e: e.matmul(pb_[:], lhsT=c_[:], rhs=B2[:, hf * 512:(hf + 1) * 512], start=True, stop=True), reads=[tc, t_B2], writes=[tpb])
                    S.op("act", lambda e: e.copy(out=acc[:, i, hf * 512:(hf + 1) * 512], in_=pb_[:]), reads=[tpb], writes=[t_acc[i]])
            for ex in range(NE):
                w1, tw1 = W1.next()
                w2 = W2[W1.i]
                dkw = W1.dkey()
                skipw = C.cfg.get("fake_w") and (q > 0 or ex >= 2)
                prew = "W" in C.cfg["phases"]
                for kh in range(0 if skipw else 2):
                    if prew:
                        S.dma("sp", dkw, w1[:, kh * 4:(kh + 1) * 4, :], C.W1B[ex, kh * 512:(kh + 1) * 512, :].rearrange("(k p) f -> p k f", p=128),
                              reads=[C.t_WB], writes=[tw1], chain=(kh > 0))
                    else:
                        S.dma("pool", dkw, w1[:, kh * 4:(kh + 1) * 4, :], C.exp_w1[l, ex, kh * 512:(kh + 1) * 512, :].rearrange("(k p) f -> p k f", p=128),
                              writes=[tw1], chain=(kh > 0))
                for kh in range(0 if skipw else 2):
                    if prew:
                        S.dma("sp", dkw, w2[:, kh * 4:(kh + 1) * 4, :], C.W2B[ex, kh * 512:(kh + 1) * 512, :].rearrange("(k p) d -> p k d", p=128),
                              reads=[C.t_WB], writes=[tw1], chain=True)
                    else:
                        S.dma("pool", dkw, w2[:, kh * 4:(kh + 1) * 4, :], C.exp_w2[l, ex, kh * 512:(kh + 1) * 512, :].rearrange("(k p) d -> p k d", p=128),
                              writes=[tw1], chain=True)
                bt_, tbt = bt.next()
                S.dma("sp", bt.dkey(), bt_[:], C.exp_b1p[l, ex], writes=[tbt])
                bs_, tbs = bs.next()
                S.op("dve", lambda e: e.tensor_scalar(out=bs_[:, 0:8], in0=bt_[:, 0:8], scalar1=1.702, scalar2=None, op0=ALU.mult), reads=[tbt], writes=[tbs])
                S.op("dve", lambda e: e.tensor_scalar(out=bs_[:, 8:16], in0=bt_[:, 8:16], scalar1=1.0, scalar2=None, op0=ALU.add), reads=[tbt, tbs], writes=[tbs])
                dsub = C.cfg.get("dsub", 9)
                for g in range(QT_ // 512 if dsub >= 2 else 0):
                    tsl = slice(g * 512, (g + 1) * 512)
                    g_, tg = gT.next()
                    for j in range(8):
                        pG, tpG = C.psum.next()
                        for k in range(8):
                            S.op("pe", lambda e: e.matmul(pG[:], lhsT=w1[:, k, j * 128:(j + 1) * 128], rhs=XT[:, k, tsl], start=(k == 0), stop=(k == 7)),
                                 reads=[tw1, t_XT], writes=[tpG])
                        pU, tpU = C.psum.next()
                        for k in range(8):
                            S.op("pe", lambda e: e.matmul(pU[:], lhsT=w1[:, k, 1024 + j * 128:1024 + (j + 1) * 128], rhs=XT[:, k, tsl], start=(k == 0), stop=(k == 7)),
                                 reads=[tw1, t_XT], writes=[tpU])
                        z_, tz = gl.next()
                        u_, tu = ub.next()
                        s_, ts = sg.next()
                        S.op("act", lambda e: e.activation(out=z_[:], in_=pG[:], func=AF.Identity, bias=bt_[:, j:j + 1]), reads=[tpG, tbt], writes=[tz])
                        S.op("act", lambda e: e.activation(out=u_[:], in_=pU[:], func=AF.Identity, bias=bs_[:, 8 + j:9 + j]), reads=[tpU, tbs], writes=[tu])
                        S.op("dve", lambda e: e.tensor_scalar(out=z_[:], in0=z_[:], scalar1=7.0, scalar2=None, op0=ALU.min), reads=[tz], writes=[tz])
                        S.op("act", lambda e: e.activation(out=s_[:], in_=z_[:], func=AF.Sigmoid, scale=1.702), reads=[tz], writes=[ts])
                        S.op("dve", lambda e: e.tensor_scalar(out=u_[:], in0=u_[:], scalar1=8.0, scalar2=-6.0, op0=ALU.min, op1=ALU.max), reads=[tu], writes=[tu])
                        S.op("dve", lambda e: e.tensor_tensor(out=z_[:], in0=z_[:], in1=s_[:], op=ALU.mult), reads=[tz, ts], writes=[tz])
                        S.op("dve", lambda e: e.tensor_tensor(out=g_[:, j, :], in0=u_[:], in1=z_[:], op=ALU.mult), reads=[tu, tz], writes=[tg])
                    for tt in range(4 if dsub >= 3 else 0):
                        i = g * 4 + tt
                        n = q * (QT_ // 128) + i
                        for hf in range(2):
                            pY, tpY = C.psum.next()
                            for j in range(8):
                                S.op("pe", lambda e: e.matmul(pY[:], lhsT=g_[:, j, tt * 128:(tt + 1) * 128], rhs=w2[:, j, hf * 512:(hf + 1) * 512], start=(j == 0), stop=(j == 7)),
                                     reads=[tg, tw1], writes=[tpY])
                            y_, tyb = yb.next()
                            S.op("act", lambda e: e.activation(out=y_[:], in_=pY[:], func=AF.Copy, scale=C.COMB[:, n, ex:ex + 1]), reads=[tpY, C.t_comb], writes=[tyb])
                            S.op("pool", lambda e: e.tensor_tensor(out=acc[:, i, hf * 512:(hf + 1) * 512], in0=acc[:, i, hf * 512:(hf + 1) * 512], in1=y_[:], op=ALU.add),
                                 reads=[tyb, t_acc[i]], writes=[t_acc[i]])
            for i in range(QT_ // 128):
                n = q * (QT_ // 128) + i
                x_, tx = xres.next()
                S.dma("sp", xres.dkey(), x_[:], C.X1[n * 128:(n + 1) * 128, :], reads=[C.t_X1], writes=[tx])
                y = acc[:, i, :]
                S.op("dve", lambda e: e.scalar_tensor_tensor(out=y, in0=x_[:], scalar=ALPHA, in1=y, op0=ALU.mult, op1=ALU.add), reads=[tx, t_acc[i]], writes=[t_acc[i]])
                ln_tile(C, L, y, t_acc[i], g2, t_g2, b2l, t_b2l)
                S.dma("sp", dkx, dst[n * 128:(n + 1) * 128, :], y, reads=[t_acc[i]], writes=[C.t_X2], chain=True)


SMALL_INPUTS = ("ret_gn_g", "ret_gn_b", "w_out", "router_w", "router_b", "ln1_g", "ln1_b", "exp_w1", "exp_w2", "exp_b2", "ln2_g", "ln2_b")


WEIGHT_KEYS = ("w_in", "conv_w", "conv_b", "rg_bx", "rg_ba", "rg_lambda", "rg_wx", "rg_wa", "exp_b1")
_CACHE = {}


def _shared_inputs(inp):
    shared = dict(host_consts())
    shared.update(prep_weights({k: inp[k] for k in WEIGHT_KEYS}))
    for k in SMALL_INPUTS:
        shared[k] = np.ascontiguousarray(inp[k], dtype=np.float32)
    return shared


def _run(inputs, n_cores=8, trace=False, first=0):
    inp = {k: np.asarray(v) for k, v in inputs.items()}
    if "nc" not in _CACHE:
        _CACHE["nc"] = build(dict(phases="AMSRGCWD", layers=DEPTH))
    nc = _CACHE["nc"]
    shared = _shared_inputs(inp)
    x = np.ascontiguousarray(inp["x"], dtype=np.float32)
    in_maps = []
    for c in range(n_cores):
        m = dict(shared)
        m["x"] = np.ascontiguousarray(x[first + c])
        in_maps.append(m)
    res = run_bass_kernel_spmd(nc, in_maps, core_ids=list(range(n_cores)), trace=trace)
    out = np.stack([np.asarray(r["out"], dtype=np.float32) for r in res.results], axis=0)
    return out, res


LAYER_KEYS = ("w_fmr", "w_fms", "w_fmp", "w_dw", "w_tm", "rg_pc", "rg_wbd", "exp_b1p") + SMALL_INPUTS


def _run_layers(inputs, n_cores=8, trace=False):
    inp = {k: np.asarray(v) for k, v in inputs.items()}
    if "nc1" not in _CACHE:
        _CACHE["nc1"] = build(dict(phases="AMSRGCD", layers=1, decl_depth=1))
    nc = _CACHE["nc1"]
    shared = _shared_inputs(inp)
    x = np.ascontiguousarray(inp["x"], dtype=np.float32)
    results = []
    for l in range(DEPTH):
        sh = dict(shared)
        for k in LAYER_KEYS:
            sh[k] = np.ascontiguousarray(shared[k][l:l + 1])
        in_maps = []
        for c in range(n_cores):
            m = dict(sh)
            m["x"] = np.ascontiguousarray(x[c])
            in_maps.append(m)
        res = run_bass_kernel_spmd(nc, in_maps, core_ids=list(range(n_cores)), trace=trace)
        results.append(res)
        x = np.stack([np.asarray(r["out"], dtype=np.float32) for r in res.results], axis=0)
    return x, results


def kernel(**inputs):
    out, _ = _run(inputs, 8)
    return out
```

```python
import numpy as np
from contextlib import ExitStack
import concourse.bass as bass
import concourse.mybir as mybir
from concourse.bass_utils import run_bass_kernel_spmd

F32 = mybir.dt.float32
BF16 = mybir.dt.bfloat16
AF = mybir.ActivationFunctionType
ALU = mybir.AluOpType
AX = mybir.AxisListType

D = 1024
T = 4096
DEPTH = 2
NT = T // 128
NG = T // 512
NEG = -30000.0
ALPHA = (2 * DEPTH) ** 0.25
LN_EPS = 1e-5
N_EXPERTS = 32

_off = {}
_o = 0
for _n, _s in (("a_q", 256), ("a_k", 256), ("a_v", 256), ("r_q", 256), ("r_k", 256), ("r_v", 256),
               ("r_g", 256), ("c_x", 256), ("c_g", 256), ("d_q", 256), ("d_k", 256), ("d_v", 256),
               ("d_qi", 512), ("d_ki", 64), ("d_w", 8)):
    _off[_n] = (_o, _s)
    _o += _s
IN_WIDTH = _o


def _cols(name):
    o, s = _off[name]
    return np.arange(o, o + s)


def _swap(c):
    c = c.reshape(-1, 64)
    return np.concatenate([c[:, 32:], c[:, :32]], axis=1).reshape(-1)


ROPE_NAMES = ("a_q", "a_k", "r_q", "r_k", "d_q", "d_k", "d_qi", "d_ki")
FM_ROPE_COLS = np.concatenate([_cols(n) for n in ROPE_NAMES])
FM_ROPE_SW = np.concatenate([_swap(_cols(n)) for n in ROPE_NAMES])
FM_PLAIN_COLS = np.concatenate([_cols("c_x"), _cols("c_g")])
DW_COLS = _cols("d_w")
TM_COLS = np.concatenate([_cols("a_v"), _cols("r_v"), _cols("d_v"), _cols("r_g"), _cols("r_k"), _cols("d_w")])
NTM = TM_COLS.shape[0]
NFMC = 21
CH = dict(a_q=0, a_k=2, r_q=4, r_k=6, d_q=8, d_k=10, d_qi=12, d_ki=16, c_x=17, c_g=19)
TMO = dict(a_v=0, r_v=256, d_v=512, r_g=768, rkz=1024)
NTMO = 1280


class Tk:
    __slots__ = ("w", "r", "name", "excl")

    def __init__(self, name="", excl=False):
        self.w = {}
        self.r = {}
        self.name = name
        self.excl = excl


class Sched:
    ENG = ("pe", "act", "dve", "pool", "sp")

    def __init__(self, nc, es):
        self.nc = nc
        self.es = es
        self.eng = {"pe": nc.tensor, "act": nc.scalar, "dve": nc.vector,
                    "pool": nc.gpsimd, "sp": nc.sync}
        self.sem = {}
        self.cnt = {}
        for k in self.ENG:
            self.sem[k] = es.enter_context(nc.semaphore("s_" + k))
            self.cnt[k] = 0
        self.seen = {k: {} for k in self.ENG}
        self.ndsem = 0
        self.nwaits = 0
        self.free_dsems = []

    def dsem(self):
        if self.free_dsems:
            return self.free_dsems.pop()
        self.ndsem += 1
        key = "d%d" % self.ndsem
        self.sem[key] = self.es.enter_context(self.nc.semaphore(key))
        self.cnt[key] = 0
        return key

    def release_dsems(self, keys):
        self.free_dsems.extend(keys)

    def _wait(self, e, deps):
        seen = self.seen[e]
        for key, val in deps.items():
            if key == "pe" and e == "pe":
                continue
            if seen.get(key, 0) >= val:
                continue
            self.eng[e].wait_ge(self.sem[key], val)
            self.nwaits += 1
            seen[key] = val

    @staticmethod
    def _merge(d, s):
        for k, v in s.items():
            if d.get(k, 0) < v:
                d[k] = v

    def _deps(self, reads, writes):
        deps = {}
        for t in reads:
            self._merge(deps, t.w)
            if t.excl:
                self._merge(deps, t.r)
        for t in writes:
            self._merge(deps, t.w)
            self._merge(deps, t.r)
        return deps

    def op(self, e, fn, reads=(), writes=()):
        self._wait(e, self._deps(reads, writes))
        ins = fn(self.eng[e])
        self.cnt[e] += 1
        ins.then_inc(self.sem[e], 1)
        me = {e: self.cnt[e]}
        for t in reads:
            self._merge(t.r, me)
        for t in writes:
            t.w = dict(me)
            t.r = {}
        return ins

    def dma(self, q, dkey, out, in_, reads=(), writes=(), chain=False, **kw):
        deps = self._deps(reads, writes)
        if not chain and self.cnt[dkey] > 0:
            self._merge(deps, {dkey: self.cnt[dkey]})
        self._wait(q, deps)
        ins = self.eng[q].dma_start(out=out, in_=in_, **kw)
        self.cnt[dkey] += 16
        ins.then_inc(self.sem[dkey], 16)
        me = {dkey: self.cnt[dkey]}
        for t in reads:
            self._merge(t.r, me)
        for t in writes:
            if chain:
                self._merge(t.w, me)
            else:
                t.w = dict(me)
            t.r = {}
        return ins

    def wait_all(self, e, toks):
        deps = {}
        for t in toks:
            self._merge(deps, t.w)
            self._merge(deps, t.r)
        self._wait(e, deps)

    def barrier(self):
        deps = {k: v for k, v in self.cnt.items() if v > 0}
        for e in self.ENG:
            self._wait(e, deps)
        self.free_dsems = [k for k in self.cnt if k.startswith("d") and k[1:].isdigit()]


class Rot:
    def __init__(self, S, aps):
        self.S = S
        self.aps = list(aps)
        self.tk = [Tk() for _ in self.aps]
        self.dk = [None] * len(self.aps)
        self.i = -1

    def next(self):
        self.i = (self.i + 1) % len(self.aps)
        return self.aps[self.i], self.tk[self.i]

    def dkey(self):
        if self.dk[self.i] is None:
            self.dk[self.i] = self.S.dsem()
        return self.dk[self.i]


_SBN = [0]


def sb(nc, es, name, shape, dt):
    _SBN[0] += 1
    return es.enter_context(nc.sbuf_tensor("%s_%d" % (name, _SBN[0]), list(shape), dt))


class Ctx:
    pass


def bcast_row(C, es, name, src_row, N):
    nc, S = C.nc, C.S
    W = min(N, 512)
    row = sb(nc, es, name + "_row", [1, W], F32)
    out = sb(nc, es, name, [128, N], F32)
    t_row, t_out = Tk(), Tk()
    dk = S.dsem()
    for c0 in range(0, N, 512):
        w = min(512, N - c0)
        S.dma("sp", dk, row[0:1, 0:w], src_row[:, c0:c0 + w], writes=[t_row])
        ps, tp = C.psum.next()
        S.op("pe", lambda e: e.matmul(ps[:, 0:w], lhsT=C.ones_row[0:1, :], rhs=row[0:1, 0:w], start=True, stop=True),
             reads=[t_row, C.t_ones], writes=[tp])
        S.op("act", lambda e: e.copy(out=out[:, c0:c0 + w], in_=ps[:, 0:w]), reads=[tp], writes=[t_out])
    return out, t_out

def phase_A(C, l, x_src):
    nc, S = C.nc, C.S
    S.barrier()
    with ExitStack() as es:
        xT = sb(nc, es, "A_xT", [128, 8, T], BF16)
        t_xT = Tk()
        cosT = sb(nc, es, "A_cosT", [128, T], F32)
        sinT = sb(nc, es, "A_sinT", [128, T], F32)
        t_tab = Tk()
        dk_tab = S.dsem()
        S.dma("sp", dk_tab, cosT[:], C.cosT[:, :], writes=[t_tab])
        S.dma("sp", dk_tab, sinT[:], C.sinT[:, :], writes=[t_tab], chain=True)
        cosTM = sb(nc, es, "A_cosTM", [128, NT, 32], F32)
        sinTM = sb(nc, es, "A_sinTM", [128, NT, 32], F32)
        S.dma("sp", dk_tab, cosTM[:], C.cosTM.rearrange("(n p) c -> p n c", p=128), writes=[t_tab], chain=True)
        S.dma("sp", dk_tab, sinTM[:], C.sinTM.rearrange("(n p) c -> p n c", p=128), writes=[t_tab], chain=True)
        zt = sb(nc, es, "A_zeta", [128, 4], F32)
        S.dma("sp", dk_tab, zt[:], C.zeta8[:, :], writes=[t_tab], chain=True)
        sel2 = sb(nc, es, "A_sel2", [8, 512], BF16)
        t_sel = Tk()
        dk_sel = S.dsem()
        S.dma("pool", dk_sel, sel2[:], C.sel2[:, :], writes=[t_sel])

        xin = Rot(S, [sb(nc, es, f"A_xin{i}", [128, D], F32) for i in range(2)])
        for n in range(NT):
            xt_, tx = xin.next()
            S.dma("sp", xin.dkey(), xt_[:], x_src[n * 128:(n + 1) * 128, :], writes=[tx])
            for half in range(2):
                ps, tp = C.psum.next()
                for j in range(4):
                    k = half * 4 + j
                    S.op("pe", lambda e: e.transpose(ps[:, j * 128:(j + 1) * 128], xt_[:, k * 128:(k + 1) * 128], C.ident_f[:]),
                         reads=[tx, C.t_const], writes=[tp])
                dst = xT[:, half * 4:(half + 1) * 4, n * 128:(n + 1) * 128]
                src = ps[:].rearrange("p (k c) -> p k c", k=4)
                if half == 0:
                    S.op("act", lambda e: e.copy(out=dst, in_=src), reads=[tp], writes=[t_xT])
                else:
                    S.op("dve", lambda e: e.tensor_copy(out=dst, in_=src), reads=[tp], writes=[t_xT])

        wtm = sb(nc, es, "A_wtm", [128, 8, NTM], BF16)
        t_wtm = Tk()
        dk_wtm = S.dsem()
        S.dma("pool", dk_wtm, wtm[:], C.w_tm[l].rearrange("(k p) c -> p k c", p=128), writes=[t_wtm])
        tmo = Rot(S, [sb(nc, es, f"A_tmo{i}", [128, NTMO], BF16) for i in range(2)])
        rk32 = Rot(S, [sb(nc, es, f"A_rk{i}", [128, 4, 64], F32) for i in range(2)])
        rkt = Rot(S, [sb(nc, es, f"A_rkt{i}", [128, 4, 64], F32) for i in range(2)])
        rku = Rot(S, [sb(nc, es, f"A_rku{i}", [128, 4, 64], F32) for i in range(2)])
        for n in range(NT):
            ot, to = tmo.next()
            banks = []
            for (c0, cn) in ((0, 512), (512, 512), (1024, NTM - 1024)):
                ps, tp = C.psum.next()
                for k in range(8):
                    S.op("pe", lambda e: e.matmul(ps[:, 0:cn], lhsT=xT[:, k, n * 128:(n + 1) * 128], rhs=wtm[:, k, c0:c0 + cn],
                                                  start=(k == 0), stop=(k == 7)),
                         reads=[t_xT, t_wtm], writes=[tp])
                banks.append((ps, tp))
            S.op("act", lambda e: e.copy(out=ot[:, 0:512], in_=banks[0][0][:, 0:512]), reads=[banks[0][1]], writes=[to])
            S.op("act", lambda e: e.copy(out=ot[:, 512:1024], in_=banks[1][0][:, 0:512]), reads=[banks[1][1]], writes=[to])
            ps2, tp2 = banks[2]
            S.op("act", lambda e: e.activation(out=C.sgn[:, n, :], in_=ps2[:, 256:264], func=AF.Sign), reads=[tp2], writes=[C.t_sgn])
            r32, tr = rk32.next()
            S.op("dve", lambda e: e.tensor_copy(out=r32[:], in_=ps2[:, 0:256].rearrange("p (h d) -> p h d", h=4)), reads=[tp2], writes=[tr])
            cb = cosTM[:, n, :].unsqueeze(1).to_broadcast([128, 4, 32])
            sbb = sinTM[:, n, :].unsqueeze(1).to_broadcast([128, 4, 32])
            ra, tra = rkt.next()
            rb, trb = rku.next()
            x1 = r32[:, :, 0:32]
            x2 = r32[:, :, 32:64]
            S.op("dve", lambda e: e.tensor_tensor(out=ra[:, :, 0:32], in0=x1, in1=cb, op=ALU.mult), reads=[tr, t_tab], writes=[tra])
            S.op("dve", lambda e: e.tensor_tensor(out=ra[:, :, 32:64], in0=x2, in1=cb, op=ALU.mult), reads=[tr, t_tab], writes=[tra])
            S.op("dve", lambda e: e.tensor_tensor(out=rb[:, :, 0:32], in0=x2, in1=sbb, op=ALU.mult), reads=[tr, t_tab], writes=[trb])
            S.op("dve", lambda e: e.tensor_tensor(out=rb[:, :, 32:64], in0=x1, in1=sbb, op=ALU.mult), reads=[tr, t_tab], writes=[trb])
            S.op("dve", lambda e: e.tensor_tensor(out=ra[:, :, 0:32], in0=ra[:, :, 0:32], in1=rb[:, :, 0:32], op=ALU.subtract), reads=[tra, trb], writes=[tra])
            S.op("dve", lambda e: e.tensor_tensor(out=ra[:, :, 32:64], in0=ra[:, :, 32:64], in1=rb[:, :, 32:64], op=ALU.add), reads=[tra, trb], writes=[tra])
            S.op("dve", lambda e: e.tensor_tensor(out=ot[:, 1024:1280].rearrange("p (h d) -> p h d", h=4), in0=ra[:],
                                                  in1=zt[:].unsqueeze(2).to_broadcast([128, 4, 64]), op=ALU.mult),
                 reads=[tra, t_tab], writes=[to])
            S.dma("sp", tmo.dkey(), C.TMO[n * 128:(n + 1) * 128, :], ot[:], reads=[to], writes=[C.t_TMO], chain=True)

        absw = sb(nc, es, "A_absw", [8, T], BF16)
        t_absw = Tk()
        wdw = sb(nc, es, "A_wdw", [128, 8, 8], BF16)
        t_wdw = Tk()
        dk_wdw = S.dsem()
        S.dma("pool", dk_wdw, wdw[:], C.w_dw[l].rearrange("(k p) c -> p k c", p=128), writes=[t_wdw])
        for g in range(NG):
            ps, tp = C.psum.next()
            for k in range(8):
                S.op("pe", lambda e: e.matmul(ps[0:8, :], lhsT=wdw[:, k, :], rhs=xT[:, k, g * 512:(g + 1) * 512],
                                              start=(k == 0), stop=(k == 7)), reads=[t_xT, t_wdw], writes=[tp])
            S.op("act", lambda e: e.activation(out=absw[:, g * 512:(g + 1) * 512], in_=ps[0:8, :], func=AF.Abs), reads=[tp], writes=[t_absw])

        wb = Rot(S, [sb(nc, es, f"A_wb{i}", [128, 8, 512], BF16) for i in range(2)])
        ws = Rot(S, [sb(nc, es, f"A_ws{i}", [128, 8, 512], BF16) for i in range(2)])
        t1r = Rot(S, [sb(nc, es, f"A_t1{i}", [128, 512], F32) for i in range(2)])
        t2r = Rot(S, [sb(nc, es, f"A_t2{i}", [128, 512], F32) for i in range(2)])
        outr = Rot(S, [sb(nc, es, f"A_out{i}", [128, 512], BF16) for i in range(4)])
        groups = [(0, 4, True, 0), (4, 4, True, 512), (8, 4, True, 1024), (12, 4, True, 1536),
                  (16, 1, True, 2048), (17, 4, False, 0)]
        for (c0, ncn, rope, wc0) in groups:
            ncols = 64 if c0 == 16 else ncn * 128
            w_, tw = wb.next()
            src = (C.w_fmr if rope else C.w_fmp)[l]
            S.dma("pool", wb.dkey(), w_[:, :, 0:ncols], src[:, wc0:wc0 + ncols].rearrange("(k p) c -> p k c", p=128), writes=[tw])
            if rope:
                wsw, tws = ws.next()
                S.dma("pool", ws.dkey(), wsw[:, :, 0:ncols], C.w_fms[l][:, wc0:wc0 + ncols].rearrange("(k p) c -> p k c", p=128), writes=[tws])
            for g in range(NG):
                tsl = slice(g * 512, (g + 1) * 512)
                for ci in range(ncn):
                    c = c0 + ci
                    M = 64 if c == 16 else 128
                    wsl = slice(ci * 128, ci * 128 + M)
                    pX, tpX = C.psum.next()
                    for k in range(8):
                        S.op("pe", lambda e: e.matmul(pX[0:M, :], lhsT=w_[:, k, wsl], rhs=xT[:, k, tsl], start=(k == 0), stop=(k == 7)),
                             reads=[t_xT, tw], writes=[tpX])
                    o_, to_ = outr.next()
                    if not rope:
                        S.op("act", lambda e: e.copy(out=o_[0:M, :], in_=pX[0:M, :]), reads=[tpX], writes=[to_])
                    else:
                        pS, tpS = C.psum.next()
                        for k in range(8):
                            S.op("pe", lambda e: e.matmul(pS[0:M, :], lhsT=wsw[:, k, wsl], rhs=xT[:, k, tsl], start=(k == 0), stop=(k == 7)),
                                 reads=[t_xT, tws], writes=[tpS])
                        a1, ta1 = t1r.next()
                        a2, ta2 = t2r.next()
                        S.op("dve", lambda e: e.tensor_tensor(out=a1[0:M, :], in0=pX[0:M, :], in1=cosT[0:M, tsl], op=ALU.mult),
                             reads=[tpX, t_tab], writes=[ta1])
                        S.op("dve", lambda e: e.tensor_tensor(out=a2[0:M, :], in0=pS[0:M, :], in1=sinT[0:M, tsl], op=ALU.mult),
                             reads=[tpS, t_tab], writes=[ta2])
                        if 12 <= c < 16:
                            S.op("dve", lambda e: e.tensor_tensor(out=a1[:], in0=a1[:], in1=a2[:], op=ALU.add), reads=[ta1, ta2], writes=[ta1])
                            pB, tpB = C.psum.next()
                            S.op("pe", lambda e: e.matmul(pB[:], lhsT=sel2[:, (c - 12) * 128:(c - 11) * 128], rhs=absw[:, tsl], start=True, stop=True),
                                 reads=[t_sel, t_absw], writes=[tpB])
                            S.op("dve", lambda e: e.tensor_tensor(out=o_[:], in0=a1[:], in1=pB[:], op=ALU.mult), reads=[ta1, tpB], writes=[to_])
                        else:
                            S.op("dve", lambda e: e.tensor_tensor(out=o_[0:M, :], in0=a1[0:M, :], in1=a2[0:M, :], op=ALU.add),
                                 reads=[ta1, ta2], writes=[to_])
                    S.dma("sp", outr.dkey(), C.FMT[c * 128:c * 128 + M, tsl], o_[0:M, :], reads=[to_], writes=[C.t_FMT], chain=True)


def host_consts():
    inv = (10000.0 ** (-np.arange(0, 64, 2, dtype=np.float32) / 64)).astype(np.float32)
    ang = np.arange(T, dtype=np.float32)[:, None] * inv[None, :]
    cos = np.cos(ang).astype(np.float32)
    sin = np.sin(ang).astype(np.float32)
    p = np.arange(128)
    d = p % 64
    cosT = np.ascontiguousarray(cos[:, d % 32].T)
    sgn = np.where(d < 32, -1.0, 1.0).astype(np.float32)
    sinT = np.ascontiguousarray((sin[:, d % 32] * sgn[None, :]).T)
    log_g = np.log(1.0 - 2.0 ** (-5.0 - np.arange(4, dtype=np.float32))).astype(np.float32)
    n = np.arange(128, dtype=np.float32)
    zeta8 = (np.exp(log_g[None, :] * (127.0 - n[:, None])) * 0.125).astype(np.float32)
    sel2 = np.zeros((8, 512), np.float32)
    for c in range(4):
        sel2[2 * c, c * 128:c * 128 + 64] = 1.0
        sel2[2 * c + 1, c * 128 + 64:c * 128 + 128] = 1.0
    d_mask = np.where(n[:, None] - n[None, :] >= 0, np.exp(log_g[:, None, None] * np.maximum(n[:, None] - n[None, :], 0.0)), 0.0)
    dmaskT = np.ascontiguousarray((d_mask * 0.125).transpose(2, 0, 1)).astype(np.float32)
    xi = np.exp(log_g[:, None] * (n[None, :] + 1.0))
    hp = np.arange(128) // 64
    xiT = np.stack([xi[2 * c + hp] for c in range(2)], axis=1).astype(np.float32)
    gch = np.exp(log_g * 128.0)
    gvec = np.stack([gch[2 * c + hp] for c in range(2)], axis=1).astype(np.float32)
    q = np.arange(128)
    caus = np.where(q[None, :] <= q[:, None], 0.0, NEG).astype(np.float32)
    causF = np.where(q[None, :] <= q[:, None], 0.0, -1e30).astype(np.float32)
    return dict(cosT=cosT, sinT=sinT, cosTM=cos, sinTM=sin, zeta8=zeta8, sel2=sel2,
                ident=np.eye(128, dtype=np.float32), caus=caus, causF=causF,
                dmaskT=dmaskT, xiT=xiT, gvec=gvec)


def prep_weights(inp):
    w_in = np.asarray(inp["w_in"], dtype=np.float32)
    out = {}
    out["w_fmr"] = np.ascontiguousarray(w_in[:, :, FM_ROPE_COLS])
    out["w_fms"] = np.ascontiguousarray(w_in[:, :, FM_ROPE_SW])
    out["w_fmp"] = np.ascontiguousarray(w_in[:, :, FM_PLAIN_COLS])
    out["w_dw"] = np.ascontiguousarray(w_in[:, :, DW_COLS])
    out["w_tm"] = np.ascontiguousarray(w_in[:, :, TM_COLS])
    if "conv_w" in inp:
        L = w_in.shape[0]
        pc = np.zeros((L, 128, 16), np.float32)
        cwt = np.asarray(inp["conv_w"], np.float32)
        for c in range(2):
            for i in range(4):
                pc[:, :, c * 4 + i] = cwt[:, i, c * 128:(c + 1) * 128]
            pc[:, :, 8 + c] = np.asarray(inp["conv_b"])[:, c * 128:(c + 1) * 128]
            pc[:, :, 10 + c] = np.asarray(inp["rg_bx"])[:, c * 128:(c + 1) * 128]
            pc[:, :, 12 + c] = np.asarray(inp["rg_ba"])[:, c * 128:(c + 1) * 128]
            pc[:, :, 14 + c] = np.asarray(inp["rg_lambda"])[:, c * 128:(c + 1) * 128]
        out["rg_pc"] = pc
        wbd = np.zeros((L, 128, 4, 128), np.float32)
        for k, nm in enumerate(("rg_wx", "rg_wa")):
            w = np.asarray(inp[nm], np.float32)
            for c in range(2):
                for b in range(2):
                    wbd[:, b * 64:(b + 1) * 64, k * 2 + c, b * 64:(b + 1) * 64] = w[:, 2 * c + b]
        out["rg_wbd"] = wbd
    if "exp_b1" in inp:
        b1 = np.asarray(inp["exp_b1"], np.float32)
        out["exp_b1p"] = np.ascontiguousarray(b1.reshape(b1.shape[0], b1.shape[1], 16, 128).transpose(0, 1, 3, 2))
    return out


def build(cfg):
    nc = bass.Bass("TRN2", target_bir_lowering=False)
    C = Ctx()
    C.nc = nc
    C.cfg = cfg
    NLD = cfg.get("decl_depth", DEPTH)
    NLAY = cfg.get("layers", DEPTH)

    def din(name, shape, dt=F32):
        return nc.dram_tensor(name, list(shape), dt, kind="ExternalInput").ap()

    def dscr(name, shape, dt):
        kind = "ExternalOutput" if name in cfg.get("debug_out", ()) else ("ExternalInput" if name in cfg.get("debug_in", ()) else "Internal")
        return nc.dram_tensor(name, list(shape), dt, kind=kind).ap()

    x_in = din("x", [T, D])
    C.cosT = din("cosT", [128, T]); C.sinT = din("sinT", [128, T])
    C.cosTM = din("cosTM", [T, 32]); C.sinTM = din("sinTM", [T, 32])
    C.zeta8 = din("zeta8", [128, 4]); C.sel2 = din("sel2", [8, 512])
    ident_d = din("ident", [128, 128])
    C.w_fmr = din("w_fmr", [NLD, D, 2112]); C.w_fms = din("w_fms", [NLD, D, 2112])
    C.w_fmp = din("w_fmp", [NLD, D, 512]); C.w_dw = din("w_dw", [NLD, D, 8])
    C.w_tm = din("w_tm", [NLD, D, NTM])
    caus_d = din("caus", [128, 128]); causF_d = din("causF", [128, 128])
    C.dmaskT = din("dmaskT", [128, 4, 128]); C.xiT = din("xiT", [128, 2, 128]); C.gvec = din("gvec", [128, 2])
    C.ret_gn_g = din("ret_gn_g", [NLD, 256]); C.ret_gn_b = din("ret_gn_b", [NLD, 256])
    C.rg_pc = din("rg_pc", [NLD, 128, 16]); C.rg_wbd = din("rg_wbd", [NLD, 128, 4, 128])
    C.w_out = din("w_out", [NLD, D, D]); C.router_w = din("router_w", [NLD, D, 32]); C.router_b = din("router_b", [NLD, 32])
    C.ln1_g = din("ln1_g", [NLD, D]); C.ln1_b = din("ln1_b", [NLD, D])
    C.X1 = dscr("X1", [T, D], F32); C.X1T = dscr("X1T", [D, T], BF16)
    C.X2 = dscr("X2", [T, D], F32); C.t_X2 = Tk()
    C.W1B = dscr("W1B", [N_EXPERTS, D, 2 * D], BF16); C.W2B = dscr("W2B", [N_EXPERTS, D, D], BF16); C.t_WB = Tk()
    C.OUT = nc.dram_tensor("out", [T, D], F32, kind="ExternalOutput").ap()
    C.exp_w1 = din("exp_w1", [NLD, N_EXPERTS, D, 2 * D]); C.exp_w2 = din("exp_w2", [NLD, N_EXPERTS, D, D])
    C.exp_b1p = din("exp_b1p", [NLD, N_EXPERTS, 128, 16]); C.exp_b2 = din("exp_b2", [NLD, N_EXPERTS, D])
    C.ln2_g = din("ln2_g", [NLD, D]); C.ln2_b = din("ln2_b", [NLD, D])
    C.t_X1 = Tk(); C.t_X1T = Tk(); C.t_xsrc = Tk(); C.t_comb = Tk()
    if "COMBD" in cfg.get("debug_out", ()):
        C.COMBD = nc.dram_tensor("COMBD", [T, 32], F32, kind="ExternalOutput").ap()
    C.MIXT = dscr("MIXT", [1024, T], BF16)
    if cfg.get("dbg_dsa") is not None:
        C.DBG1 = nc.dram_tensor("DBG1", [128, T], F32, kind="ExternalOutput").ap()
        C.DBG2 = nc.dram_tensor("DBG2", [128, NT * 8], F32, kind="ExternalOutput").ap()
        C.DBG3 = nc.dram_tensor("DBG3", [128, T], BF16, kind="ExternalOutput").ap()
        C.t_dbg = Tk()
    C.t_MIXT = Tk()
    C.FMT = dscr("FMT", [NFMC * 128, T], BF16)
    C.TMO = dscr("TMO", [T, NTMO], BF16)
    C.t_FMT = Tk(); C.t_TMO = Tk()

    with ExitStack() as es:
        S = Sched(nc, es)
        C.S = S
        banks = [es.enter_context(nc.psum_tensor(f"ps{i}", [128, 512], F32)) for i in range(8)]
        C.psum = Rot(S, banks[0:6])
        C.acc = Rot(S, banks[6:8])
        for t in C.psum.tk + C.acc.tk:
            t.excl = True
        C.ident_f = sb(nc, es, "ident_f", [128, 128], F32)
        C.ident_b = sb(nc, es, "ident_b", [128, 128], BF16)
        C.t_const = Tk()
        dk = S.dsem()
        S.dma("sp", dk, C.ident_f[:], ident_d[:, :], writes=[C.t_const])
        dk2 = S.dsem()
        S.dma("pool", dk2, C.ident_b[:], ident_d[:, :], writes=[C.t_const], chain=True)
        C.caus_b = sb(nc, es, "caus_b", [128, 128], BF16)
        C.caus_f = sb(nc, es, "caus_f", [128, 128], F32)
        C.ones_f = sb(nc, es, "ones_f", [128, 64], F32)
        S.dma("pool", dk2, C.caus_b[:], caus_d[:, :], writes=[C.t_const], chain=True)
        S.dma("sp", dk, C.caus_f[:], causF_d[:, :], writes=[C.t_const], chain=True)
        C.t_ones = Tk()
        S.op("pool", lambda e: e.memset(C.ones_f[:], 1.0), writes=[C.t_ones])
        C.ones_row = sb(nc, es, "ones_row", [1, 128], F32)
        S.op("pool", lambda e: e.memset(C.ones_row[:], 1.0), writes=[C.t_ones])
        C.sgn = sb(nc, es, "sgn", [128, NT, 8], F32)
        C.COMB = sb(nc, es, "COMB", [128, NT, 32], F32)
        C.t_sgn = Tk()

        for l in range(cfg.get("layers", DEPTH)):
            if "A" in cfg["phases"]:
                phase_A(C, l, x_in if l == 0 else C.X2)
            if "M" in cfg["phases"]:
                phase_moba(C)
            if "S" in cfg["phases"]:
                phase_dsa(C)
            if "R" in cfg["phases"]:
                phase_ret(C, l)
            if "G" in cfg["phases"]:
                phase_rglru(C, l)
            x_src = x_in if l == 0 else C.X2
            if "C" in cfg["phases"]:
                phase_C(C, l, x_src)
            if "W" in cfg["phases"]:
                phase_W(C, l)
            if "D" in cfg["phases"]:
                phase_D(C, l, C.X2 if l < NLAY - 1 else C.OUT)
        S.barrier()
        C.stats = ({k: S.cnt[k] for k in S.ENG}, S.nwaits, S.ndsem)
    return nc


def attn_group(C, A, g, heads, bias_aps, bias_tks, out_row0):
    nc, S = C.nc, C.S
    tsl = slice(g * 512, (g + 1) * 512)
    nkt = 4 * g + 4
    for h in heads:
        c, pb = h // 2, (h % 2) * 64
        oT, toT = C.acc.next()
        for kt in range(nkt):
            ps, tp = C.psum.next()
            S.op("pe", lambda e: e.matmul(ps[:], lhsT=A.KT[pb:pb + 64, c, kt * 128:(kt + 1) * 128], rhs=A.QT[pb:pb + 64, c, tsl],
                                          start=True, stop=False), reads=[A.t_KT, A.t_QT], writes=[tp])
            for jj in range(4):
                S.op("pe", lambda e: e.matmul(ps[:, jj * 128:(jj + 1) * 128], lhsT=bias_aps[jj][:, kt * 128:(kt + 1) * 128], rhs=C.ident_b[:],
                                              start=False, stop=(jj == 3)), reads=[bias_tks[jj], C.t_const], writes=[tp])
            pT, tpT = A.pT.next()
            S.op("act", lambda e: e.activation(out=pT[:], in_=ps[:], func=AF.Exp, scale=0.125), reads=[tp], writes=[tpT])
            S.op("pe", lambda e: e.matmul(oT[0:65, :], lhsT=A.V[:, kt, h, :], rhs=pT[:], start=(kt == 0), stop=(kt == nkt - 1)),
                 reads=[tpT, A.t_V], writes=[toT])
        rec, trec = A.rec.next()
        S.op("dve", lambda e: e.reciprocal(out=rec[64:65, :], in_=oT[64:65, :]), reads=[toT], writes=[trec])
        osb, tosb = A.osb.next()
        S.op("act", lambda e: e.copy(out=osb[0:64, :], in_=oT[0:64, :]), reads=[toT], writes=[tosb])
        pb_, tpb_ = C.psum.next()
        S.op("pe", lambda e: e.matmul(pb_[0:64, :], lhsT=C.ones_f[64:65, 0:64], rhs=rec[64:65, :], start=True, stop=True),
             reads=[trec, C.t_ones], writes=[tpb_])
        om, tom = A.om.next()
        S.op("dve", lambda e: e.tensor_tensor(out=om[0:64, :], in0=osb[0:64, :], in1=pb_[0:64, :], op=ALU.mult), reads=[tosb, tpb_], writes=[tom])
        S.dma("sp", A.om.dkey(), C.MIXT[out_row0 + h * 64:out_row0 + (h + 1) * 64, tsl], om[0:64, :], reads=[tom], writes=[C.t_MIXT], chain=True)


def attn_alloc(C, es, qch, kch, vcol, pref):
    nc, S = C.nc, C.S
    A = Ctx()
    A.KT = sb(nc, es, pref + "KT", [128, 2, T], BF16)
    A.t_KT = Tk()
    dk = S.dsem()
    S.dma("sp", dk, A.KT[:], C.FMT[kch * 128:(kch + 2) * 128, :].rearrange("(c p) t -> p c t", p=128), reads=[C.t_FMT], writes=[A.t_KT])
    A.V = sb(nc, es, pref + "V", [128, NT, 4, 65], BF16)
    A.t_V = Tk()
    S.op("pool", lambda e: e.memset(A.V[:, :, :, 64:65], 1.0), writes=[A.t_V])
    dk2 = S.dsem()
    for h in range(4):
        S.dma("sp", dk2, A.V[:, :, h, 0:64], C.TMO[:, vcol + h * 64:vcol + (h + 1) * 64].rearrange("(n p) d -> p n d", p=128),
              reads=[C.t_TMO], writes=[A.t_V], chain=True)
    A.QTr = Rot(S, [sb(nc, es, pref + f"QT{i}", [128, 2, 512], BF16) for i in range(2)])
    A.qch = qch
    A.pT = Rot(S, [sb(nc, es, pref + f"pT{i}", [128, 512], BF16) for i in range(4)])
    A.rec = Rot(S, [sb(nc, es, pref + f"rec{i}", [128, 512], F32) for i in range(2)])
    A.osb = Rot(S, [sb(nc, es, pref + f"osb{i}", [128, 512], F32) for i in range(2)])
    A.om = Rot(S, [sb(nc, es, pref + f"om{i}", [128, 512], BF16) for i in range(2)])
    return A


def attn_load_q(C, A, g):
    S = C.S
    q_, tq = A.QTr.next()
    S.dma("sp", A.QTr.dkey(), q_[:], C.FMT[A.qch * 128:(A.qch + 2) * 128, g * 512:(g + 1) * 512].rearrange("(c p) t -> p c t", p=128),
          reads=[C.t_FMT], writes=[tq])
    A.QT = _Shift(q_, g * 512)
    A.t_QT = tq


class _Shift:
    def __init__(self, ap, off):
        self.ap = ap
        self.off = off

    def __getitem__(self, key):
        p, c, t = key
        t = slice(t.start - self.off, t.stop - self.off)
        return self.ap[p, c, t]


def phase_moba(C):
    nc, S = C.nc, C.S
    S.barrier()
    with ExitStack() as es:
        A = attn_alloc(C, es, CH["a_q"], CH["a_k"], TMO["a_v"], "M_")
        kms = sb(nc, es, "M_kms", [128, 2, 16], F32)
        kmb = sb(nc, es, "M_kmb", [128, 2, 16], BF16)
        t_km = Tk()
        for c in range(2):
            S.op("dve", lambda e: e.tensor_reduce(out=kms[:, c, :], in_=A.KT[:, c, :].rearrange("p (n k) -> p n k", k=256), axis=AX.X, op=ALU.add),
                 reads=[A.t_KT], writes=[t_km])
        S.op("dve", lambda e: e.tensor_scalar(out=kmb[:], in0=kms[:], scalar1=1.0 / 256.0, scalar2=None, op0=ALU.mult), reads=[t_km], writes=[t_km])
        bias = Rot(S, [sb(nc, es, f"M_bias{i}", [128, T], BF16) for i in range(8)])
        gsb = Rot(S, [sb(nc, es, f"M_g{i}", [128, 16], F32) for i in range(4)])
        m8 = Rot(S, [sb(nc, es, f"M_m8{i}", [128, 8], F32) for i in range(4)])
        sbi = Rot(S, [sb(nc, es, f"M_sb{i}", [128, 16], F32) for i in range(4)])
        for g in range(NG):
            attn_load_q(C, A, g)
            for h in range(4):
                c, pb = h // 2, (h % 2) * 64
                baps, btks = [], []
                for jj in range(4):
                    j = 4 * g + jj
                    own = j // 2
                    b_, tb = bias.next()
                    if own > 0:
                        pg, tpg = C.psum.next()
                        S.op("pe", lambda e: e.matmul(pg[:, 0:16], lhsT=A.QT[pb:pb + 64, c, slice(j * 128, (j + 1) * 128)], rhs=kmb[pb:pb + 64, c, :],
                                                      start=True, stop=True), reads=[A.t_QT, t_km], writes=[tpg])
                        g_, tg = gsb.next()
                        S.op("dve", lambda e: e.tensor_copy(out=g_[:], in_=pg[:, 0:16]), reads=[tpg], writes=[tg])
                        if own < 16:
                            S.op("dve", lambda e: e.memset(g_[:, own:16], -1e30), writes=[tg])
                        m_, tm = m8.next()
                        S.op("dve", lambda e: e.max(out=m_[:], in_=g_[:]), reads=[tg], writes=[tm])
                        S.op("dve", lambda e: e.tensor_scalar(out=m_[:, 2:3], in0=m_[:, 2:3], scalar1=-1e29, scalar2=None, op0=ALU.max), reads=[tm], writes=[tm])
                        s_, ts = sbi.next()
                        S.op("dve", lambda e: e.tensor_scalar(out=s_[:], in0=g_[:], scalar1=m_[:, 2:3], scalar2=NEG, op0=ALU.is_lt, op1=ALU.mult),
                             reads=[tg, tm], writes=[ts])
                        S.op("pool", lambda e: e.tensor_copy(out=b_[:, 0:own * 256].rearrange("p (n k) -> p n k", k=256),
                                                              in_=s_[:, 0:own].unsqueeze(2).to_broadcast([128, own, 256])), reads=[ts], writes=[tb])
                    if j % 2 == 1:
                        S.op("pool", lambda e: e.memset(b_[:, (j - 1) * 128:j * 128], 0.0), writes=[tb])
                    S.op("pool", lambda e: e.tensor_copy(out=b_[:, j * 128:(j + 1) * 128], in_=C.caus_b[:]), reads=[C.t_const], writes=[tb])
                    if jj < 3:
                        S.op("pool", lambda e: e.memset(b_[:, (j + 1) * 128:(4 * g + 4) * 128], NEG), writes=[tb])
                    baps.append(b_)
                    btks.append(tb)
                attn_group(C, A, g, [h], baps, btks, 0)


DSA_ITERS = 12


def phase_dsa(C):
    nc, S = C.nc, C.S
    S.barrier()
    with ExitStack() as es:
        A = attn_alloc(C, es, CH["d_q"], CH["d_k"], TMO["d_v"], "D_")
        KI = sb(nc, es, "D_KI", [128, T], BF16)
        t_KI = Tk()
        dk = S.dsem()
        r0 = CH["d_ki"] * 128
        S.dma("sp", dk, KI[0:64, :], C.FMT[r0:r0 + 64, :], reads=[C.t_FMT], writes=[t_KI])
        S.dma("sp", dk, KI[64:128, :], C.FMT[r0:r0 + 64, :], reads=[C.t_FMT], writes=[t_KI], chain=True)
        if C.cfg.get("dbg_dsa") is not None:
            dkd3 = S.dsem()
            S.dma("sp", dkd3, C.DBG3[:, :], KI[:], reads=[t_KI], writes=[C.t_dbg])
        QI = Rot(S, [sb(nc, es, f"D_QI{i}", [128, 4, 512], BF16) for i in range(2)])
        accr = Rot(S, [sb(nc, es, f"D_acc{i}", [128, T], F32) for i in range(2)])
        rel = Rot(S, [sb(nc, es, f"D_rel{i}", [128, 512], F32) for i in range(3)])
        bias = Rot(S, [sb(nc, es, f"D_bias{i}", [128, T], BF16) for i in range(8)])
        sm = Rot(S, [sb(nc, es, f"D_sm{i}", [128, 8], F32) for i in range(2)])
        stp = Rot(S, [sb(nc, es, f"D_stp{i}", [128, DSA_ITERS], F32) for i in range(2)])
        pw = sb(nc, es, "D_pw", [128, DSA_ITERS], F32)
        thr0 = sb(nc, es, "D_thr0", [128, 1], F32)
        t_pw = Tk()
        for it in range(DSA_ITERS):
            S.op("pool", lambda e: e.memset(pw[:, it:it + 1], 2.0 ** (-(it + 1))), writes=[t_pw])
        S.op("pool", lambda e: e.memset(thr0[:], -1e29), writes=[t_pw])
        for g in range(NG):
            attn_load_q(C, A, g)
            qi_, tqi = QI.next()
            S.dma("sp", QI.dkey(), qi_[:], C.FMT[CH["d_qi"] * 128:(CH["d_qi"] + 4) * 128, g * 512:(g + 1) * 512].rearrange("(c p) t -> p c t", p=128),
                  reads=[C.t_FMT], writes=[tqi])
            baps, btks = [], []
            for jj in range(4):
                j = 4 * g + jj
                L = (j + 1) * 128
                acc_, tacc = accr.next()
                for kc in range((L + 511) // 512):
                    w = min(512, L - kc * 512)
                    ksl = slice(kc * 512, kc * 512 + w)
                    for h in range(8):
                        c, pb = h // 2, (h % 2) * 64
                        ps, tp = C.psum.next()
                        S.op("pe", lambda e: e.matmul(ps[:, 0:w], lhsT=qi_[pb:pb + 64, c, jj * 128:(jj + 1) * 128], rhs=KI[pb:pb + 64, ksl],
                                                      start=True, stop=True), reads=[tqi, t_KI], writes=[tp])
                        r_, tr = rel.next()
                        S.op("act", lambda e: e.activation(out=r_[:, 0:w], in_=ps[:, 0:w], func=AF.Relu), reads=[tp], writes=[tr])
                        if h == 0:
                            S.op("dve", lambda e: e.tensor_scalar(out=acc_[:, ksl], in0=r_[:, 0:w], scalar1=C.sgn[:, j, 0:1], scalar2=None, op0=ALU.mult),
                                 reads=[tr, C.t_sgn], writes=[tacc])
                        else:
                            S.op("dve", lambda e: e.scalar_tensor_tensor(out=acc_[:, ksl], in0=r_[:, 0:w], scalar=C.sgn[:, j, h:h + 1], in1=acc_[:, ksl],
                                                                         op0=ALU.mult, op1=ALU.add), reads=[tr, C.t_sgn, tacc], writes=[tacc])
                b_, tb = bias.next()
                s_, ts = sm.next()
                if C.cfg.get("dbg_dsa") is not None and j == C.cfg["dbg_dsa"]:
                    dkd = S.dsem()
                    S.dma("sp", dkd, C.DBG1[:, :], acc_[:], reads=[tacc], writes=[C.t_dbg])
                    S.dma("sp", dkd, C.DBG2[:, :], C.sgn[:].rearrange("p n h -> p (n h)"), reads=[C.t_sgn], writes=[C.t_dbg], chain=True)
                if j >= 2:
                    S.op("dve", lambda e: e.tensor_reduce(out=s_[:, 0:1], in_=acc_[:, 0:L], axis=AX.X, op=ALU.max), reads=[tacc], writes=[ts])
                    S.op("dve", lambda e: e.tensor_reduce(out=s_[:, 1:2], in_=acc_[:, 0:L], axis=AX.X, op=ALU.min), reads=[tacc], writes=[ts])
                S.op("dve", lambda e: e.tensor_tensor(out=acc_[:, j * 128:L], in0=acc_[:, j * 128:L], in1=C.caus_f[:], op=ALU.add),
                     reads=[tacc, C.t_const], writes=[tacc])
                if j >= 2:
                    st_, tst = stp.next()
                    S.op("dve", lambda e: e.tensor_tensor(out=s_[:, 2:3], in0=s_[:, 0:1], in1=s_[:, 1:2], op=ALU.subtract), reads=[ts], writes=[ts])
                    S.op("dve", lambda e: e.tensor_scalar(out=s_[:, 2:3], in0=s_[:, 2:3], scalar1=1.0001, scalar2=1e-6, op0=ALU.mult, op1=ALU.add),
                         reads=[ts], writes=[ts])
                    S.op("dve", lambda e: e.tensor_tensor(out=st_[:], in0=pw[:], in1=s_[:, 2:3].to_broadcast([128, DSA_ITERS]), op=ALU.mult),
                         reads=[ts, t_pw], writes=[tst])
                    S.op("dve", lambda e: e.tensor_copy(out=s_[:, 3:4], in_=s_[:, 1:2]), reads=[ts], writes=[ts])
                    for it in range(DSA_ITERS):
                        S.op("dve", lambda e: e.tensor_tensor(out=s_[:, 4:5], in0=s_[:, 3:4], in1=st_[:, it:it + 1], op=ALU.add), reads=[ts, tst], writes=[ts])
                        S.op("dve", lambda e: e.tensor_scalar(out=b_[:, 0:L], in0=acc_[:, 0:L], scalar1=s_[:, 4:5], scalar2=None, op0=ALU.is_ge, op1=ALU.add,
                                                              accum_out=s_[:, 5:6]), reads=[ts, tacc], writes=[ts, tb])
                        S.op("dve", lambda e: e.scalar_tensor_tensor(out=s_[:, 6:7], in0=s_[:, 5:6], scalar=256.0, in1=st_[:, it:it + 1], op0=ALU.is_ge, op1=ALU.mult),
                             reads=[ts, tst], writes=[ts])
                        S.op("dve", lambda e: e.tensor_tensor(out=s_[:, 3:4], in0=s_[:, 3:4], in1=s_[:, 6:7], op=ALU.add), reads=[ts], writes=[ts])
                    thr = s_[:, 3:4]
                else:
                    thr = thr0[:]
                S.op("dve", lambda e: e.tensor_scalar(out=b_[:, 0:L], in0=acc_[:, 0:L], scalar1=thr, scalar2=NEG, op0=ALU.is_lt, op1=ALU.mult),
                     reads=[ts, tacc, t_pw], writes=[tb])
                if jj < 3:
                    S.op("pool", lambda e: e.memset(b_[:, L:(4 * g + 4) * 128], NEG), writes=[tb])
                baps.append(b_)
                btks.append(tb)
            attn_group(C, A, g, [0, 1, 2, 3], baps, btks, 768)


def phase_ret(C, l):
    nc, S = C.nc, C.S
    S.barrier()
    with ExitStack() as es:
        RG = sb(nc, es, "R_RG", [128, NT, 256], BF16)
        OF = sb(nc, es, "R_OF", [128, NT, 256], F32)
        t_of, t_rg = Tk(), Tk()
        dk0 = S.dsem()
        S.dma("sp", dk0, RG[:], C.TMO[:, TMO["r_g"]:TMO["r_g"] + 256].rearrange("(n p) c -> p n c", p=128), reads=[C.t_TMO], writes=[t_rg])
        gng, t_gng = bcast_row(C, es, "R_gng", C.ret_gn_g[l:l + 1, :], 256)
        gnb, t_gnb = bcast_row(C, es, "R_gnb", C.ret_gn_b[l:l + 1, :], 256)
        with ExitStack() as es1:
            RQ = sb(nc, es1, "R_RQ", [128, 2, T], BF16)
            RK = sb(nc, es1, "R_RK", [128, 2, T], BF16)
            RQX = sb(nc, es1, "R_RQX", [128, 2, T], BF16)
            V = sb(nc, es1, "R_V", [128, NT, 256], BF16)
            RKZ = sb(nc, es1, "R_RKZ", [128, NT, 256], BF16)
            dmT = sb(nc, es1, "R_dm", [128, 4, 128], F32)
            xiT = sb(nc, es1, "R_xi", [128, 2, 128], BF16)
            gv = sb(nc, es1, "R_gv", [128, 2], F32)
            Rf = sb(nc, es1, "R_Rf", [128, 2, 64], F32)
            Rb = Rot(S, [sb(nc, es1, f"R_Rb{i}", [128, 2, 64], BF16) for i in range(2)])
            t_in, t_c, t_rqx, t_Rf = Tk(), Tk(), Tk(), Tk()
            dk = S.dsem()
            S.dma("sp", dk, RQ[:], C.FMT[CH["r_q"] * 128:(CH["r_q"] + 2) * 128, :].rearrange("(c p) t -> p c t", p=128), reads=[C.t_FMT], writes=[t_in])
            S.dma("sp", dk, RK[:], C.FMT[CH["r_k"] * 128:(CH["r_k"] + 2) * 128, :].rearrange("(c p) t -> p c t", p=128), reads=[C.t_FMT], writes=[t_in], chain=True)
            S.dma("sp", dk, V[:], C.TMO[:, TMO["r_v"]:TMO["r_v"] + 256].rearrange("(n p) c -> p n c", p=128), reads=[C.t_TMO], writes=[t_in], chain=True)
            S.dma("sp", dk, RKZ[:], C.TMO[:, TMO["rkz"]:TMO["rkz"] + 256].rearrange("(n p) c -> p n c", p=128), reads=[C.t_TMO], writes=[t_in], chain=True)
            dk2 = S.dsem()
            S.dma("sp", dk2, dmT[:], C.dmaskT[:, :, :], writes=[t_c])
            S.dma("sp", dk2, gv[:], C.gvec[:, :], writes=[t_c], chain=True)
            dk3 = S.dsem()
            S.dma("pool", dk3, xiT[:], C.xiT[:, :, :], writes=[t_c], chain=True)
            cut = C.cfg.get("cut", 99)
            if cut <= 1:
                return
            for c in range(2):
                for n in range(NT):
                    csl = slice(n * 128, (n + 1) * 128)
                    eng = "dve" if n % 2 == 0 else "pool"
                    S.op(eng, lambda e: e.tensor_tensor(out=RQX[:, c, csl], in0=RQ[:, c, csl], in1=xiT[:, c, :], op=ALU.mult), reads=[t_in, t_c], writes=[])
            t_rqx.w = {"dve": S.cnt["dve"], "pool": S.cnt["pool"]}
            S.op("dve", lambda e: e.memset(Rf[:], 0.0), writes=[t_Rf])
            if cut <= 2:
                return
            inr = Rot(S, [sb(nc, es1, f"R_in{i}", [128, 4, 128], BF16) for i in range(2)])
            rb_prev, trb_prev = None, None
            lsub = C.cfg.get("lsub", 9)
            for n in range(C.cfg.get("nchunks", NT)):
                csl = slice(n * 128, (n + 1) * 128)
                pIa, tpIa = C.psum.next()
                pIb, tpIb = C.psum.next()
                in_, tin = inr.next()
                for (pI, tpI, hs) in ((pIa, tpIa, (0, 2)), (pIb, tpIb, (1, 3))):
                    for i, h in enumerate(hs):
                        c, pb = h // 2, (h % 2) * 64
                        S.op("pe", lambda e: e.matmul(pI[:, i * 128:(i + 1) * 128], lhsT=RK[pb:pb + 64, c, csl], rhs=RQ[pb:pb + 64, c, csl], start=True, stop=True),
                             reads=[t_in], writes=[tpI])
                for (pI, tpI, hs) in ((pIa, tpIa, (0, 2)), (pIb, tpIb, (1, 3))):
                    for i, h in enumerate(hs):
                        S.op("dve", lambda e: e.tensor_tensor(out=in_[:, h, :], in0=pI[:, i * 128:(i + 1) * 128], in1=dmT[:, h, :], op=ALU.mult), reads=[tpI, t_c], writes=[tin])
                if lsub <= 1:
                    continue
                pO, tpO = C.psum.next()
                for h in range(4):
                    c, pb = h // 2, (h % 2) * 64
                    S.op("pe", lambda e: e.matmul(pO[:, h * 64:(h + 1) * 64], lhsT=in_[:, h, :], rhs=V[:, n, h * 64:(h + 1) * 64], start=True, stop=(n == 0)),
                         reads=[tin, t_in], writes=[tpO])
                    if n > 0:
                        S.op("pe", lambda e: e.matmul(pO[:, h * 64:(h + 1) * 64], lhsT=RQX[pb:pb + 64, c, csl], rhs=rb_prev[pb:pb + 64, c, :], start=False, stop=True),
                             reads=[t_rqx, trb_prev], writes=[tpO])
                S.op("act", lambda e: e.copy(out=OF[:, n, :], in_=pO[:, 0:256]), reads=[tpO], writes=[t_of])
                if lsub <= 2:
                    continue
                if n < NT - 1:
                    rb_, trb = Rb.next()
                    for c in range(2):
                        pK, tpK = C.psum.next()
                        S.op("pe", lambda e: e.matmul(pK[:, 0:128], lhsT=RKZ[:, n, c * 128:(c + 1) * 128], rhs=V[:, n, c * 128:(c + 1) * 128], start=True, stop=True),
                             reads=[t_in], writes=[tpK])
                        for hh in range(2):
                            ps_ = slice(hh * 64, (hh + 1) * 64)
                            S.op("dve", lambda e: e.scalar_tensor_tensor(out=Rf[ps_, c, :], in0=Rf[ps_, c, :], scalar=gv[ps_, c:c + 1], in1=pK[ps_, hh * 64:(hh + 1) * 64],
                                                                         op0=ALU.mult, op1=ALU.add), reads=[tpK, t_Rf, t_c], writes=[t_Rf])
                    S.op("act", lambda e: e.copy(out=rb_[:], in_=Rf[:]), reads=[t_Rf], writes=[trb])
                    rb_prev, trb_prev = rb_, trb
            S.barrier()
        if cut <= 3:
            return
        SQ = sb(nc, es, "R_SQ", [128, NT, 256], BF16)
        SG = sb(nc, es, "R_SG", [128, NT, 256], F32)
        mu = sb(nc, es, "R_mu", [128, NT * 4], F32)
        ss = sb(nc, es, "R_ss", [128, NT * 4], F32)
        t_mu, t_sq, t_sg = Tk(), Tk(), Tk()
        S.op("act", lambda e: e.activation(out=SG[:], in_=RG[:], func=AF.Silu), reads=[t_rg], writes=[t_sg])
        for n in range(NT):
            O3 = OF[:, n, :].rearrange("p (h d) -> p h d", d=64)
            S3 = SQ[:, n, :].rearrange("p (h d) -> p h d", d=64)
            m_ = mu[:, n * 4:(n + 1) * 4]
            s_ = ss[:, n * 4:(n + 1) * 4]
            S.op("dve", lambda e: e.tensor_reduce(out=m_, in_=O3, axis=AX.X, op=ALU.add), reads=[t_of], writes=[t_mu])
            S.op("dve", lambda e: e.tensor_scalar(out=m_, in0=m_, scalar1=-1.0 / 64.0, scalar2=None, op0=ALU.mult), reads=[t_mu], writes=[t_mu])
            S.op("dve", lambda e: e.tensor_tensor(out=O3, in0=O3, in1=m_.unsqueeze(2).to_broadcast([128, 4, 64]), op=ALU.add), reads=[t_of, t_mu], writes=[t_of])
            S.op("act", lambda e: e.activation(out=SQ[:, n, :], in_=OF[:, n, :], func=AF.Square), reads=[t_of], writes=[t_sq])
            S.op("dve", lambda e: e.tensor_reduce(out=s_, in_=S3, axis=AX.X, op=ALU.add), reads=[t_sq], writes=[t_mu])
            S.op("dve", lambda e: e.tensor_scalar(out=s_, in0=s_, scalar1=1.0 / 64.0, scalar2=LN_EPS, op0=ALU.mult, op1=ALU.add), reads=[t_mu], writes=[t_mu])
        S.op("act", lambda e: e.activation(out=ss[:], in_=ss[:], func=AF.Sqrt), reads=[t_mu], writes=[t_mu])
        S.op("dve", lambda e: e.reciprocal(out=ss[:], in_=ss[:]), reads=[t_mu], writes=[t_mu])
        for n in range(NT):
            O3 = OF[:, n, :].rearrange("p (h d) -> p h d", d=64)
            s_ = ss[:, n * 4:(n + 1) * 4]
            S.op("dve", lambda e: e.tensor_tensor(out=O3, in0=O3, in1=s_.unsqueeze(2).to_broadcast([128, 4, 64]), op=ALU.mult), reads=[t_of, t_mu], writes=[t_of])
            S.op("dve", lambda e: e.tensor_tensor(out=OF[:, n, :], in0=OF[:, n, :], in1=gng[:], op=ALU.mult), reads=[t_of, t_gng], writes=[t_of])
            S.op("dve", lambda e: e.tensor_tensor(out=OF[:, n, :], in0=OF[:, n, :], in1=gnb[:], op=ALU.add), reads=[t_of, t_gnb], writes=[t_of])
            S.op("dve", lambda e: e.tensor_tensor(out=SQ[:, n, :], in0=OF[:, n, :], in1=SG[:, n, :], op=ALU.mult), reads=[t_of, t_sg, t_sq], writes=[t_sq])
        if cut <= 4:
            return
        stg = Rot(S, [sb(nc, es, f"R_stg{i}", [128, 2, 512], BF16) for i in range(2)])
        for g in range(NG):
            st_, tst = stg.next()
            for c in range(2):
                pT, tpT = C.psum.next()
                pTb = pT[:].bitcast(BF16)
                for jj in range(4):
                    n = 4 * g + jj
                    S.op("pe", lambda e: e.transpose(pTb[:, jj * 128:(jj + 1) * 128], SQ[:, n, c * 128:(c + 1) * 128], C.ident_b[:]), reads=[t_sq, C.t_const], writes=[tpT])
                S.op("act", lambda e: e.copy(out=st_[:, c, :], in_=pTb[:, 0:512]), reads=[tpT], writes=[tst])
            S.dma("sp", stg.dkey(), C.MIXT[256:512, g * 512:(g + 1) * 512].rearrange("(c p) t -> p c t", p=128), st_[:], reads=[tst], writes=[C.t_MIXT], chain=True)


def phase_rglru(C, l):
    nc, S = C.nc, C.S
    S.barrier()
    with ExitStack() as es:
        pc = sb(nc, es, "G_pc", [128, 16], F32)
        wbd = sb(nc, es, "G_wbd", [128, 4, 128], BF16)
        t_pc, t_w = Tk(), Tk()
        dk = S.dsem()
        S.dma("sp", dk, pc[:], C.rg_pc[l], writes=[t_pc])
        dkw = S.dsem()
        S.dma("pool", dkw, wbd[:], C.rg_wbd[l], writes=[t_w])
        sc = sb(nc, es, "G_sc", [128, 4], F32)
        S.op("act", lambda e: e.activation(out=sc[:, 0:2], in_=pc[:, 14:16], func=AF.Exp, scale=-1.0), reads=[t_pc], writes=[t_pc])
        S.op("dve", lambda e: e.tensor_scalar(out=sc[:, 0:2], in0=sc[:, 0:2], scalar1=1.0, scalar2=None, op0=ALU.add), reads=[t_pc], writes=[t_pc])
        S.op("act", lambda e: e.activation(out=sc[:, 0:2], in_=sc[:, 0:2], func=AF.Ln), reads=[t_pc], writes=[t_pc])
        S.op("dve", lambda e: e.tensor_scalar(out=sc[:, 2:4], in0=sc[:, 0:2], scalar1=-16.0, scalar2=None, op0=ALU.mult), reads=[t_pc], writes=[t_pc])
        S.op("dve", lambda e: e.tensor_scalar(out=sc[:, 0:2], in0=sc[:, 0:2], scalar1=-8.0, scalar2=None, op0=ALU.mult), reads=[t_pc], writes=[t_pc])
        CX = sb(nc, es, "G_CX", [128, T], BF16)
        CG = sb(nc, es, "G_CG", [128, T], BF16)
        xc = sb(nc, es, "G_xc", [128, T], F32)
        xcb = sb(nc, es, "G_xcb", [128, T], BF16)
        gx = sb(nc, es, "G_gx", [128, T], F32)
        ga = sb(nc, es, "G_ga", [128, T], F32)
        a2 = sb(nc, es, "G_a2", [128, T], F32)
        gl = sb(nc, es, "G_gl", [128, T], F32)
        hh = sb(nc, es, "G_h", [128, T], F32)
        ob = sb(nc, es, "G_ob", [128, T], BF16)
        t_cx, t_cg, t_xc, t_xcb, t_gx, t_ga, t_a2, t_gl, t_h, t_ob = (Tk() for _ in range(10))
        dkx, dkg, dko = S.dsem(), S.dsem(), S.dsem()
        for c in range(2):
            S.dma("sp", dkx, CX[:], C.FMT[(CH["c_x"] + c) * 128:(CH["c_x"] + c + 1) * 128, :], reads=[C.t_FMT], writes=[t_cx])
            S.dma("sp", dkg, CG[:], C.FMT[(CH["c_g"] + c) * 128:(CH["c_g"] + c + 1) * 128, :], reads=[C.t_FMT], writes=[t_cg])
            cw = lambda i: pc[:, c * 4 + i:c * 4 + i + 1]
            S.op("dve", lambda e: e.tensor_scalar(out=xc[:], in0=CX[:], scalar1=cw(3), scalar2=pc[:, 8 + c:9 + c], op0=ALU.mult, op1=ALU.add),
                 reads=[t_cx, t_pc], writes=[t_xc])
            for sh in (1, 2, 3):
                S.op("dve", lambda e: e.scalar_tensor_tensor(out=xc[:, sh:T], in0=CX[:, 0:T - sh], scalar=cw(3 - sh), in1=xc[:, sh:T], op0=ALU.mult, op1=ALU.add),
                     reads=[t_cx, t_pc, t_xc], writes=[t_xc])
            S.op("act", lambda e: e.copy(out=xcb[:], in_=xc[:]), reads=[t_xc], writes=[t_xcb])
            for g in range(NG):
                tsl = slice(g * 512, (g + 1) * 512)
                pX, tpX = C.psum.next()
                S.op("pe", lambda e: e.matmul(pX[:], lhsT=wbd[:, c, :], rhs=xcb[:, tsl], start=True, stop=True), reads=[t_w, t_xcb], writes=[tpX])
                S.op("act", lambda e: e.activation(out=gx[:, tsl], in_=pX[:], func=AF.Sigmoid, bias=pc[:, 10 + c:11 + c]), reads=[tpX, t_pc], writes=[t_gx])
                pA, tpA = C.psum.next()
                S.op("pe", lambda e: e.matmul(pA[:], lhsT=wbd[:, 2 + c, :], rhs=xcb[:, tsl], start=True, stop=True), reads=[t_w, t_xcb], writes=[tpA])
                S.op("act", lambda e: e.activation(out=ga[:, tsl], in_=pA[:], func=AF.Sigmoid, bias=pc[:, 12 + c:13 + c]), reads=[tpA, t_pc], writes=[t_ga])
            S.op("act", lambda e: e.copy(out=gl[:], in_=CG[:]), reads=[t_cg], writes=[t_gl])
            S.op("pool", lambda e: e.tensor_tensor(out=hh[:], in0=gl[:], in1=gl[:], op=ALU.mult), reads=[t_gl], writes=[t_h])
            S.op("dve", lambda e: e.tensor_scalar(out=hh[:], in0=hh[:], scalar1=0.044715, scalar2=1.0, op0=ALU.mult, op1=ALU.add), reads=[t_h], writes=[t_h])
            S.op("dve", lambda e: e.tensor_tensor(out=hh[:], in0=hh[:], in1=gl[:], op=ALU.mult), reads=[t_h, t_gl], writes=[t_h])
            S.op("act", lambda e: e.activation(out=hh[:], in_=hh[:], func=AF.Sigmoid, scale=1.5957691216), reads=[t_h], writes=[t_h])
            S.op("dve", lambda e: e.tensor_tensor(out=gl[:], in0=gl[:], in1=hh[:], op=ALU.mult), reads=[t_h, t_gl], writes=[t_gl])
            S.op("act", lambda e: e.activation(out=a2[:], in_=ga[:], func=AF.Exp, scale=sc[:, 2 + c:3 + c]), reads=[t_ga, t_pc], writes=[t_a2])
            S.op("act", lambda e: e.activation(out=ga[:], in_=ga[:], func=AF.Exp, scale=sc[:, c:c + 1]), reads=[t_ga, t_pc], writes=[t_ga])
            S.op("dve", lambda e: e.tensor_scalar(out=a2[:], in0=a2[:], scalar1=-1.0, scalar2=1.0, op0=ALU.mult, op1=ALU.add), reads=[t_a2], writes=[t_a2])
            S.op("act", lambda e: e.activation(out=a2[:], in_=a2[:], func=AF.Sqrt), reads=[t_a2], writes=[t_a2])
            S.op("dve", lambda e: e.tensor_tensor(out=gx[:], in0=gx[:], in1=xc[:], op=ALU.mult), reads=[t_gx, t_xc], writes=[t_gx])
            S.op("dve", lambda e: e.tensor_tensor(out=gx[:], in0=gx[:], in1=a2[:], op=ALU.mult), reads=[t_gx, t_a2], writes=[t_gx])
            S.op("dve", lambda e: e.tensor_tensor_scan(out=hh[:], data0=ga[:], data1=gx[:], initial=0.0, op0=ALU.mult, op1=ALU.add), reads=[t_ga, t_gx, t_h], writes=[t_h])
            S.op("dve", lambda e: e.tensor_tensor(out=ob[:], in0=hh[:], in1=gl[:], op=ALU.mult), reads=[t_h, t_gl], writes=[t_ob])
            S.dma("sp", dko, C.MIXT[512 + c * 128:512 + (c + 1) * 128, :], ob[:], reads=[t_ob], writes=[C.t_MIXT], chain=True)


def ln_tile(C, L, y, ty, gbc, t_g, bbc, t_b):
    S = C.S
    st, tst = L.st.next()
    jk, tjk = L.jk.next()
    S.op("dve", lambda e: e.tensor_reduce(out=st[:, 0:1], in_=y[:], axis=AX.X, op=ALU.add), reads=[ty], writes=[tst])
    S.op("act", lambda e: e.activation(out=jk[:], in_=y[:], func=AF.Square), reads=[ty], writes=[tjk])
    S.op("dve", lambda e: e.tensor_reduce(out=st[:, 1:2], in_=jk[:], axis=AX.X, op=ALU.add), reads=[tjk, tst], writes=[tst])
    S.op("dve", lambda e: e.tensor_scalar(out=st[:, 0:2], in0=st[:, 0:2], scalar1=1.0 / D, scalar2=None, op0=ALU.mult), reads=[tst], writes=[tst])
    S.op("dve", lambda e: e.tensor_tensor(out=st[:, 2:3], in0=st[:, 0:1], in1=st[:, 0:1], op=ALU.mult), reads=[tst], writes=[tst])
    S.op("dve", lambda e: e.tensor_tensor(out=st[:, 1:2], in0=st[:, 1:2], in1=st[:, 2:3], op=ALU.subtract), reads=[tst], writes=[tst])
    S.op("dve", lambda e: e.tensor_scalar(out=st[:, 1:2], in0=st[:, 1:2], scalar1=LN_EPS, scalar2=None, op0=ALU.add), reads=[tst], writes=[tst])
    S.op("act", lambda e: e.activation(out=st[:, 1:2], in_=st[:, 1:2], func=AF.Sqrt), reads=[tst], writes=[tst])
    S.op("dve", lambda e: e.reciprocal(out=st[:, 1:2], in_=st[:, 1:2]), reads=[tst], writes=[tst])
    S.op("dve", lambda e: e.tensor_scalar(out=y[:], in0=y[:], scalar1=st[:, 0:1], scalar2=st[:, 1:2], op0=ALU.subtract, op1=ALU.mult), reads=[tst, ty], writes=[ty])
    S.op("dve", lambda e: e.tensor_tensor(out=y[:], in0=y[:], in1=gbc[:], op=ALU.mult), reads=[ty, t_g], writes=[ty])
    S.op("dve", lambda e: e.tensor_tensor(out=y[:], in0=y[:], in1=bbc[:], op=ALU.add), reads=[ty, t_b], writes=[ty])


def ln_alloc(C, es, pref):
    nc, S = C.nc, C.S
    L = Ctx()
    L.st = Rot(S, [sb(nc, es, pref + f"st{i}", [128, 4], F32) for i in range(2)])
    L.jk = Rot(S, [sb(nc, es, pref + f"jk{i}", [128, D], BF16) for i in range(2)])
    return L


def phase_C(C, l, x_src):
    nc, S = C.nc, C.S
    S.barrier()
    with ExitStack() as es:
        Wo = sb(nc, es, "C_Wo", [128, 8, D], BF16)
        t_wo = Tk()
        dkw = S.dsem()
        for hf in range(2):
            S.dma("pool", dkw, Wo[:, hf * 4:(hf + 1) * 4, :], C.w_out[l][hf * 512:(hf + 1) * 512, :].rearrange("(k p) d -> p k d", p=128), writes=[t_wo], chain=True)
        RW = sb(nc, es, "C_RW", [128, 8, 32], F32)
        RWh = sb(nc, es, "C_RWh", [128, 8, 32], BF16)
        RWl = sb(nc, es, "C_RWl", [128, 8, 32], BF16)
        t_rw = Tk()
        dkr = S.dsem()
        S.dma("sp", dkr, RW[:], C.router_w[l].rearrange("(k p) e -> p k e", p=128), writes=[t_rw])
        S.op("act", lambda e: e.copy(out=RWh[:], in_=RW[:]), reads=[t_rw], writes=[t_rw])
        S.op("dve", lambda e: e.tensor_tensor(out=RWl[:], in0=RW[:], in1=RWh[:], op=ALU.subtract), reads=[t_rw], writes=[t_rw])
        g1, t_g1 = bcast_row(C, es, "C_g1", C.ln1_g[l:l + 1, :], D)
        b1, t_b1 = bcast_row(C, es, "C_b1", C.ln1_b[l:l + 1, :], D)
        rb, t_rb = bcast_row(C, es, "C_rb", C.router_b[l:l + 1, :], 32)
        L = ln_alloc(C, es, "C_")
        MT = Rot(S, [sb(nc, es, f"C_MT{i}", [128, 8, 512], BF16) for i in range(2)])
        xin = Rot(S, [sb(nc, es, f"C_x{i}", [128, D], F32) for i in range(2)])
        yr = Rot(S, [sb(nc, es, f"C_y{i}", [128, D], F32) for i in range(2)])
        xtb = Rot(S, [sb(nc, es, f"C_xtb{i}", [128, 8, 128], BF16) for i in range(2)])
        xtf = Rot(S, [sb(nc, es, f"C_xtf{i}", [128, 8, 128], BF16) for i in range(2)])
        sm = Rot(S, [sb(nc, es, f"C_sm{i}", [128, 128], F32) for i in range(2)])
        for g in range(NG):
            mt, tmt = MT.next()
            S.dma("sp", MT.dkey(), mt[:], C.MIXT[:, g * 512:(g + 1) * 512].rearrange("(k p) t -> p k t", p=128), reads=[C.t_MIXT], writes=[tmt])
            for jj in range(4):
                n = 4 * g + jj
                x_, tx = xin.next()
                S.dma("sp", xin.dkey(), x_[:], x_src[n * 128:(n + 1) * 128, :], reads=[C.t_xsrc], writes=[tx])
                y_, ty = yr.next()
                for hf in range(2):
                    ps, tp = C.psum.next()
                    for k in range(8):
                        S.op("pe", lambda e: e.matmul(ps[:], lhsT=mt[:, k, jj * 128:(jj + 1) * 128], rhs=Wo[:, k, hf * 512:(hf + 1) * 512], start=(k == 0), stop=(k == 7)),
                             reads=[tmt, t_wo], writes=[tp])
                    S.op("dve", lambda e: e.scalar_tensor_tensor(out=y_[:, hf * 512:(hf + 1) * 512], in0=x_[:, hf * 512:(hf + 1) * 512], scalar=ALPHA, in1=ps[:],
                                                                 op0=ALU.mult, op1=ALU.add), reads=[tx, tp], writes=[ty])
                ln_tile(C, L, y_, ty, g1, t_g1, b1, t_b1)
                S.dma("sp", yr.dkey(), C.X1[n * 128:(n + 1) * 128, :], y_[:], reads=[ty], writes=[C.t_X1], chain=True)
                tb_, ttb = xtb.next()
                tf_, ttf = xtf.next()
                for hf in range(2):
                    ps, tp = C.psum.next()
                    for j in range(4):
                        k = hf * 4 + j
                        S.op("pe", lambda e: e.transpose(ps[:, j * 128:(j + 1) * 128], y_[:, k * 128:(k + 1) * 128], C.ident_f[:]), reads=[ty, C.t_const], writes=[tp])
                    src = ps[:].rearrange("p (k c) -> p k c", k=4)
                    S.op("act", lambda e: e.copy(out=tb_[:, hf * 4:(hf + 1) * 4, :], in_=src), reads=[tp], writes=[ttb])
                    S.op("dve", lambda e: e.tensor_tensor(out=tf_[:, hf * 4:(hf + 1) * 4, :], in0=src, in1=tb_[:, hf * 4:(hf + 1) * 4, :], op=ALU.subtract), reads=[tp, ttb], writes=[ttf])
                S.dma("sp", xtb.dkey(), C.X1T[:, n * 128:(n + 1) * 128].rearrange("(k p) t -> p k t", p=128), tb_[:], reads=[ttb], writes=[C.t_X1T], chain=True)
                pr, tpr = C.psum.next()
                i3 = 0
                for (xa, wa) in ((tb_, RWh), (tf_, RWh), (tb_, RWl)):
                    for k in range(8):
                        S.op("pe", lambda e: e.matmul(pr[:, 0:32], lhsT=xa[:, k, :], rhs=wa[:, k, :], start=(i3 == 0), stop=(i3 == 23)), reads=[ttf, ttb, t_rw], writes=[tpr])
                        i3 += 1
                s_, ts = sm.next()
                lg, ex, m8 = s_[:, 0:32], s_[:, 32:64], s_[:, 64:72]
                S.op("dve", lambda e: e.tensor_tensor(out=lg, in0=pr[:, 0:32], in1=rb[:], op=ALU.add), reads=[tpr, t_rb], writes=[ts])
                S.op("dve", lambda e: e.max(out=m8, in_=lg), reads=[ts], writes=[ts])
                S.op("dve", lambda e: e.tensor_scalar(out=s_[:, 72:73], in0=s_[:, 64:65], scalar1=-1.0, scalar2=None, op0=ALU.mult), reads=[ts], writes=[ts])
                S.op("act", lambda e: e.activation(out=ex, in_=lg, func=AF.Exp, bias=s_[:, 72:73]), reads=[ts], writes=[ts])
                S.op("dve", lambda e: e.scalar_tensor_tensor(out=ex, in0=lg, scalar=s_[:, 67:68], in1=ex, op0=ALU.is_ge, op1=ALU.mult), reads=[ts], writes=[ts])
                S.op("dve", lambda e: e.tensor_reduce(out=s_[:, 73:74], in_=ex, axis=AX.X, op=ALU.add), reads=[ts], writes=[ts])
                S.op("dve", lambda e: e.reciprocal(out=s_[:, 73:74], in_=s_[:, 73:74]), reads=[ts], writes=[ts])
                S.op("dve", lambda e: e.tensor_scalar(out=C.COMB[:, n, :], in0=ex, scalar1=s_[:, 73:74], scalar2=None, op0=ALU.mult), reads=[ts], writes=[C.t_comb])
        if "COMBD" in C.cfg.get("debug_out", ()):
            dkc = S.dsem()
            S.dma("sp", dkc, C.COMBD.rearrange("(n p) e -> p n e", p=128), C.COMB[:], reads=[C.t_comb], writes=[Tk()])


def phase_W(C, l):
    nc, S = C.nc, C.S
    S.barrier()
    NE = C.cfg.get("n_experts", N_EXPERTS)
    with ExitStack() as es:
        stg = Rot(S, [sb(nc, es, f"W_stg{i}", [128, 8192], F32) for i in range(2)])
        ob = Rot(S, [sb(nc, es, f"W_ob{i}", [128, 8192], BF16) for i in range(2)])
        for ex in range(NE):
            pieces = [(C.exp_w1[l, ex, 0:512, :], C.W1B[ex, 0:512, :], 4, 2048), (C.exp_w1[l, ex, 512:1024, :], C.W1B[ex, 512:1024, :], 4, 2048),
                      (C.exp_w2[l, ex], C.W2B[ex], 8, 1024)]
            for (src, dst, kk, ff) in pieces:
                st_, tst = stg.next()
                S.dma("sp", stg.dkey(), st_[:].rearrange("p (k f) -> p k f", k=kk), src.rearrange("(k p) f -> p k f", p=128), writes=[tst])
                o_, to = ob.next()
                S.op("act", lambda e: e.copy(out=o_[:, 0:2048], in_=st_[:, 0:2048]), reads=[tst], writes=[to])
                S.op("dve", lambda e: e.tensor_copy(out=o_[:, 2048:6144], in_=st_[:, 2048:6144]), reads=[tst], writes=[to])
                S.op("pool", lambda e: e.tensor_copy(out=o_[:, 6144:8192], in_=st_[:, 6144:8192]), reads=[tst], writes=[to])
                to.w = {"act": S.cnt["act"], "dve": S.cnt["dve"], "pool": S.cnt["pool"]}
                S.dma("sp", ob.dkey(), dst.rearrange("(k p) f -> p k f", p=128), o_[:].rearrange("p (k f) -> p k f", k=kk), reads=[to], writes=[C.t_WB], chain=True)


SIGMAX = float(1.0 / (1.0 + np.exp(-1.702 * 7.0)))
QT_ = 1024


def phase_D(C, l, dst):
    nc, S = C.nc, C.S
    S.barrier()
    NE = C.cfg.get("n_experts", N_EXPERTS)
    with ExitStack() as es:
        g2, t_g2 = bcast_row(C, es, "D_g2", C.ln2_g[l:l + 1, :], D)
        b2l, t_b2l = bcast_row(C, es, "D_b2l", C.ln2_b[l:l + 1, :], D)
        L = ln_alloc(C, es, "D_")
        B2 = sb(nc, es, "D_B2", [32, D], BF16)
        t_B2 = Tk()
        dkb = S.dsem()
        S.dma("pool", dkb, B2[:], C.exp_b2[l], writes=[t_B2])
        W1 = Rot(S, [sb(nc, es, f"D_W1_{i}", [128, 8, 2048], BF16) for i in range(2)])
        W2 = [sb(nc, es, f"D_W2_{i}", [128, 8, 1024], BF16) for i in range(2)]
        bt = Rot(S, [sb(nc, es, f"D_bt{i}", [128, 16], F32) for i in range(2)])
        bs = Rot(S, [sb(nc, es, f"D_bs{i}", [128, 16], F32) for i in range(2)])
        XT = sb(nc, es, "D_XT", [128, 8, QT_], BF16)
        acc = sb(nc, es, "D_acc", [128, QT_ // 128, D], F32)
        gT = Rot(S, [sb(nc, es, f"D_gT{i}", [128, 8, 512], BF16) for i in range(2)])
        sg = Rot(S, [sb(nc, es, f"D_sg{i}", [128, 512], F32) for i in range(2)])
        gl = Rot(S, [sb(nc, es, f"D_gl{i}", [128, 512], F32) for i in range(2)])
        ub = Rot(S, [sb(nc, es, f"D_ub{i}", [128, 512], F32) for i in range(2)])
        yb = Rot(S, [sb(nc, es, f"D_yb{i}", [128, 512], F32) for i in range(2)])
        cT = Rot(S, [sb(nc, es, f"D_cT{i}", [32, 128], BF16) for i in range(2)])
        xres = Rot(S, [sb(nc, es, f"D_xr{i}", [128, D], F32) for i in range(1)])
        t_XT, t_acc = Tk(), [Tk() for _ in range(QT_ // 128)]
        dkx = S.dsem()
        for q in range(T // QT_):
            q0 = q * QT_
            S.dma("sp", dkx, XT[:], C.X1T[:, q0:q0 + QT_].rearrange("(k p) t -> p k t", p=128), reads=[C.t_X1T], writes=[t_XT])
            for i in range(QT_ // 128):
                n = q * (QT_ // 128) + i
                pc_, tpc = C.psum.next()
                S.op("pe", lambda e: e.transpose(pc_[0:32, 0:128], C.COMB[:, n, :], C.ident_f[:]), reads=[C.t_comb, C.t_const], writes=[tpc])
                c_, tc = cT.next()
                S.op("act", lambda e: e.copy(out=c_[:], in_=pc_[0:32, 0:128]), reads=[tpc], writes=[tc])
                for hf in range(2):
                    pb_, tpb = C.psum.next()
                    S.op("pe", lambda e: e.matmul(pb_[:], lhsT=c_[:], rhs=B2[:, hf * 512:(hf + 1) * 512], start=True, stop=True), reads=[tc, t_B2], writes=[tpb])
                    S.op("act", lambda e: e.copy(out=acc[:, i, hf * 512:(hf + 1) * 512], in_=pb_[:]), reads=[tpb], writes=[t_acc[i]])
            for ex in range(NE):
                w1, tw1 = W1.next()
                w2 = W2[W1.i]
                dkw = W1.dkey()
                skipw = C.cfg.get("fake_w") and (q > 0 or ex >= 2)
                prew = "W" in C.cfg["phases"]
                for kh in range(0 if skipw else 2):
                    if prew:
                        S.dma("sp", dkw, w1[:, kh * 4:(kh + 1) * 4, :], C.W1B[ex, kh * 512:(kh + 1) * 512, :].rearrange("(k p) f -> p k f", p=128),
                              reads=[C.t_WB], writes=[tw1], chain=(kh > 0))
                    else:
                        S.dma("pool", dkw, w1[:, kh * 4:(kh + 1) * 4, :], C.exp_w1[l, ex, kh * 512:(kh + 1) * 512, :].rearrange("(k p) f -> p k f", p=128),
                              writes=[tw1], chain=(kh > 0))
                for kh in range(0 if skipw else 2):
                    if prew:
                        S.dma("sp", dkw, w2[:, kh * 4:(kh + 1) * 4, :], C.W2B[ex, kh * 512:(kh + 1) * 512, :].rearrange("(k p) d -> p k d", p=128),
                              reads=[C.t_WB], writes=[tw1], chain=True)
                    else:
                        S.dma("pool", dkw, w2[:, kh * 4:(kh + 1) * 4, :], C.exp_w2[l, ex, kh * 512:(kh + 1) * 512, :].rearrange("(k p) d -> p k d", p=128),
                              writes=[tw1], chain=True)
                bt_, tbt = bt.next()
                S.dma("sp", bt.dkey(), bt_[:], C.exp_b1p[l, ex], writes=[tbt])
                bs_, tbs = bs.next()
                S.op("dve", lambda e: e.tensor_scalar(out=bs_[:, 0:8], in0=bt_[:, 0:8], scalar1=1.702, scalar2=None, op0=ALU.mult), reads=[tbt], writes=[tbs])
                S.op("dve", lambda e: e.tensor_scalar(out=bs_[:, 8:16], in0=bt_[:, 8:16], scalar1=1.0, scalar2=None, op0=ALU.add), reads=[tbt, tbs], writes=[tbs])
                dsub = C.cfg.get("dsub", 9)
                gts = []
                for g in range(QT_ // 512 if dsub >= 2 else 0):
                    tsl = slice(g * 512, (g + 1) * 512)
                    g_, tg = gT.next()
                    gts.append((g_, tg))
                    for j in range(8):
                        pG, tpG = C.psum.next()
                        for k in range(8):
                            S.op("pe", lambda e: e.matmul(pG[:], lhsT=w1[:, k, j * 128:(j + 1) * 128], rhs=XT[:, k, tsl], start=(k == 0), stop=(k == 7)),
                                 reads=[tw1, t_XT], writes=[tpG])
                        pU, tpU = C.psum.next()
                        for k in range(8):
                            S.op("pe", lambda e: e.matmul(pU[:], lhsT=w1[:, k, 1024 + j * 128:1024 + (j + 1) * 128], rhs=XT[:, k, tsl], start=(k == 0), stop=(k == 7)),
                                 reads=[tw1, t_XT], writes=[tpU])
                        z_, tz = gl.next()
                        u_, tu = ub.next()
                        s_, ts = sg.next()
                        S.op("act", lambda e: e.activation(out=z_[:], in_=pG[:], func=AF.Identity, bias=bt_[:, j:j + 1]), reads=[tpG, tbt], writes=[tz])
                        S.op("act", lambda e: e.activation(out=u_[:], in_=pU[:], func=AF.Identity, bias=bs_[:, 8 + j:9 + j]), reads=[tpU, tbs], writes=[tu])
                        S.op("dve", lambda e: e.tensor_scalar(out=z_[:], in0=z_[:], scalar1=7.0, scalar2=None, op0=ALU.min), reads=[tz], writes=[tz])
                        S.op("act", lambda e: e.activation(out=s_[:], in_=z_[:], func=AF.Sigmoid, scale=1.702), reads=[tz], writes=[ts])
                        S.op("dve", lambda e: e.tensor_scalar(out=u_[:], in0=u_[:], scalar1=8.0, scalar2=-6.0, op0=ALU.min, op1=ALU.max), reads=[tu], writes=[tu])
                        S.op("dve", lambda e: e.tensor_tensor(out=z_[:], in0=z_[:], in1=s_[:], op=ALU.mult), reads=[tz, ts], writes=[tz])
                        S.op("dve", lambda e: e.tensor_tensor(out=g_[:, j, :], in0=u_[:], in1=z_[:], op=ALU.mult), reads=[tu, tz], writes=[tg])
                for g in range(QT_ // 512 if dsub >= 2 else 0):
                    g_, tg = gts[g]
                    for tt in range(4 if dsub >= 3 else 0):
                        i = g * 4 + tt
                        n = q * (QT_ // 128) + i
                        for hf in range(2):
                            pY, tpY = C.psum.next()
                            for j in range(8):
                                S.op("pe", lambda e: e.matmul(pY[:], lhsT=g_[:, j, tt * 128:(tt + 1) * 128], rhs=w2[:, j, hf * 512:(hf + 1) * 512], start=(j == 0), stop=(j == 7)),
                                     reads=[tg, tw1], writes=[tpY])
                            y_, tyb = yb.next()
                            S.op("act", lambda e: e.activation(out=y_[:], in_=pY[:], func=AF.Copy, scale=C.COMB[:, n, ex:ex + 1]), reads=[tpY, C.t_comb], writes=[tyb])
                            S.op("pool", lambda e: e.tensor_tensor(out=acc[:, i, hf * 512:(hf + 1) * 512], in0=acc[:, i, hf * 512:(hf + 1) * 512], in1=y_[:], op=ALU.add),
                                 reads=[tyb, t_acc[i]], writes=[t_acc[i]])
            for i in range(QT_ // 128):
                n = q * (QT_ // 128) + i
                x_, tx = xres.next()
                S.dma("sp", xres.dkey(), x_[:], C.X1[n * 128:(n + 1) * 128, :], reads=[C.t_X1], writes=[tx])
                y = acc[:, i, :]
                S.op("dve", lambda e: e.scalar_tensor_tensor(out=y, in0=x_[:], scalar=ALPHA, in1=y, op0=ALU.mult, op1=ALU.add), reads=[tx, t_acc[i]], writes=[t_acc[i]])
                ln_tile(C, L, y, t_acc[i], g2, t_g2, b2l, t_b2l)
                S.dma("sp", dkx, dst[n * 128:(n + 1) * 128, :], y, reads=[t_acc[i]], writes=[C.t_X2], chain=True)


SMALL_INPUTS = ("ret_gn_g", "ret_gn_b", "w_out", "router_w", "router_b", "ln1_g", "ln1_b", "exp_w1", "exp_w2", "exp_b2", "ln2_g", "ln2_b")


WEIGHT_KEYS = ("w_in", "conv_w", "conv_b", "rg_bx", "rg_ba", "rg_lambda", "rg_wx", "rg_wa", "exp_b1")
_CACHE = {}


def _shared_inputs(inp):
    shared = dict(host_consts())
    shared.update(prep_weights({k: inp[k] for k in WEIGHT_KEYS}))
    for k in SMALL_INPUTS:
        shared[k] = np.ascontiguousarray(inp[k], dtype=np.float32)
    return shared


def _run(inputs, n_cores=8, trace=False, first=0):
    inp = {k: np.asarray(v) for k, v in inputs.items()}
    if "nc" not in _CACHE:
        _CACHE["nc"] = build(dict(phases="AMSRGCWD", layers=DEPTH))
    nc = _CACHE["nc"]
    shared = _shared_inputs(inp)
    x = np.ascontiguousarray(inp["x"], dtype=np.float32)
    in_maps = []
    for c in range(n_cores):
        m = dict(shared)
        m["x"] = np.ascontiguousarray(x[first + c])
        in_maps.append(m)
    res = run_bass_kernel_spmd(nc, in_maps, core_ids=list(range(n_cores)), trace=trace)
    out = np.stack([np.asarray(r["out"], dtype=np.float32) for r in res.results], axis=0)
    return out, res


LAYER_KEYS = ("w_fmr", "w_fms", "w_fmp", "w_dw", "w_tm", "rg_pc", "rg_wbd", "exp_b1p") + SMALL_INPUTS


def _run_layers(inputs, n_cores=8, trace=False):
    inp = {k: np.asarray(v) for k, v in inputs.items()}
    if "nc1" not in _CACHE:
        _CACHE["nc1"] = build(dict(phases="AMSRGCD", layers=1, decl_depth=1))
    nc = _CACHE["nc1"]
    shared = _shared_inputs(inp)
    x = np.ascontiguousarray(inp["x"], dtype=np.float32)
    results = []
    for l in range(DEPTH):
        sh = dict(shared)
        for k in LAYER_KEYS:
            sh[k] = np.ascontiguousarray(shared[k][l:l + 1])
        in_maps = []
        for c in range(n_cores):
            m = dict(sh)
            m["x"] = np.ascontiguousarray(x[c])
            in_maps.append(m)
        res = run_bass_kernel_spmd(nc, in_maps, core_ids=list(range(n_cores)), trace=trace)
        results.append(res)
        x = np.stack([np.asarray(r["out"], dtype=np.float32) for r in res.results], axis=0)
    return x, results


def kernel(**inputs):
    out, _ = _run(inputs, 8)
    return out
```

```python
import numpy as np
from contextlib import ExitStack
import concourse.bass as bass
import concourse.mybir as mybir
from concourse.bass_utils import run_bass_kernel_spmd

F32 = mybir.dt.float32
BF16 = mybir.dt.bfloat16
AF = mybir.ActivationFunctionType
ALU = mybir.AluOpType
AX = mybir.AxisListType

D = 1024
T = 4096
DEPTH = 2
NT = T // 128
NG = T // 512
NEG = -30000.0
ALPHA = (2 * DEPTH) ** 0.25
LN_EPS = 1e-5
N_EXPERTS = 32

_off = {}
_o = 0
for _n, _s in (("a_q", 256), ("a_k", 256), ("a_v", 256), ("r_q", 256), ("r_k", 256), ("r_v", 256),
               ("r_g", 256), ("c_x", 256), ("c_g", 256), ("d_q", 256), ("d_k", 256), ("d_v", 256),
               ("d_qi", 512), ("d_ki", 64), ("d_w", 8)):
    _off[_n] = (_o, _s)
    _o += _s
IN_WIDTH = _o


def _cols(name):
    o, s = _off[name]
    return np.arange(o, o + s)


def _swap(c):
    c = c.reshape(-1, 64)
    return np.concatenate([c[:, 32:], c[:, :32]], axis=1).reshape(-1)


ROPE_NAMES = ("a_q", "a_k", "r_q", "r_k", "d_q", "d_k", "d_qi", "d_ki")
FM_ROPE_COLS = np.concatenate([_cols(n) for n in ROPE_NAMES])
FM_ROPE_SW = np.concatenate([_swap(_cols(n)) for n in ROPE_NAMES])
FM_PLAIN_COLS = np.concatenate([_cols("c_x"), _cols("c_g")])
DW_COLS = _cols("d_w")
TM_COLS = np.concatenate([_cols("a_v"), _cols("r_v"), _cols("d_v"), _cols("r_g"), _cols("r_k"), _cols("d_w")])
NTM = TM_COLS.shape[0]
NFMC = 21
CH = dict(a_q=0, a_k=2, r_q=4, r_k=6, d_q=8, d_k=10, d_qi=12, d_ki=16, c_x=17, c_g=19)
TMO = dict(a_v=0, r_v=256, d_v=512, r_g=768, rkz=1024)
NTMO = 1280


class Tk:
    __slots__ = ("w", "r", "name", "excl")

    def __init__(self, name="", excl=False):
        self.w = {}
        self.r = {}
        self.name = name
        self.excl = excl


class Sched:
    ENG = ("pe", "act", "dve", "pool", "sp")

    def __init__(self, nc, es):
        self.nc = nc
        self.es = es
        self.eng = {"pe": nc.tensor, "act": nc.scalar, "dve": nc.vector,
                    "pool": nc.gpsimd, "sp": nc.sync}
        self.sem = {}
        self.cnt = {}
        for k in self.ENG:
            self.sem[k] = es.enter_context(nc.semaphore("s_" + k))
            self.cnt[k] = 0
        self.seen = {k: {} for k in self.ENG}
        self.ndsem = 0
        self.nwaits = 0
        self.free_dsems = []

    def dsem(self):
        if self.free_dsems:
            return self.free_dsems.pop()
        self.ndsem += 1
        key = "d%d" % self.ndsem
        self.sem[key] = self.es.enter_context(self.nc.semaphore(key))
        self.cnt[key] = 0
        return key

    def release_dsems(self, keys):
        self.free_dsems.extend(keys)

    def _wait(self, e, deps):
        seen = self.seen[e]
        for key, val in deps.items():
            if key == "pe" and e == "pe":
                continue
            if seen.get(key, 0) >= val:
                continue
            self.eng[e].wait_ge(self.sem[key], val)
            self.nwaits += 1
            seen[key] = val

    @staticmethod
    def _merge(d, s):
        for k, v in s.items():
            if d.get(k, 0) < v:
                d[k] = v

    def _deps(self, reads, writes):
        deps = {}
        for t in reads:
            self._merge(deps, t.w)
            if t.excl:
                self._merge(deps, t.r)
        for t in writes:
            self._merge(deps, t.w)
            self._merge(deps, t.r)
        return deps

    def op(self, e, fn, reads=(), writes=()):
        self._wait(e, self._deps(reads, writes))
        ins = fn(self.eng[e])
        self.cnt[e] += 1
        ins.then_inc(self.sem[e], 1)
        me = {e: self.cnt[e]}
        for t in reads:
            self._merge(t.r, me)
        for t in writes:
            t.w = dict(me)
            t.r = {}
        return ins

    def dma(self, q, dkey, out, in_, reads=(), writes=(), chain=False, **kw):
        deps = self._deps(reads, writes)
        if not chain and self.cnt[dkey] > 0:
            self._merge(deps, {dkey: self.cnt[dkey]})
        self._wait(q, deps)
        ins = self.eng[q].dma_start(out=out, in_=in_, **kw)
        self.cnt[dkey] += 16
        ins.then_inc(self.sem[dkey], 16)
        me = {dkey: self.cnt[dkey]}
        for t in reads:
            self._merge(t.r, me)
        for t in writes:
            if chain:
                self._merge(t.w, me)
            else:
                t.w = dict(me)
            t.r = {}
        return ins

    def wait_all(self, e, toks):
        deps = {}
        for t in toks:
            self._merge(deps, t.w)
            self._merge(deps, t.r)
        self._wait(e, deps)

    def barrier(self):
        deps = {k: v for k, v in self.cnt.items() if v > 0}
        for e in self.ENG:
            self._wait(e, deps)
        self.free_dsems = [k for k in self.cnt if k.startswith("d") and k[1:].isdigit()]


class Rot:
    def __init__(self, S, aps):
        self.S = S
        self.aps = list(aps)
        self.tk = [Tk() for _ in self.aps]
        self.dk = [None] * len(self.aps)
        self.i = -1

    def next(self):
        self.i = (self.i + 1) % len(self.aps)
        return self.aps[self.i], self.tk[self.i]

    def dkey(self):
        if self.dk[self.i] is None:
            self.dk[self.i] = self.S.dsem()
        return self.dk[self.i]


_SBN = [0]


def sb(nc, es, name, shape, dt):
    _SBN[0] += 1
    return es.enter_context(nc.sbuf_tensor("%s_%d" % (name, _SBN[0]), list(shape), dt))


class Ctx:
    pass


def bcast_row(C, es, name, src_row, N):
    nc, S = C.nc, C.S
    W = min(N, 512)
    row = sb(nc, es, name + "_row", [1, W], F32)
    out = sb(nc, es, name, [128, N], F32)
    t_row, t_out = Tk(), Tk()
    dk = S.dsem()
    for c0 in range(0, N, 512):
        w = min(512, N - c0)
        S.dma("sp", dk, row[0:1, 0:w], src_row[:, c0:c0 + w], writes=[t_row])
        ps, tp = C.psum.next()
        S.op("pe", lambda e: e.matmul(ps[:, 0:w], lhsT=C.ones_row[0:1, :], rhs=row[0:1, 0:w], start=True, stop=True),
             reads=[t_row, C.t_ones], writes=[tp])
        S.op("act", lambda e: e.copy(out=out[:, c0:c0 + w], in_=ps[:, 0:w]), reads=[tp], writes=[t_out])
    return out, t_out

def phase_A(C, l, x_src):
    nc, S = C.nc, C.S
    S.barrier()
    with ExitStack() as es:
        xT = sb(nc, es, "A_xT", [128, 8, T], BF16)
        t_xT = Tk()
        cosT = sb(nc, es, "A_cosT", [128, T], F32)
        sinT = sb(nc, es, "A_sinT", [128, T], F32)
        t_tab = Tk()
        dk_tab = S.dsem()
        S.dma("sp", dk_tab, cosT[:], C.cosT[:, :], writes=[t_tab])
        S.dma("sp", dk_tab, sinT[:], C.sinT[:, :], writes=[t_tab], chain=True)
        cosTM = sb(nc, es, "A_cosTM", [128, NT, 32], F32)
        sinTM = sb(nc, es, "A_sinTM", [128, NT, 32], F32)
        S.dma("sp", dk_tab, cosTM[:], C.cosTM.rearrange("(n p) c -> p n c", p=128), writes=[t_tab], chain=True)
        S.dma("sp", dk_tab, sinTM[:], C.sinTM.rearrange("(n p) c -> p n c", p=128), writes=[t_tab], chain=True)
        zt = sb(nc, es, "A_zeta", [128, 4], F32)
        S.dma("sp", dk_tab, zt[:], C.zeta8[:, :], writes=[t_tab], chain=True)
        sel2 = sb(nc, es, "A_sel2", [8, 512], BF16)
        t_sel = Tk()
        dk_sel = S.dsem()
        S.dma("pool", dk_sel, sel2[:], C.sel2[:, :], writes=[t_sel])

        xin = Rot(S, [sb(nc, es, f"A_xin{i}", [128, D], F32) for i in range(2)])
        for n in range(NT):
            xt_, tx = xin.next()
            S.dma("sp", xin.dkey(), xt_[:], x_src[n * 128:(n + 1) * 128, :], writes=[tx])
            for half in range(2):
                ps, tp = C.psum.next()
                for j in range(4):
                    k = half * 4 + j
                    S.op("pe", lambda e: e.transpose(ps[:, j * 128:(j + 1) * 128], xt_[:, k * 128:(k + 1) * 128], C.ident_f[:]),
                         reads=[tx, C.t_const], writes=[tp])
                dst = xT[:, half * 4:(half + 1) * 4, n * 128:(n + 1) * 128]
                src = ps[:].rearrange("p (k c) -> p k c", k=4)
                if half == 0:
                    S.op("act", lambda e: e.copy(out=dst, in_=src), reads=[tp], writes=[t_xT])
                else:
                    S.op("dve", lambda e: e.tensor_copy(out=dst, in_=src), reads=[tp], writes=[t_xT])

        wtm = sb(nc, es, "A_wtm", [128, 8, NTM], BF16)
        t_wtm = Tk()
        dk_wtm = S.dsem()
        S.dma("pool", dk_wtm, wtm[:], C.w_tm[l].rearrange("(k p) c -> p k c", p=128), writes=[t_wtm])
        tmo = Rot(S, [sb(nc, es, f"A_tmo{i}", [128, NTMO], BF16) for i in range(2)])
        rk32 = Rot(S, [sb(nc, es, f"A_rk{i}", [128, 4, 64], F32) for i in range(2)])
        rkt = Rot(S, [sb(nc, es, f"A_rkt{i}", [128, 4, 64], F32) for i in range(2)])
        rku = Rot(S, [sb(nc, es, f"A_rku{i}", [128, 4, 64], F32) for i in range(2)])
        for n in range(NT):
            ot, to = tmo.next()
            banks = []
            for (c0, cn) in ((0, 512), (512, 512), (1024, NTM - 1024)):
                ps, tp = C.psum.next()
                for k in range(8):
                    S.op("pe", lambda e: e.matmul(ps[:, 0:cn], lhsT=xT[:, k, n * 128:(n + 1) * 128], rhs=wtm[:, k, c0:c0 + cn],
                                                  start=(k == 0), stop=(k == 7)),
                         reads=[t_xT, t_wtm], writes=[tp])
                banks.append((ps, tp))
            S.op("act", lambda e: e.copy(out=ot[:, 0:512], in_=banks[0][0][:, 0:512]), reads=[banks[0][1]], writes=[to])
            S.op("act", lambda e: e.copy(out=ot[:, 512:1024], in_=banks[1][0][:, 0:512]), reads=[banks[1][1]], writes=[to])
            ps2, tp2 = banks[2]
            S.op("act", lambda e: e.activation(out=C.sgn[:, n, :], in_=ps2[:, 256:264], func=AF.Sign), reads=[tp2], writes=[C.t_sgn])
            r32, tr = rk32.next()
            S.op("dve", lambda e: e.tensor_copy(out=r32[:], in_=ps2[:, 0:256].rearrange("p (h d) -> p h d", h=4)), reads=[tp2], writes=[tr])
            cb = cosTM[:, n, :].unsqueeze(1).to_broadcast([128, 4, 32])
            sbb = sinTM[:, n, :].unsqueeze(1).to_broadcast([128, 4, 32])
            ra, tra = rkt.next()
            rb, trb = rku.next()
            x1 = r32[:, :, 0:32]
            x2 = r32[:, :, 32:64]
            S.op("dve", lambda e: e.tensor_tensor(out=ra[:, :, 0:32], in0=x1, in1=cb, op=ALU.mult), reads=[tr, t_tab], writes=[tra])
            S.op("dve", lambda e: e.tensor_tensor(out=ra[:, :, 32:64], in0=x2, in1=cb, op=ALU.mult), reads=[tr, t_tab], writes=[tra])
            S.op("dve", lambda e: e.tensor_tensor(out=rb[:, :, 0:32], in0=x2, in1=sbb, op=ALU.mult), reads=[tr, t_tab], writes=[trb])
            S.op("dve", lambda e: e.tensor_tensor(out=rb[:, :, 32:64], in0=x1, in1=sbb, op=ALU.mult), reads=[tr, t_tab], writes=[trb])
            S.op("dve", lambda e: e.tensor_tensor(out=ra[:, :, 0:32], in0=ra[:, :, 0:32], in1=rb[:, :, 0:32], op=ALU.subtract), reads=[tra, trb], writes=[tra])
            S.op("dve", lambda e: e.tensor_tensor(out=ra[:, :, 32:64], in0=ra[:, :, 32:64], in1=rb[:, :, 32:64], op=ALU.add), reads=[tra, trb], writes=[tra])
            S.op("dve", lambda e: e.tensor_tensor(out=ot[:, 1024:1280].rearrange("p (h d) -> p h d", h=4), in0=ra[:],
                                                  in1=zt[:].unsqueeze(2).to_broadcast([128, 4, 64]), op=ALU.mult),
                 reads=[tra, t_tab], writes=[to])
            S.dma("sp", tmo.dkey(), C.TMO[n * 128:(n + 1) * 128, :], ot[:], reads=[to], writes=[C.t_TMO], chain=True)

        absw = sb(nc, es, "A_absw", [8, T], BF16)
        t_absw = Tk()
        wdw = sb(nc, es, "A_wdw", [128, 8, 8], BF16)
        t_wdw = Tk()
        dk_wdw = S.dsem()
        S.dma("pool", dk_wdw, wdw[:], C.w_dw[l].rearrange("(k p) c -> p k c", p=128), writes=[t_wdw])
        for g in range(NG):
            ps, tp = C.psum.next()
            for k in range(8):
                S.op("pe", lambda e: e.matmul(ps[0:8, :], lhsT=wdw[:, k, :], rhs=xT[:, k, g * 512:(g + 1) * 512],
                                              start=(k == 0), stop=(k == 7)), reads=[t_xT, t_wdw], writes=[tp])
            S.op("act", lambda e: e.activation(out=absw[:, g * 512:(g + 1) * 512], in_=ps[0:8, :], func=AF.Abs), reads=[tp], writes=[t_absw])

        wb = Rot(S, [sb(nc, es, f"A_wb{i}", [128, 8, 512], BF16) for i in range(2)])
        ws = Rot(S, [sb(nc, es, f"A_ws{i}", [128, 8, 512], BF16) for i in range(2)])
        t1r = Rot(S, [sb(nc, es, f"A_t1{i}", [128, 512], F32) for i in range(2)])
        t2r = Rot(S, [sb(nc, es, f"A_t2{i}", [128, 512], F32) for i in range(2)])
        outr = Rot(S, [sb(nc, es, f"A_out{i}", [128, 512], BF16) for i in range(4)])
        groups = [(0, 4, True, 0), (4, 4, True, 512), (8, 4, True, 1024), (12, 4, True, 1536),
                  (16, 1, True, 2048), (17, 4, False, 0)]
        for (c0, ncn, rope, wc0) in groups:
            ncols = 64 if c0 == 16 else ncn * 128
            w_, tw = wb.next()
            src = (C.w_fmr if rope else C.w_fmp)[l]
            S.dma("pool", wb.dkey(), w_[:, :, 0:ncols], src[:, wc0:wc0 + ncols].rearrange("(k p) c -> p k c", p=128), writes=[tw])
            if rope:
                wsw, tws = ws.next()
                S.dma("pool", ws.dkey(), wsw[:, :, 0:ncols], C.w_fms[l][:, wc0:wc0 + ncols].rearrange("(k p) c -> p k c", p=128), writes=[tws])
            for g in range(NG):
                tsl = slice(g * 512, (g + 1) * 512)
                for ci in range(ncn):
                    c = c0 + ci
                    M = 64 if c == 16 else 128
                    wsl = slice(ci * 128, ci * 128 + M)
                    pX, tpX = C.psum.next()
                    for k in range(8):
                        S.op("pe", lambda e: e.matmul(pX[0:M, :], lhsT=w_[:, k, wsl], rhs=xT[:, k, tsl], start=(k == 0), stop=(k == 7)),
                             reads=[t_xT, tw], writes=[tpX])
                    o_, to_ = outr.next()
                    if not rope:
                        S.op("act", lambda e: e.copy(out=o_[0:M, :], in_=pX[0:M, :]), reads=[tpX], writes=[to_])
                    else:
                        pS, tpS = C.psum.next()
                        for k in range(8):
                            S.op("pe", lambda e: e.matmul(pS[0:M, :], lhsT=wsw[:, k, wsl], rhs=xT[:, k, tsl], start=(k == 0), stop=(k == 7)),
                                 reads=[t_xT, tws], writes=[tpS])
                        a1, ta1 = t1r.next()
                        a2, ta2 = t2r.next()
                        S.op("dve", lambda e: e.tensor_tensor(out=a1[0:M, :], in0=pX[0:M, :], in1=cosT[0:M, tsl], op=ALU.mult),
                             reads=[tpX, t_tab], writes=[ta1])
                        S.op("dve", lambda e: e.tensor_tensor(out=a2[0:M, :], in0=pS[0:M, :], in1=sinT[0:M, tsl], op=ALU.mult),
                             reads=[tpS, t_tab], writes=[ta2])
                        if 12 <= c < 16:
                            S.op("dve", lambda e: e.tensor_tensor(out=a1[:], in0=a1[:], in1=a2[:], op=ALU.add), reads=[ta1, ta2], writes=[ta1])
                            pB, tpB = C.psum.next()
                            S.op("pe", lambda e: e.matmul(pB[:], lhsT=sel2[:, (c - 12) * 128:(c - 11) * 128], rhs=absw[:, tsl], start=True, stop=True),
                                 reads=[t_sel, t_absw], writes=[tpB])
                            S.op("dve", lambda e: e.tensor_tensor(out=o_[:], in0=a1[:], in1=pB[:], op=ALU.mult), reads=[ta1, tpB], writes=[to_])
                        else:
                            S.op("dve", lambda e: e.tensor_tensor(out=o_[0:M, :], in0=a1[0:M, :], in1=a2[0:M, :], op=ALU.add),
                                 reads=[ta1, ta2], writes=[to_])
                    S.dma("sp", outr.dkey(), C.FMT[c * 128:c * 128 + M, tsl], o_[0:M, :], reads=[to_], writes=[C.t_FMT], chain=True)


def host_consts():
    inv = (10000.0 ** (-np.arange(0, 64, 2, dtype=np.float32) / 64)).astype(np.float32)
    ang = np.arange(T, dtype=np.float32)[:, None] * inv[None, :]
    cos = np.cos(ang).astype(np.float32)
    sin = np.sin(ang).astype(np.float32)
    p = np.arange(128)
    d = p % 64
    cosT = np.ascontiguousarray(cos[:, d % 32].T)
    sgn = np.where(d < 32, -1.0, 1.0).astype(np.float32)
    sinT = np.ascontiguousarray((sin[:, d % 32] * sgn[None, :]).T)
    log_g = np.log(1.0 - 2.0 ** (-5.0 - np.arange(4, dtype=np.float32))).astype(np.float32)
    n = np.arange(128, dtype=np.float32)
    zeta8 = (np.exp(log_g[None, :] * (127.0 - n[:, None])) * 0.125).astype(np.float32)
    sel2 = np.zeros((8, 512), np.float32)
    for c in range(4):
        sel2[2 * c, c * 128:c * 128 + 64] = 1.0
        sel2[2 * c + 1, c * 128 + 64:c * 128 + 128] = 1.0
    d_mask = np.where(n[:, None] - n[None, :] >= 0, np.exp(log_g[:, None, None] * np.maximum(n[:, None] - n[None, :], 0.0)), 0.0)
    dmaskT = np.ascontiguousarray((d_mask * 0.125).transpose(2, 0, 1)).astype(np.float32)
    xi = np.exp(log_g[:, None] * (n[None, :] + 1.0))
    hp = np.arange(128) // 64
    xiT = np.stack([xi[2 * c + hp] for c in range(2)], axis=1).astype(np.float32)
    gch = np.exp(log_g * 128.0)
    gvec = np.stack([gch[2 * c + hp] for c in range(2)], axis=1).astype(np.float32)
    q = np.arange(128)
    caus = np.where(q[None, :] <= q[:, None], 0.0, NEG).astype(np.float32)
    causF = np.where(q[None, :] <= q[:, None], 0.0, -1e30).astype(np.float32)
    return dict(cosT=cosT, sinT=sinT, cosTM=cos, sinTM=sin, zeta8=zeta8, sel2=sel2,
                ident=np.eye(128, dtype=np.float32), caus=caus, causF=causF,
                dmaskT=dmaskT, xiT=xiT, gvec=gvec)


def prep_weights(inp):
    w_in = np.asarray(inp["w_in"], dtype=np.float32)
    out = {}
    out["w_fmr"] = np.ascontiguousarray(w_in[:, :, FM_ROPE_COLS])
    out["w_fms"] = np.ascontiguousarray(w_in[:, :, FM_ROPE_SW])
    out["w_fmp"] = np.ascontiguousarray(w_in[:, :, FM_PLAIN_COLS])
    out["w_dw"] = np.ascontiguousarray(w_in[:, :, DW_COLS])
    out["w_tm"] = np.ascontiguousarray(w_in[:, :, TM_COLS])
    if "conv_w" in inp:
        L = w_in.shape[0]
        pc = np.zeros((L, 128, 16), np.float32)
        cwt = np.asarray(inp["conv_w"], np.float32)
        for c in range(2):
            for i in range(4):
                pc[:, :, c * 4 + i] = cwt[:, i, c * 128:(c + 1) * 128]
            pc[:, :, 8 + c] = np.asarray(inp["conv_b"])[:, c * 128:(c + 1) * 128]
            pc[:, :, 10 + c] = np.asarray(inp["rg_bx"])[:, c * 128:(c + 1) * 128]
            pc[:, :, 12 + c] = np.asarray(inp["rg_ba"])[:, c * 128:(c + 1) * 128]
            pc[:, :, 14 + c] = np.asarray(inp["rg_lambda"])[:, c * 128:(c + 1) * 128]
        out["rg_pc"] = pc
        wbd = np.zeros((L, 128, 4, 128), np.float32)
        for k, nm in enumerate(("rg_wx", "rg_wa")):
            w = np.asarray(inp[nm], np.float32)
            for c in range(2):
                for b in range(2):
                    wbd[:, b * 64:(b + 1) * 64, k * 2 + c, b * 64:(b + 1) * 64] = w[:, 2 * c + b]
        out["rg_wbd"] = wbd
    if "exp_b1" in inp:
        b1 = np.asarray(inp["exp_b1"], np.float32)
        out["exp_b1p"] = np.ascontiguousarray(b1.reshape(b1.shape[0], b1.shape[1], 16, 128).transpose(0, 1, 3, 2))
    return out


def build(cfg):
    nc = bass.Bass("TRN2", target_bir_lowering=False)
    C = Ctx()
    C.nc = nc
    C.cfg = cfg
    NLD = cfg.get("decl_depth", DEPTH)
    NLAY = cfg.get("layers", DEPTH)

    def din(name, shape, dt=F32):
        return nc.dram_tensor(name, list(shape), dt, kind="ExternalInput").ap()

    def dscr(name, shape, dt):
        kind = "ExternalOutput" if name in cfg.get("debug_out", ()) else ("ExternalInput" if name in cfg.get("debug_in", ()) else "Internal")
        return nc.dram_tensor(name, list(shape), dt, kind=kind).ap()

    x_in = din("x", [T, D])
    C.cosT = din("cosT", [128, T]); C.sinT = din("sinT", [128, T])
    C.cosTM = din("cosTM", [T, 32]); C.sinTM = din("sinTM", [T, 32])
    C.zeta8 = din("zeta8", [128, 4]); C.sel2 = din("sel2", [8, 512])
    ident_d = din("ident", [128, 128])
    C.w_fmr = din("w_fmr", [NLD, D, 2112]); C.w_fms = din("w_fms", [NLD, D, 2112])
    C.w_fmp = din("w_fmp", [NLD, D, 512]); C.w_dw = din("w_dw", [NLD, D, 8])
    C.w_tm = din("w_tm", [NLD, D, NTM])
    caus_d = din("caus", [128, 128]); causF_d = din("causF", [128, 128])
    C.dmaskT = din("dmaskT", [128, 4, 128]); C.xiT = din("xiT", [128, 2, 128]); C.gvec = din("gvec", [128, 2])
    C.ret_gn_g = din("ret_gn_g", [NLD, 256]); C.ret_gn_b = din("ret_gn_b", [NLD, 256])
    C.rg_pc = din("rg_pc", [NLD, 128, 16]); C.rg_wbd = din("rg_wbd", [NLD, 128, 4, 128])
    C.w_out = din("w_out", [NLD, D, D]); C.router_w = din("router_w", [NLD, D, 32]); C.router_b = din("router_b", [NLD, 32])
    C.ln1_g = din("ln1_g", [NLD, D]); C.ln1_b = din("ln1_b", [NLD, D])
    C.X1 = dscr("X1", [T, D], F32); C.X1T = dscr("X1T", [D, T], BF16)
    C.X2 = dscr("X2", [T, D], F32); C.t_X2 = Tk()
    C.W1B = dscr("W1B", [N_EXPERTS, D, 2 * D], BF16); C.W2B = dscr("W2B", [N_EXPERTS, D, D], BF16); C.t_WB = Tk()
    C.OUT = nc.dram_tensor("out", [T, D], F32, kind="ExternalOutput").ap()
    C.exp_w1 = din("exp_w1", [NLD, N_EXPERTS, D, 2 * D]); C.exp_w2 = din("exp_w2", [NLD, N_EXPERTS, D, D])
    C.exp_b1p = din("exp_b1p", [NLD, N_EXPERTS, 128, 16]); C.exp_b2 = din("exp_b2", [NLD, N_EXPERTS, D])
    C.ln2_g = din("ln2_g", [NLD, D]); C.ln2_b = din("ln2_b", [NLD, D])
    C.t_X1 = Tk(); C.t_X1T = Tk(); C.t_xsrc = Tk(); C.t_comb = Tk()
    if "COMBD" in cfg.get("debug_out", ()):
        C.COMBD = nc.dram_tensor("COMBD", [T, 32], F32, kind="ExternalOutput").ap()
    C.MIXT = dscr("MIXT", [1024, T], BF16)
    if cfg.get("dbg_dsa") is not None:
        C.DBG1 = nc.dram_tensor("DBG1", [128, T], F32, kind="ExternalOutput").ap()
        C.DBG2 = nc.dram_tensor("DBG2", [128, NT * 8], F32, kind="ExternalOutput").ap()
        C.DBG3 = nc.dram_tensor("DBG3", [128, T], BF16, kind="ExternalOutput").ap()
        C.t_dbg = Tk()
    C.t_MIXT = Tk()
    C.FMT = dscr("FMT", [NFMC * 128, T], BF16)
    C.TMO = dscr("TMO", [T, NTMO], BF16)
    C.t_FMT = Tk(); C.t_TMO = Tk()

    with ExitStack() as es:
        S = Sched(nc, es)
        C.S = S
        banks = [es.enter_context(nc.psum_tensor(f"ps{i}", [128, 512], F32)) for i in range(8)]
        C.psum = Rot(S, banks[0:6])
        C.acc = Rot(S, banks[6:8])
        for t in C.psum.tk + C.acc.tk:
            t.excl = True
        C.ident_f = sb(nc, es, "ident_f", [128, 128], F32)
        C.ident_b = sb(nc, es, "ident_b", [128, 128], BF16)
        C.t_const = Tk()
        dk = S.dsem()
        S.dma("sp", dk, C.ident_f[:], ident_d[:, :], writes=[C.t_const])
        dk2 = S.dsem()
        S.dma("pool", dk2, C.ident_b[:], ident_d[:, :], writes=[C.t_const], chain=True)
        C.caus_b = sb(nc, es, "caus_b", [128, 128], BF16)
        C.caus_f = sb(nc, es, "caus_f", [128, 128], F32)
        C.ones_f = sb(nc, es, "ones_f", [128, 64], F32)
        S.dma("pool", dk2, C.caus_b[:], caus_d[:, :], writes=[C.t_const], chain=True)
        S.dma("sp", dk, C.caus_f[:], causF_d[:, :], writes=[C.t_const], chain=True)
        C.t_ones = Tk()
        S.op("pool", lambda e: e.memset(C.ones_f[:], 1.0), writes=[C.t_ones])
        C.ones_row = sb(nc, es, "ones_row", [1, 128], F32)
        S.op("pool", lambda e: e.memset(C.ones_row[:], 1.0), writes=[C.t_ones])
        C.sgn = sb(nc, es, "sgn", [128, NT, 8], F32)
        C.COMB = sb(nc, es, "COMB", [128, NT, 32], F32)
        C.t_sgn = Tk()

        for l in range(cfg.get("layers", DEPTH)):
            if "A" in cfg["phases"]:
                phase_A(C, l, x_in if l == 0 else C.X2)
            if "M" in cfg["phases"]:
                phase_moba(C)
            if "S" in cfg["phases"]:
                phase_dsa(C)
            if "R" in cfg["phases"]:
                phase_ret(C, l)
            if "G" in cfg["phases"]:
                phase_rglru(C, l)
            x_src = x_in if l == 0 else C.X2
            if "C" in cfg["phases"]:
                phase_C(C, l, x_src)
            if "W" in cfg["phases"]:
                phase_W(C, l)
            if "D" in cfg["phases"]:
                phase_D(C, l, C.X2 if l < NLAY - 1 else C.OUT)
        S.barrier()
        C.stats = ({k: S.cnt[k] for k in S.ENG}, S.nwaits, S.ndsem)
    return nc


def attn_group(C, A, g, heads, bias_aps, bias_tks, out_row0):
    nc, S = C.nc, C.S
    tsl = slice(g * 512, (g + 1) * 512)
    nkt = 4 * g + 4
    for h in heads:
        c, pb = h // 2, (h % 2) * 64
        oT, toT = C.acc.next()
        for kt in range(nkt):
            ps, tp = C.psum.next()
            S.op("pe", lambda e: e.matmul(ps[:], lhsT=A.KT[pb:pb + 64, c, kt * 128:(kt + 1) * 128], rhs=A.QT[pb:pb + 64, c, tsl],
                                          start=True, stop=False), reads=[A.t_KT, A.t_QT], writes=[tp])
            for jj in range(4):
                S.op("pe", lambda e: e.matmul(ps[:, jj * 128:(jj + 1) * 128], lhsT=bias_aps[jj][:, kt * 128:(kt + 1) * 128], rhs=C.ident_b[:],
                                              start=False, stop=(jj == 3)), reads=[bias_tks[jj], C.t_const], writes=[tp])
            pT, tpT = A.pT.next()
            S.op("act", lambda e: e.activation(out=pT[:], in_=ps[:], func=AF.Exp, scale=0.125), reads=[tp], writes=[tpT])
            S.op("pe", lambda e: e.matmul(oT[0:65, :], lhsT=A.V[:, kt, h, :], rhs=pT[:], start=(kt == 0), stop=(kt == nkt - 1)),
                 reads=[tpT, A.t_V], writes=[toT])
        rec, trec = A.rec.next()
        S.op("dve", lambda e: e.reciprocal(out=rec[64:65, :], in_=oT[64:65, :]), reads=[toT], writes=[trec])
        osb, tosb = A.osb.next()
        S.op("act", lambda e: e.copy(out=osb[0:64, :], in_=oT[0:64, :]), reads=[toT], writes=[tosb])
        pb_, tpb_ = C.psum.next()
        S.op("pe", lambda e: e.matmul(pb_[0:64, :], lhsT=C.ones_f[64:65, 0:64], rhs=rec[64:65, :], start=True, stop=True),
             reads=[trec, C.t_ones], writes=[tpb_])
        om, tom = A.om.next()
        S.op("dve", lambda e: e.tensor_tensor(out=om[0:64, :], in0=osb[0:64, :], in1=pb_[0:64, :], op=ALU.mult), reads=[tosb, tpb_], writes=[tom])
        S.dma("sp", A.om.dkey(), C.MIXT[out_row0 + h * 64:out_row0 + (h + 1) * 64, tsl], om[0:64, :], reads=[tom], writes=[C.t_MIXT], chain=True)


def attn_alloc(C, es, qch, kch, vcol, pref):
    nc, S = C.nc, C.S
    A = Ctx()
    A.KT = sb(nc, es, pref + "KT", [128, 2, T], BF16)
    A.t_KT = Tk()
    dk = S.dsem()
    S.dma("sp", dk, A.KT[:], C.FMT[kch * 128:(kch + 2) * 128, :].rearrange("(c p) t -> p c t", p=128), reads=[C.t_FMT], writes=[A.t_KT])
    A.V = sb(nc, es, pref + "V", [128, NT, 4, 65], BF16)
    A.t_V = Tk()
    S.op("pool", lambda e: e.memset(A.V[:, :, :, 64:65], 1.0), writes=[A.t_V])
    dk2 = S.dsem()
    for h in range(4):
        S.dma("sp", dk2, A.V[:, :, h, 0:64], C.TMO[:, vcol + h * 64:vcol + (h + 1) * 64].rearrange("(n p) d -> p n d", p=128),
              reads=[C.t_TMO], writes=[A.t_V], chain=True)
    A.QTr = Rot(S, [sb(nc, es, pref + f"QT{i}", [128, 2, 512], BF16) for i in range(2)])
    A.qch = qch
    A.pT = Rot(S, [sb(nc, es, pref + f"pT{i}", [128, 512], BF16) for i in range(4)])
    A.rec = Rot(S, [sb(nc, es, pref + f"rec{i}", [128, 512], F32) for i in range(2)])
    A.osb = Rot(S, [sb(nc, es, pref + f"osb{i}", [128, 512], F32) for i in range(2)])
    A.om = Rot(S, [sb(nc, es, pref + f"om{i}", [128, 512], BF16) for i in range(2)])
    return A


def attn_load_q(C, A, g):
    S = C.S
    q_, tq = A.QTr.next()
    S.dma("sp", A.QTr.dkey(), q_[:], C.FMT[A.qch * 128:(A.qch + 2) * 128, g * 512:(g + 1) * 512].rearrange("(c p) t -> p c t", p=128),
          reads=[C.t_FMT], writes=[tq])
    A.QT = _Shift(q_, g * 512)
    A.t_QT = tq


class _Shift:
    def __init__(self, ap, off):
        self.ap = ap
        self.off = off

    def __getitem__(self, key):
        p, c, t = key
        t = slice(t.start - self.off, t.stop - self.off)
        return self.ap[p, c, t]


def phase_moba(C):
    nc, S = C.nc, C.S
    S.barrier()
    with ExitStack() as es:
        A = attn_alloc(C, es, CH["a_q"], CH["a_k"], TMO["a_v"], "M_")
        kms = sb(nc, es, "M_kms", [128, 2, 16], F32)
        kmb = sb(nc, es, "M_kmb", [128, 2, 16], BF16)
        t_km = Tk()
        for c in range(2):
            S.op("dve", lambda e: e.tensor_reduce(out=kms[:, c, :], in_=A.KT[:, c, :].rearrange("p (n k) -> p n k", k=256), axis=AX.X, op=ALU.add),
                 reads=[A.t_KT], writes=[t_km])
        S.op("dve", lambda e: e.tensor_scalar(out=kmb[:], in0=kms[:], scalar1=1.0 / 256.0, scalar2=None, op0=ALU.mult), reads=[t_km], writes=[t_km])
        bias = Rot(S, [sb(nc, es, f"M_bias{i}", [128, T], BF16) for i in range(8)])
        gsb = Rot(S, [sb(nc, es, f"M_g{i}", [128, 16], F32) for i in range(4)])
        m8 = Rot(S, [sb(nc, es, f"M_m8{i}", [128, 8], F32) for i in range(4)])
        sbi = Rot(S, [sb(nc, es, f"M_sb{i}", [128, 16], F32) for i in range(4)])
        for g in range(NG):
            attn_load_q(C, A, g)
            for h in range(4):
                c, pb = h // 2, (h % 2) * 64
                baps, btks = [], []
                for jj in range(4):
                    j = 4 * g + jj
                    own = j // 2
                    b_, tb = bias.next()
                    if own > 0:
                        pg, tpg = C.psum.next()
                        S.op("pe", lambda e: e.matmul(pg[:, 0:16], lhsT=A.QT[pb:pb + 64, c, slice(j * 128, (j + 1) * 128)], rhs=kmb[pb:pb + 64, c, :],
                                                      start=True, stop=True), reads=[A.t_QT, t_km], writes=[tpg])
                        g_, tg = gsb.next()
                        S.op("dve", lambda e: e.tensor_copy(out=g_[:], in_=pg[:, 0:16]), reads=[tpg], writes=[tg])
                        if own < 16:
                            S.op("dve", lambda e: e.memset(g_[:, own:16], -1e30), writes=[tg])
                        m_, tm = m8.next()
                        S.op("dve", lambda e: e.max(out=m_[:], in_=g_[:]), reads=[tg], writes=[tm])
                        S.op("dve", lambda e: e.tensor_scalar(out=m_[:, 2:3], in0=m_[:, 2:3], scalar1=-1e29, scalar2=None, op0=ALU.max), reads=[tm], writes=[tm])
                        s_, ts = sbi.next()
                        S.op("dve", lambda e: e.tensor_scalar(out=s_[:], in0=g_[:], scalar1=m_[:, 2:3], scalar2=NEG, op0=ALU.is_lt, op1=ALU.mult),
                             reads=[tg, tm], writes=[ts])
                        S.op("pool", lambda e: e.tensor_copy(out=b_[:, 0:own * 256].rearrange("p (n k) -> p n k", k=256),
                                                              in_=s_[:, 0:own].unsqueeze(2).to_broadcast([128, own, 256])), reads=[ts], writes=[tb])
                    if j % 2 == 1:
                        S.op("pool", lambda e: e.memset(b_[:, (j - 1) * 128:j * 128], 0.0), writes=[tb])
                    S.op("pool", lambda e: e.tensor_copy(out=b_[:, j * 128:(j + 1) * 128], in_=C.caus_b[:]), reads=[C.t_const], writes=[tb])
                    if jj < 3:
                        S.op("pool", lambda e: e.memset(b_[:, (j + 1) * 128:(4 * g + 4) * 128], NEG), writes=[tb])
                    baps.append(b_)
                    btks.append(tb)
                attn_group(C, A, g, [h], baps, btks, 0)


DSA_ITERS = 12


def phase_dsa(C):
    nc, S = C.nc, C.S
    S.barrier()
    with ExitStack() as es:
        A = attn_alloc(C, es, CH["d_q"], CH["d_k"], TMO["d_v"], "D_")
        KI = sb(nc, es, "D_KI", [128, T], BF16)
        t_KI = Tk()
        dk = S.dsem()
        r0 = CH["d_ki"] * 128
        S.dma("sp", dk, KI[0:64, :], C.FMT[r0:r0 + 64, :], reads=[C.t_FMT], writes=[t_KI])
        S.dma("sp", dk, KI[64:128, :], C.FMT[r0:r0 + 64, :], reads=[C.t_FMT], writes=[t_KI], chain=True)
        if C.cfg.get("dbg_dsa") is not None:
            dkd3 = S.dsem()
            S.dma("sp", dkd3, C.DBG3[:, :], KI[:], reads=[t_KI], writes=[C.t_dbg])
        QI = Rot(S, [sb(nc, es, f"D_QI{i}", [128, 4, 512], BF16) for i in range(2)])
        accr = Rot(S, [sb(nc, es, f"D_acc{i}", [128, T], F32) for i in range(2)])
        rel = Rot(S, [sb(nc, es, f"D_rel{i}", [128, 512], F32) for i in range(3)])
        bias = Rot(S, [sb(nc, es, f"D_bias{i}", [128, T], BF16) for i in range(8)])
        sm = Rot(S, [sb(nc, es, f"D_sm{i}", [128, 8], F32) for i in range(2)])
        stp = Rot(S, [sb(nc, es, f"D_stp{i}", [128, DSA_ITERS], F32) for i in range(2)])
        pw = sb(nc, es, "D_pw", [128, DSA_ITERS], F32)
        thr0 = sb(nc, es, "D_thr0", [128, 1], F32)
        t_pw = Tk()
        for it in range(DSA_ITERS):
            S.op("pool", lambda e: e.memset(pw[:, it:it + 1], 2.0 ** (-(it + 1))), writes=[t_pw])
        S.op("pool", lambda e: e.memset(thr0[:], -1e29), writes=[t_pw])
        for g in range(NG):
            attn_load_q(C, A, g)
            qi_, tqi = QI.next()
            S.dma("sp", QI.dkey(), qi_[:], C.FMT[CH["d_qi"] * 128:(CH["d_qi"] + 4) * 128, g * 512:(g + 1) * 512].rearrange("(c p) t -> p c t", p=128),
                  reads=[C.t_FMT], writes=[tqi])
            baps, btks = [], []
            for jj in range(4):
                j = 4 * g + jj
                L = (j + 1) * 128
                acc_, tacc = accr.next()
                for kc in range((L + 511) // 512):
                    w = min(512, L - kc * 512)
                    ksl = slice(kc * 512, kc * 512 + w)
                    for h in range(8):
                        c, pb = h // 2, (h % 2) * 64
                        ps, tp = C.psum.next()
                        S.op("pe", lambda e: e.matmul(ps[:, 0:w], lhsT=qi_[pb:pb + 64, c, jj * 128:(jj + 1) * 128], rhs=KI[pb:pb + 64, ksl],
                                                      start=True, stop=True), reads=[tqi, t_KI], writes=[tp])
                        r_, tr = rel.next()
                        S.op("act", lambda e: e.activation(out=r_[:, 0:w], in_=ps[:, 0:w], func=AF.Relu), reads=[tp], writes=[tr])
                        if h == 0:
                            S.op("dve", lambda e: e.tensor_scalar(out=acc_[:, ksl], in0=r_[:, 0:w], scalar1=C.sgn[:, j, 0:1], scalar2=None, op0=ALU.mult),
                                 reads=[tr, C.t_sgn], writes=[tacc])
                        else:
                            S.op("dve", lambda e: e.scalar_tensor_tensor(out=acc_[:, ksl], in0=r_[:, 0:w], scalar=C.sgn[:, j, h:h + 1], in1=acc_[:, ksl],
                                                                         op0=ALU.mult, op1=ALU.add), reads=[tr, C.t_sgn, tacc], writes=[tacc])
                b_, tb = bias.next()
                s_, ts = sm.next()
                if C.cfg.get("dbg_dsa") is not None and j == C.cfg["dbg_dsa"]:
                    dkd = S.dsem()
                    S.dma("sp", dkd, C.DBG1[:, :], acc_[:], reads=[tacc], writes=[C.t_dbg])
                    S.dma("sp", dkd, C.DBG2[:, :], C.sgn[:].rearrange("p n h -> p (n h)"), reads=[C.t_sgn], writes=[C.t_dbg], chain=True)
                if j >= 2:
                    S.op("dve", lambda e: e.tensor_reduce(out=s_[:, 0:1], in_=acc_[:, 0:L], axis=AX.X, op=ALU.max), reads=[tacc], writes=[ts])
                    S.op("dve", lambda e: e.tensor_reduce(out=s_[:, 1:2], in_=acc_[:, 0:L], axis=AX.X, op=ALU.min), reads=[tacc], writes=[ts])
                S.op("dve", lambda e: e.tensor_tensor(out=acc_[:, j * 128:L], in0=acc_[:, j * 128:L], in1=C.caus_f[:], op=ALU.add),
                     reads=[tacc, C.t_const], writes=[tacc])
                if j >= 2:
                    st_, tst = stp.next()
                    S.op("dve", lambda e: e.tensor_tensor(out=s_[:, 2:3], in0=s_[:, 0:1], in1=s_[:, 1:2], op=ALU.subtract), reads=[ts], writes=[ts])
                    S.op("dve", lambda e: e.tensor_scalar(out=s_[:, 2:3], in0=s_[:, 2:3], scalar1=1.0001, scalar2=1e-6, op0=ALU.mult, op1=ALU.add),
                         reads=[ts], writes=[ts])
                    S.op("dve", lambda e: e.tensor_tensor(out=st_[:], in0=pw[:], in1=s_[:, 2:3].to_broadcast([128, DSA_ITERS]), op=ALU.mult),
                         reads=[ts, t_pw], writes=[tst])
                    S.op("dve", lambda e: e.tensor_copy(out=s_[:, 3:4], in_=s_[:, 1:2]), reads=[ts], writes=[ts])
                    for it in range(DSA_ITERS):
                        S.op("dve", lambda e: e.tensor_tensor(out=s_[:, 4:5], in0=s_[:, 3:4], in1=st_[:, it:it + 1], op=ALU.add), reads=[ts, tst], writes=[ts])
                        S.op("dve", lambda e: e.tensor_scalar(out=b_[:, 0:L], in0=acc_[:, 0:L], scalar1=s_[:, 4:5], scalar2=None, op0=ALU.is_ge, op1=ALU.add,
                                                              accum_out=s_[:, 5:6]), reads=[ts, tacc], writes=[ts, tb])
                        S.op("dve", lambda e: e.scalar_tensor_tensor(out=s_[:, 6:7], in0=s_[:, 5:6], scalar=256.0, in1=st_[:, it:it + 1], op0=ALU.is_ge, op1=ALU.mult),
                             reads=[ts, tst], writes=[ts])
                        S.op("dve", lambda e: e.tensor_tensor(out=s_[:, 3:4], in0=s_[:, 3:4], in1=s_[:, 6:7], op=ALU.add), reads=[ts], writes=[ts])
                    thr = s_[:, 3:4]
                else:
                    thr = thr0[:]
                S.op("dve", lambda e: e.tensor_scalar(out=b_[:, 0:L], in0=acc_[:, 0:L], scalar1=thr, scalar2=NEG, op0=ALU.is_lt, op1=ALU.mult),
                     reads=[ts, tacc, t_pw], writes=[tb])
                if jj < 3:
                    S.op("pool", lambda e: e.memset(b_[:, L:(4 * g + 4) * 128], NEG), writes=[tb])
                baps.append(b_)
                btks.append(tb)
            attn_group(C, A, g, [0, 1, 2, 3], baps, btks, 768)


def phase_ret(C, l):
    nc, S = C.nc, C.S
    S.barrier()
    with ExitStack() as es:
        RG = sb(nc, es, "R_RG", [128, NT, 256], BF16)
        OF = sb(nc, es, "R_OF", [128, NT, 256], F32)
        t_of, t_rg = Tk(), Tk()
        dk0 = S.dsem()
        S.dma("sp", dk0, RG[:], C.TMO[:, TMO["r_g"]:TMO["r_g"] + 256].rearrange("(n p) c -> p n c", p=128), reads=[C.t_TMO], writes=[t_rg])
        gng, t_gng = bcast_row(C, es, "R_gng", C.ret_gn_g[l:l + 1, :], 256)
        gnb, t_gnb = bcast_row(C, es, "R_gnb", C.ret_gn_b[l:l + 1, :], 256)
        with ExitStack() as es1:
            RQ = sb(nc, es1, "R_RQ", [128, 2, T], BF16)
            RK = sb(nc, es1, "R_RK", [128, 2, T], BF16)
            RQX = sb(nc, es1, "R_RQX", [128, 2, T], BF16)
            V = sb(nc, es1, "R_V", [128, NT, 256], BF16)
            RKZ = sb(nc, es1, "R_RKZ", [128, NT, 256], BF16)
            dmT = sb(nc, es1, "R_dm", [128, 4, 128], F32)
            xiT = sb(nc, es1, "R_xi", [128, 2, 128], BF16)
            gv = sb(nc, es1, "R_gv", [128, 2], F32)
            Rf = sb(nc, es1, "R_Rf", [128, 2, 64], F32)
            Rb = Rot(S, [sb(nc, es1, f"R_Rb{i}", [128, 2, 64], BF16) for i in range(2)])
            t_in, t_c, t_rqx, t_Rf = Tk(), Tk(), Tk(), Tk()
            dk = S.dsem()
            S.dma("sp", dk, RQ[:], C.FMT[CH["r_q"] * 128:(CH["r_q"] + 2) * 128, :].rearrange("(c p) t -> p c t", p=128), reads=[C.t_FMT], writes=[t_in])
            S.dma("sp", dk, RK[:], C.FMT[CH["r_k"] * 128:(CH["r_k"] + 2) * 128, :].rearrange("(c p) t -> p c t", p=128), reads=[C.t_FMT], writes=[t_in], chain=True)
            S.dma("sp", dk, V[:], C.TMO[:, TMO["r_v"]:TMO["r_v"] + 256].rearrange("(n p) c -> p n c", p=128), reads=[C.t_TMO], writes=[t_in], chain=True)
            S.dma("sp", dk, RKZ[:], C.TMO[:, TMO["rkz"]:TMO["rkz"] + 256].rearrange("(n p) c -> p n c", p=128), reads=[C.t_TMO], writes=[t_in], chain=True)
            dk2 = S.dsem()
            S.dma("sp", dk2, dmT[:], C.dmaskT[:, :, :], writes=[t_c])
            S.dma("sp", dk2, gv[:], C.gvec[:, :], writes=[t_c], chain=True)
            dk3 = S.dsem()
            S.dma("pool", dk3, xiT[:], C.xiT[:, :, :], writes=[t_c], chain=True)
            cut = C.cfg.get("cut", 99)
            if cut <= 1:
                return
            for c in range(2):
                for n in range(NT):
                    csl = slice(n * 128, (n + 1) * 128)
                    eng = "dve" if n % 2 == 0 else "pool"
                    S.op(eng, lambda e: e.tensor_tensor(out=RQX[:, c, csl], in0=RQ[:, c, csl], in1=xiT[:, c, :], op=ALU.mult), reads=[t_in, t_c], writes=[])
            t_rqx.w = {"dve": S.cnt["dve"], "pool": S.cnt["pool"]}
            S.op("dve", lambda e: e.memset(Rf[:], 0.0), writes=[t_Rf])
            if cut <= 2:
                return
            inr = Rot(S, [sb(nc, es1, f"R_in{i}", [128, 4, 128], BF16) for i in range(2)])
            rb_prev, trb_prev = None, None
            lsub = C.cfg.get("lsub", 9)
            for n in range(C.cfg.get("nchunks", NT)):
                csl = slice(n * 128, (n + 1) * 128)
                pIa, tpIa = C.psum.next()
                pIb, tpIb = C.psum.next()
                in_, tin = inr.next()
                for (pI, tpI, hs) in ((pIa, tpIa, (0, 2)), (pIb, tpIb, (1, 3))):
                    for i, h in enumerate(hs):
                        c, pb = h // 2, (h % 2) * 64
                        S.op("pe", lambda e: e.matmul(pI[:, i * 128:(i + 1) * 128], lhsT=RK[pb:pb + 64, c, csl], rhs=RQ[pb:pb + 64, c, csl], start=True, stop=True),
                             reads=[t_in], writes=[tpI])
                for (pI, tpI, hs) in ((pIa, tpIa, (0, 2)), (pIb, tpIb, (1, 3))):
                    for i, h in enumerate(hs):
                        S.op("dve", lambda e: e.tensor_tensor(out=in_[:, h, :], in0=pI[:, i * 128:(i + 1) * 128], in1=dmT[:, h, :], op=ALU.mult), reads=[tpI, t_c], writes=[tin])
                if lsub <= 1:
                    continue
                pO, tpO = C.psum.next()
                for h in range(4):
                    c, pb = h // 2, (h % 2) * 64
                    S.op("pe", lambda e: e.matmul(pO[:, h * 64:(h + 1) * 64], lhsT=in_[:, h, :], rhs=V[:, n, h * 64:(h + 1) * 64], start=True, stop=(n == 0)),
                         reads=[tin, t_in], writes=[tpO])
                    if n > 0:
                        S.op("pe", lambda e: e.matmul(pO[:, h * 64:(h + 1) * 64], lhsT=RQX[pb:pb + 64, c, csl], rhs=rb_prev[pb:pb + 64, c, :], start=False, stop=True),
                             reads=[t_rqx, trb_prev], writes=[tpO])
                S.op("act", lambda e: e.copy(out=OF[:, n, :], in_=pO[:, 0:256]), reads=[tpO], writes=[t_of])
                if lsub <= 2:
                    continue
                if n < NT - 1:
                    rb_, trb = Rb.next()
                    for c in range(2):
                        pK, tpK = C.psum.next()
                        S.op("pe", lambda e: e.matmul(pK[:, 0:128], lhsT=RKZ[:, n, c * 128:(c + 1) * 128], rhs=V[:, n, c * 128:(c + 1) * 128], start=True, stop=True),
                             reads=[t_in], writes=[tpK])
                        for hh in range(2):
                            ps_ = slice(hh * 64, (hh + 1) * 64)
                            S.op("dve", lambda e: e.scalar_tensor_tensor(out=Rf[ps_, c, :], in0=Rf[ps_, c, :], scalar=gv[ps_, c:c + 1], in1=pK[ps_, hh * 64:(hh + 1) * 64],
                                                                         op0=ALU.mult, op1=ALU.add), reads=[tpK, t_Rf, t_c], writes=[t_Rf])
                    S.op("act", lambda e: e.copy(out=rb_[:], in_=Rf[:]), reads=[t_Rf], writes=[trb])
                    rb_prev, trb_prev = rb_, trb
            S.barrier()
        if cut <= 3:
            return
        SQ = sb(nc, es, "R_SQ", [128, NT, 256], BF16)
        SG = sb(nc, es, "R_SG", [128, NT, 256], F32)
        mu = sb(nc, es, "R_mu", [128, NT * 4], F32)
        ss = sb(nc, es, "R_ss", [128, NT * 4], F32)
        t_mu, t_sq, t_sg = Tk(), Tk(), Tk()
        S.op("act", lambda e: e.activation(out=SG[:], in_=RG[:], func=AF.Silu), reads=[t_rg], writes=[t_sg])
        for n in range(NT):
            O3 = OF[:, n, :].rearrange("p (h d) -> p h d", d=64)
            S3 = SQ[:, n, :].rearrange("p (h d) -> p h d", d=64)
            m_ = mu[:, n * 4:(n + 1) * 4]
            s_ = ss[:, n * 4:(n + 1) * 4]
            S.op("dve", lambda e: e.tensor_reduce(out=m_, in_=O3, axis=AX.X, op=ALU.add), reads=[t_of], writes=[t_mu])
            S.op("dve", lambda e: e.tensor_scalar(out=m_, in0=m_, scalar1=-1.0 / 64.0, scalar2=None, op0=ALU.mult), reads=[t_mu], writes=[t_mu])
            S.op("dve", lambda e: e.tensor_tensor(out=O3, in0=O3, in1=m_.unsqueeze(2).to_broadcast([128, 4, 64]), op=ALU.add), reads=[t_of, t_mu], writes=[t_of])
            S.op("act", lambda e: e.activation(out=SQ[:, n, :], in_=OF[:, n, :], func=AF.Square), reads=[t_of], writes=[t_sq])
            S.op("dve", lambda e: e.tensor_reduce(out=s_, in_=S3, axis=AX.X, op=ALU.add), reads=[t_sq], writes=[t_mu])
            S.op("dve", lambda e: e.tensor_scalar(out=s_, in0=s_, scalar1=1.0 / 64.0, scalar2=LN_EPS, op0=ALU.mult, op1=ALU.add), reads=[t_mu], writes=[t_mu])
        S.op("act", lambda e: e.activation(out=ss[:], in_=ss[:], func=AF.Sqrt), reads=[t_mu], writes=[t_mu])
        S.op("dve", lambda e: e.reciprocal(out=ss[:], in_=ss[:]), reads=[t_mu], writes=[t_mu])
        for n in range(NT):
            O3 = OF[:, n, :].rearrange("p (h d) -> p h d", d=64)
            s_ = ss[:, n * 4:(n + 1) * 4]
            S.op("dve", lambda e: e.tensor_tensor(out=O3, in0=O3, in1=s_.unsqueeze(2).to_broadcast([128, 4, 64]), op=ALU.mult), reads=[t_of, t_mu], writes=[t_of])
            S.op("dve", lambda e: e.tensor_tensor(out=OF[:, n, :], in0=OF[:, n, :], in1=gng[:], op=ALU.mult), reads=[t_of, t_gng], writes=[t_of])
            S.op("dve", lambda e: e.tensor_tensor(out=OF[:, n, :], in0=OF[:, n, :], in1=gnb[:], op=ALU.add), reads=[t_of, t_gnb], writes=[t_of])
            S.op("dve", lambda e: e.tensor_tensor(out=SQ[:, n, :], in0=OF[:, n, :], in1=SG[:, n, :], op=ALU.mult), reads=[t_of, t_sg, t_sq], writes=[t_sq])
        if cut <= 4:
            return
        stg = Rot(S, [sb(nc, es, f"R_stg{i}", [128, 2, 512], BF16) for i in range(2)])
        for g in range(NG):
            st_, tst = stg.next()
            for c in range(2):
                pT, tpT = C.psum.next()
                pTb = pT[:].bitcast(BF16)
                for jj in range(4):
                    n = 4 * g + jj
                    S.op("pe", lambda e: e.transpose(pTb[:, jj * 128:(jj + 1) * 128], SQ[:, n, c * 128:(c + 1) * 128], C.ident_b[:]), reads=[t_sq, C.t_const], writes=[tpT])
                S.op("act", lambda e: e.copy(out=st_[:, c, :], in_=pTb[:, 0:512]), reads=[tpT], writes=[tst])
            S.dma("sp", stg.dkey(), C.MIXT[256:512, g * 512:(g + 1) * 512].rearrange("(c p) t -> p c t", p=128), st_[:], reads=[tst], writes=[C.t_MIXT], chain=True)


def phase_rglru(C, l):
    nc, S = C.nc, C.S
    S.barrier()
    with ExitStack() as es:
        pc = sb(nc, es, "G_pc", [128, 16], F32)
        wbd = sb(nc, es, "G_wbd", [128, 4, 128], BF16)
        t_pc, t_w = Tk(), Tk()
        dk = S.dsem()
        S.dma("sp", dk, pc[:], C.rg_pc[l], writes=[t_pc])
        dkw = S.dsem()
        S.dma("pool", dkw, wbd[:], C.rg_wbd[l], writes=[t_w])
        sc = sb(nc, es, "G_sc", [128, 4], F32)
        S.op("act", lambda e: e.activation(out=sc[:, 0:2], in_=pc[:, 14:16], func=AF.Exp, scale=-1.0), reads=[t_pc], writes=[t_pc])
        S.op("dve", lambda e: e.tensor_scalar(out=sc[:, 0:2], in0=sc[:, 0:2], scalar1=1.0, scalar2=None, op0=ALU.add), reads=[t_pc], writes=[t_pc])
        S.op("act", lambda e: e.activation(out=sc[:, 0:2], in_=sc[:, 0:2], func=AF.Ln), reads=[t_pc], writes=[t_pc])
        S.op("dve", lambda e: e.tensor_scalar(out=sc[:, 2:4], in0=sc[:, 0:2], scalar1=-16.0, scalar2=None, op0=ALU.mult), reads=[t_pc], writes=[t_pc])
        S.op("dve", lambda e: e.tensor_scalar(out=sc[:, 0:2], in0=sc[:, 0:2], scalar1=-8.0, scalar2=None, op0=ALU.mult), reads=[t_pc], writes=[t_pc])
        CX = sb(nc, es, "G_CX", [128, T], BF16)
        CG = sb(nc, es, "G_CG", [128, T], BF16)
        xc = sb(nc, es, "G_xc", [128, T], F32)
        xcb = sb(nc, es, "G_xcb", [128, T], BF16)
        gx = sb(nc, es, "G_gx", [128, T], F32)
        ga = sb(nc, es, "G_ga", [128, T], F32)
        a2 = sb(nc, es, "G_a2", [128, T], F32)
        gl = sb(nc, es, "G_gl", [128, T], F32)
        hh = sb(nc, es, "G_h", [128, T], F32)
        ob = sb(nc, es, "G_ob", [128, T], BF16)
        t_cx, t_cg, t_xc, t_xcb, t_gx, t_ga, t_a2, t_gl, t_h, t_ob = (Tk() for _ in range(10))
        dkx, dkg, dko = S.dsem(), S.dsem(), S.dsem()
        for c in range(2):
            S.dma("sp", dkx, CX[:], C.FMT[(CH["c_x"] + c) * 128:(CH["c_x"] + c + 1) * 128, :], reads=[C.t_FMT], writes=[t_cx])
            S.dma("sp", dkg, CG[:], C.FMT[(CH["c_g"] + c) * 128:(CH["c_g"] + c + 1) * 128, :], reads=[C.t_FMT], writes=[t_cg])
            cw = lambda i: pc[:, c * 4 + i:c * 4 + i + 1]
            S.op("dve", lambda e: e.tensor_scalar(out=xc[:], in0=CX[:], scalar1=cw(3), scalar2=pc[:, 8 + c:9 + c], op0=ALU.mult, op1=ALU.add),
                 reads=[t_cx, t_pc], writes=[t_xc])
            for sh in (1, 2, 3):
                S.op("dve", lambda e: e.scalar_tensor_tensor(out=xc[:, sh:T], in0=CX[:, 0:T - sh], scalar=cw(3 - sh), in1=xc[:, sh:T], op0=ALU.mult, op1=ALU.add),
                     reads=[t_cx, t_pc, t_xc], writes=[t_xc])
            S.op("act", lambda e: e.copy(out=xcb[:], in_=xc[:]), reads=[t_xc], writes=[t_xcb])
            for g in range(NG):
                tsl = slice(g * 512, (g + 1) * 512)
                pX, tpX = C.psum.next()
                S.op("pe", lambda e: e.matmul(pX[:], lhsT=wbd[:, c, :], rhs=xcb[:, tsl], start=True, stop=True), reads=[t_w, t_xcb], writes=[tpX])
                S.op("act", lambda e: e.activation(out=gx[:, tsl], in_=pX[:], func=AF.Sigmoid, bias=pc[:, 10 + c:11 + c]), reads=[tpX, t_pc], writes=[t_gx])
                pA, tpA = C.psum.next()
                S.op("pe", lambda e: e.matmul(pA[:], lhsT=wbd[:, 2 + c, :], rhs=xcb[:, tsl], start=True, stop=True), reads=[t_w, t_xcb], writes=[tpA])
                S.op("act", lambda e: e.activation(out=ga[:, tsl], in_=pA[:], func=AF.Sigmoid, bias=pc[:, 12 + c:13 + c]), reads=[tpA, t_pc], writes=[t_ga])
            S.op("act", lambda e: e.copy(out=gl[:], in_=CG[:]), reads=[t_cg], writes=[t_gl])
            S.op("pool", lambda e: e.tensor_tensor(out=hh[:], in0=gl[:], in1=gl[:], op=ALU.mult), reads=[t_gl], writes=[t_h])
            S.op("dve", lambda e: e.tensor_scalar(out=hh[:], in0=hh[:], scalar1=0.044715, scalar2=1.0, op0=ALU.mult, op1=ALU.add), reads=[t_h], writes=[t_h])
            S.op("dve", lambda e: e.tensor_tensor(out=hh[:], in0=hh[:], in1=gl[:], op=ALU.mult), reads=[t_h, t_gl], writes=[t_h])
            S.op("act", lambda e: e.activation(out=hh[:], in_=hh[:], func=AF.Sigmoid, scale=1.5957691216), reads=[t_h], writes=[t_h])
            S.op("dve", lambda e: e.tensor_tensor(out=gl[:], in0=gl[:], in1=hh[:], op=ALU.mult), reads=[t_h, t_gl], writes=[t_gl])
            S.op("act", lambda e: e.activation(out=a2[:], in_=ga[:], func=AF.Exp, scale=sc[:, 2 + c:3 + c]), reads=[t_ga, t_pc], writes=[t_a2])
            S.op("act", lambda e: e.activation(out=ga[:], in_=ga[:], func=AF.Exp, scale=sc[:, c:c + 1]), reads=[t_ga, t_pc], writes=[t_ga])
            S.op("dve", lambda e: e.tensor_scalar(out=a2[:], in0=a2[:], scalar1=-1.0, scalar2=1.0, op0=ALU.mult, op1=ALU.add), reads=[t_a2], writes=[t_a2])
            S.op("act", lambda e: e.activation(out=a2[:], in_=a2[:], func=AF.Sqrt), reads=[t_a2], writes=[t_a2])
            S.op("dve", lambda e: e.tensor_tensor(out=gx[:], in0=gx[:], in1=xc[:], op=ALU.mult), reads=[t_gx, t_xc], writes=[t_gx])
            S.op("dve", lambda e: e.tensor_tensor(out=gx[:], in0=gx[:], in1=a2[:], op=ALU.mult), reads=[t_gx, t_a2], writes=[t_gx])
            S.op("dve", lambda e: e.tensor_tensor_scan(out=hh[:], data0=ga[:], data1=gx[:], initial=0.0, op0=ALU.mult, op1=ALU.add), reads=[t_ga, t_gx, t_h], writes=[t_h])
            S.op("dve", lambda e: e.tensor_tensor(out=ob[:], in0=hh[:], in1=gl[:], op=ALU.mult), reads=[t_h, t_gl], writes=[t_ob])
            S.dma("sp", dko, C.MIXT[512 + c * 128:512 + (c + 1) * 128, :], ob[:], reads=[t_ob], writes=[C.t_MIXT], chain=True)


def ln_tile(C, L, y, ty, gbc, t_g, bbc, t_b):
    S = C.S
    st, tst = L.st.next()
    jk, tjk = L.jk.next()
    S.op("dve", lambda e: e.tensor_reduce(out=st[:, 0:1], in_=y[:], axis=AX.X, op=ALU.add), reads=[ty], writes=[tst])
    S.op("act", lambda e: e.activation(out=jk[:], in_=y[:], func=AF.Square), reads=[ty], writes=[tjk])
    S.op("dve", lambda e: e.tensor_reduce(out=st[:, 1:2], in_=jk[:], axis=AX.X, op=ALU.add), reads=[tjk, tst], writes=[tst])
    S.op("dve", lambda e: e.tensor_scalar(out=st[:, 0:2], in0=st[:, 0:2], scalar1=1.0 / D, scalar2=None, op0=ALU.mult), reads=[tst], writes=[tst])
    S.op("dve", lambda e: e.tensor_tensor(out=st[:, 2:3], in0=st[:, 0:1], in1=st[:, 0:1], op=ALU.mult), reads=[tst], writes=[tst])
    S.op("dve", lambda e: e.tensor_tensor(out=st[:, 1:2], in0=st[:, 1:2], in1=st[:, 2:3], op=ALU.subtract), reads=[tst], writes=[tst])
    S.op("dve", lambda e: e.tensor_scalar(out=st[:, 1:2], in0=st[:, 1:2], scalar1=LN_EPS, scalar2=None, op0=ALU.add), reads=[tst], writes=[tst])
    S.op("act", lambda e: e.activation(out=st[:, 1:2], in_=st[:, 1:2], func=AF.Sqrt), reads=[tst], writes=[tst])
    S.op("dve", lambda e: e.reciprocal(out=st[:, 1:2], in_=st[:, 1:2]), reads=[tst], writes=[tst])
    S.op("dve", lambda e: e.tensor_scalar(out=y[:], in0=y[:], scalar1=st[:, 0:1], scalar2=st[:, 1:2], op0=ALU.subtract, op1=ALU.mult), reads=[tst, ty], writes=[ty])
    S.op("dve", lambda e: e.tensor_tensor(out=y[:], in0=y[:], in1=gbc[:], op=ALU.mult), reads=[ty, t_g], writes=[ty])
    S.op("dve", lambda e: e.tensor_tensor(out=y[:], in0=y[:], in1=bbc[:], op=ALU.add), reads=[ty, t_b], writes=[ty])


def ln_alloc(C, es, pref):
    nc, S = C.nc, C.S
    L = Ctx()
    L.st = Rot(S, [sb(nc, es, pref + f"st{i}", [128, 4], F32) for i in range(2)])
    L.jk = Rot(S, [sb(nc, es, pref + f"jk{i}", [128, D], BF16) for i in range(2)])
    return L


def phase_C(C, l, x_src):
    nc, S = C.nc, C.S
    S.barrier()
    with ExitStack() as es:
        Wo = sb(nc, es, "C_Wo", [128, 8, D], BF16)
        t_wo = Tk()
        dkw = S.dsem()
        for hf in range(2):
            S.dma("pool", dkw, Wo[:, hf * 4:(hf + 1) * 4, :], C.w_out[l][hf * 512:(hf + 1) * 512, :].rearrange("(k p) d -> p k d", p=128), writes=[t_wo], chain=True)
        RW = sb(nc, es, "C_RW", [128, 8, 32], F32)
        RWh = sb(nc, es, "C_RWh", [128, 8, 32], BF16)
        RWl = sb(nc, es, "C_RWl", [128, 8, 32], BF16)
        t_rw = Tk()
        dkr = S.dsem()
        S.dma("sp", dkr, RW[:], C.router_w[l].rearrange("(k p) e -> p k e", p=128), writes=[t_rw])
        S.op("act", lambda e: e.copy(out=RWh[:], in_=RW[:]), reads=[t_rw], writes=[t_rw])
        S.op("dve", lambda e: e.tensor_tensor(out=RWl[:], in0=RW[:], in1=RWh[:], op=ALU.subtract), reads=[t_rw], writes=[t_rw])
        g1, t_g1 = bcast_row(C, es, "C_g1", C.ln1_g[l:l + 1, :], D)
        b1, t_b1 = bcast_row(C, es, "C_b1", C.ln1_b[l:l + 1, :], D)
        rb, t_rb = bcast_row(C, es, "C_rb", C.router_b[l:l + 1, :], 32)
        L = ln_alloc(C, es, "C_")
        MT = Rot(S, [sb(nc, es, f"C_MT{i}", [128, 8, 512], BF16) for i in range(2)])
        xin = Rot(S, [sb(nc, es, f"C_x{i}", [128, D], F32) for i in range(2)])
        yr = Rot(S, [sb(nc, es, f"C_y{i}", [128, D], F32) for i in range(2)])
        xtb = Rot(S, [sb(nc, es, f"C_xtb{i}", [128, 8, 128], BF16) for i in range(2)])
        xtf = Rot(S, [sb(nc, es, f"C_xtf{i}", [128, 8, 128], BF16) for i in range(2)])
        sm = Rot(S, [sb(nc, es, f"C_sm{i}", [128, 128], F32) for i in range(2)])
        for g in range(NG):
            mt, tmt = MT.next()
            S.dma("sp", MT.dkey(), mt[:], C.MIXT[:, g * 512:(g + 1) * 512].rearrange("(k p) t -> p k t", p=128), reads=[C.t_MIXT], writes=[tmt])
            for jj in range(4):
                n = 4 * g + jj
                x_, tx = xin.next()
                S.dma("sp", xin.dkey(), x_[:], x_src[n * 128:(n + 1) * 128, :], reads=[C.t_xsrc], writes=[tx])
                y_, ty = yr.next()
                for hf in range(2):
                    ps, tp = C.psum.next()
                    for k in range(8):
                        S.op("pe", lambda e: e.matmul(ps[:], lhsT=mt[:, k, jj * 128:(jj + 1) * 128], rhs=Wo[:, k, hf * 512:(hf + 1) * 512], start=(k == 0), stop=(k == 7)),
                             reads=[tmt, t_wo], writes=[tp])
                    S.op("dve", lambda e: e.scalar_tensor_tensor(out=y_[:, hf * 512:(hf + 1) * 512], in0=x_[:, hf * 512:(hf + 1) * 512], scalar=ALPHA, in1=ps[:],
                                                                 op0=ALU.mult, op1=ALU.add), reads=[tx, tp], writes=[ty])
                ln_tile(C, L, y_, ty, g1, t_g1, b1, t_b1)
                S.dma("sp", yr.dkey(), C.X1[n * 128:(n + 1) * 128, :], y_[:], reads=[ty], writes=[C.t_X1], chain=True)
                tb_, ttb = xtb.next()
                tf_, ttf = xtf.next()
                for hf in range(2):
                    ps, tp = C.psum.next()
                    for j in range(4):
                        k = hf * 4 + j
                        S.op("pe", lambda e: e.transpose(ps[:, j * 128:(j + 1) * 128], y_[:, k * 128:(k + 1) * 128], C.ident_f[:]), reads=[ty, C.t_const], writes=[tp])
                    src = ps[:].rearrange("p (k c) -> p k c", k=4)
                    S.op("act", lambda e: e.copy(out=tb_[:, hf * 4:(hf + 1) * 4, :], in_=src), reads=[tp], writes=[ttb])
                    S.op("dve", lambda e: e.tensor_tensor(out=tf_[:, hf * 4:(hf + 1) * 4, :], in0=src, in1=tb_[:, hf * 4:(hf + 1) * 4, :], op=ALU.subtract), reads=[tp, ttb], writes=[ttf])
                S.dma("sp", xtb.dkey(), C.X1T[:, n * 128:(n + 1) * 128].rearrange("(k p) t -> p k t", p=128), tb_[:], reads=[ttb], writes=[C.t_X1T], chain=True)
                pr, tpr = C.psum.next()
                i3 = 0
                for (xa, wa) in ((tb_, RWh), (tf_, RWh), (tb_, RWl)):
                    for k in range(8):
                        S.op("pe", lambda e: e.matmul(pr[:, 0:32], lhsT=xa[:, k, :], rhs=wa[:, k, :], start=(i3 == 0), stop=(i3 == 23)), reads=[ttf, ttb, t_rw], writes=[tpr])
                        i3 += 1
                s_, ts = sm.next()
                lg, ex, m8 = s_[:, 0:32], s_[:, 32:64], s_[:, 64:72]
                S.op("dve", lambda e: e.tensor_tensor(out=lg, in0=pr[:, 0:32], in1=rb[:], op=ALU.add), reads=[tpr, t_rb], writes=[ts])
                S.op("dve", lambda e: e.max(out=m8, in_=lg), reads=[ts], writes=[ts])
                S.op("dve", lambda e: e.tensor_scalar(out=s_[:, 72:73], in0=s_[:, 64:65], scalar1=-1.0, scalar2=None, op0=ALU.mult), reads=[ts], writes=[ts])
                S.op("act", lambda e: e.activation(out=ex, in_=lg, func=AF.Exp, bias=s_[:, 72:73]), reads=[ts], writes=[ts])
                S.op("dve", lambda e: e.scalar_tensor_tensor(out=ex, in0=lg, scalar=s_[:, 67:68], in1=ex, op0=ALU.is_ge, op1=ALU.mult), reads=[ts], writes=[ts])
                S.op("dve", lambda e: e.tensor_reduce(out=s_[:, 73:74], in_=ex, axis=AX.X, op=ALU.add), reads=[ts], writes=[ts])
                S.op("dve", lambda e: e.reciprocal(out=s_[:, 73:74], in_=s_[:, 73:74]), reads=[ts], writes=[ts])
                S.op("dve", lambda e: e.tensor_scalar(out=C.COMB[:, n, :], in0=ex, scalar1=s_[:, 73:74], scalar2=None, op0=ALU.mult), reads=[ts], writes=[C.t_comb])
        if "COMBD" in C.cfg.get("debug_out", ()):
            dkc = S.dsem()
            S.dma("sp", dkc, C.COMBD.rearrange("(n p) e -> p n e", p=128), C.COMB[:], reads=[C.t_comb], writes=[Tk()])


def phase_W(C, l):
    nc, S = C.nc, C.S
    S.barrier()
    NE = C.cfg.get("n_experts", N_EXPERTS)
    with ExitStack() as es:
        stg = Rot(S, [sb(nc, es, f"W_stg{i}", [128, 8192], F32) for i in range(2)])
        ob = Rot(S, [sb(nc, es, f"W_ob{i}", [128, 8192], BF16) for i in range(2)])
        for ex in range(NE):
            pieces = [(C.exp_w1[l, ex, 0:512, :], C.W1B[ex, 0:512, :], 4, 2048), (C.exp_w1[l, ex, 512:1024, :], C.W1B[ex, 512:1024, :], 4, 2048),
                      (C.exp_w2[l, ex], C.W2B[ex], 8, 1024)]
            for (src, dst, kk, ff) in pieces:
                st_, tst = stg.next()
                S.dma("sp", stg.dkey(), st_[:].rearrange("p (k f) -> p k f", k=kk), src.rearrange("(k p) f -> p k f", p=128), writes=[tst])
                o_, to = ob.next()
                S.op("act", lambda e: e.copy(out=o_[:, 0:2048], in_=st_[:, 0:2048]), reads=[tst], writes=[to])
                S.op("dve", lambda e: e.tensor_copy(out=o_[:, 2048:6144], in_=st_[:, 2048:6144]), reads=[tst], writes=[to])
                S.op("pool", lambda e: e.tensor_copy(out=o_[:, 6144:8192], in_=st_[:, 6144:8192]), reads=[tst], writes=[to])
                to.w = {"act": S.cnt["act"], "dve": S.cnt["dve"], "pool": S.cnt["pool"]}
                S.dma("pool", ob.dkey(), dst.rearrange("(k p) f -> p k f", p=128), o_[:].rearrange("p (k f) -> p k f", k=kk), reads=[to], writes=[C.t_WB], chain=True)


SIGMAX = float(1.0 / (1.0 + np.exp(-1.702 * 7.0)))
QT_ = 1024


def phase_D(C, l, dst):
    nc, S = C.nc, C.S
    S.barrier()
    NE = C.cfg.get("n_experts", N_EXPERTS)
    with ExitStack() as es:
        g2, t_g2 = bcast_row(C, es, "D_g2", C.ln2_g[l:l + 1, :], D)
        b2l, t_b2l = bcast_row(C, es, "D_b2l", C.ln2_b[l:l + 1, :], D)
        L = ln_alloc(C, es, "D_")
        B2 = sb(nc, es, "D_B2", [32, D], BF16)
        t_B2 = Tk()
        dkb = S.dsem()
        S.dma("pool", dkb, B2[:], C.exp_b2[l], writes=[t_B2])
        W1 = Rot(S, [sb(nc, es, f"D_W1_{i}", [128, 8, 2048], BF16) for i in range(2)])
        W2 = [sb(nc, es, f"D_W2_{i}", [128, 8, 1024], BF16) for i in range(2)]
        bt = Rot(S, [sb(nc, es, f"D_bt{i}", [128, 16], F32) for i in range(2)])
        bs = Rot(S, [sb(nc, es, f"D_bs{i}", [128, 16], F32) for i in range(2)])
        XT = sb(nc, es, "D_XT", [128, 8, QT_], BF16)
        acc = sb(nc, es, "D_acc", [128, QT_ // 128, D], F32)
        gT = Rot(S, [sb(nc, es, f"D_gT{i}", [128, 8, 512], BF16) for i in range(2)])
        sg = Rot(S, [sb(nc, es, f"D_sg{i}", [128, 512], F32) for i in range(2)])
        gl = Rot(S, [sb(nc, es, f"D_gl{i}", [128, 512], F32) for i in range(2)])
        ub = Rot(S, [sb(nc, es, f"D_ub{i}", [128, 512], F32) for i in range(2)])
        yb = Rot(S, [sb(nc, es, f"D_yb{i}", [128, 512], F32) for i in range(2)])
        cT = Rot(S, [sb(nc, es, f"D_cT{i}", [32, 128], BF16) for i in range(2)])
        xres = Rot(S, [sb(nc, es, f"D_xr{i}", [128, D], F32) for i in range(1)])
        t_XT, t_acc = Tk(), [Tk() for _ in range(QT_ // 128)]
        dkx = S.dsem()
        for q in range(T // QT_):
            q0 = q * QT_
            S.dma("sp", dkx, XT[:], C.X1T[:, q0:q0 + QT_].rearrange("(k p) t -> p k t", p=128), reads=[C.t_X1T], writes=[t_XT])
            for i in range(QT_ // 128):
                n = q * (QT_ // 128) + i
                pc_, tpc = C.psum.next()
                S.op("pe", lambda e: e.transpose(pc_[0:32, 0:128], C.COMB[:, n, :], C.ident_f[:]), reads=[C.t_comb, C.t_const], writes=[tpc])
                c_, tc = cT.next()
                S.op("act", lambda e: e.copy(out=c_[:], in_=pc_[0:32, 0:128]), reads=[tpc], writes=[tc])
                for hf in range(2):
                    pb_, tpb = C.psum.next()
                    S.op("pe", lambda e: e.matmul(pb_[:], lhsT=c_[:], rhs=B2[:, hf * 512:(hf + 1) * 512], start=True, stop=True), reads=[tc, t_B2], writes=[tpb])
                    S.op("act", lambda e: e.copy(out=acc[:, i, hf * 512:(hf + 1) * 512], in_=pb_[:]), reads=[tpb], writes=[t_acc[i]])
            for ex in range(NE):
                w1, tw1 = W1.next()
                w2 = W2[W1.i]
                dkw = W1.dkey()
                skipw = C.cfg.get("fake_w") and (q > 0 or ex >= 2)
                prew = "W" in C.cfg["phases"]
                for kh in range(0 if skipw else 2):
                    if prew:
                        S.dma("sp", dkw, w1[:, kh * 4:(kh + 1) * 4, :], C.W1B[ex, kh * 512:(kh + 1) * 512, :].rearrange("(k p) f -> p k f", p=128),
                              reads=[C.t_WB], writes=[tw1], chain=(kh > 0))
                    else:
                        S.dma("pool", dkw, w1[:, kh * 4:(kh + 1) * 4, :], C.exp_w1[l, ex, kh * 512:(kh + 1) * 512, :].rearrange("(k p) f -> p k f", p=128),
                              writes=[tw1], chain=(kh > 0))
                for kh in range(0 if skipw else 2):
                    if prew:
                        S.dma("sp", dkw, w2[:, kh * 4:(kh + 1) * 4, :], C.W2B[ex, kh * 512:(kh + 1) * 512, :].rearrange("(k p) d -> p k d", p=128),
                              reads=[C.t_WB], writes=[tw1], chain=True)
                    else:
                        S.dma("pool", dkw, w2[:, kh * 4:(kh + 1) * 4, :], C.exp_w2[l, ex, kh * 512:(kh + 1) * 512, :].rearrange("(k p) d -> p k d", p=128),
                              writes=[tw1], chain=True)
                bt_, tbt = bt.next()
                S.dma("sp", bt.dkey(), bt_[:], C.exp_b1p[l, ex], writes=[tbt])
                bs_, tbs = bs.next()
                S.op("dve", lambda e: e.tensor_scalar(out=bs_[:, 0:8], in0=bt_[:, 0:8], scalar1=1.702, scalar2=None, op0=ALU.mult), reads=[tbt], writes=[tbs])
                S.op("dve", lambda e: e.tensor_scalar(out=bs_[:, 8:16], in0=bt_[:, 8:16], scalar1=1.0, scalar2=None, op0=ALU.add), reads=[tbt, tbs], writes=[tbs])
                dsub = C.cfg.get("dsub", 9)
                gts = []
                for g in range(QT_ // 512 if dsub >= 2 else 0):
                    tsl = slice(g * 512, (g + 1) * 512)
                    g_, tg = gT.next()
                    gts.append((g_, tg))
                    for j in range(8):
                        pG, tpG = C.psum.next()
                        for k in range(8):
                            S.op("pe", lambda e: e.matmul(pG[:], lhsT=w1[:, k, j * 128:(j + 1) * 128], rhs=XT[:, k, tsl], start=(k == 0), stop=(k == 7)),
                                 reads=[tw1, t_XT], writes=[tpG])
                        pU, tpU = C.psum.next()
                        for k in range(8):
                            S.op("pe", lambda e: e.matmul(pU[:], lhsT=w1[:, k, 1024 + j * 128:1024 + (j + 1) * 128], rhs=XT[:, k, tsl], start=(k == 0), stop=(k == 7)),
                                 reads=[tw1, t_XT], writes=[tpU])
                        z_, tz = gl.next()
                        u_, tu = ub.next()
                        s_, ts = sg.next()
                        S.op("act", lambda e: e.activation(out=z_[:], in_=pG[:], func=AF.Identity, bias=bt_[:, j:j + 1]), reads=[tpG, tbt], writes=[tz])
                        S.op("act", lambda e: e.activation(out=u_[:], in_=pU[:], func=AF.Identity, bias=bs_[:, 8 + j:9 + j]), reads=[tpU, tbs], writes=[tu])
                        S.op("dve", lambda e: e.tensor_scalar(out=z_[:], in0=z_[:], scalar1=7.0, scalar2=None, op0=ALU.min), reads=[tz], writes=[tz])
                        S.op("act", lambda e: e.activation(out=s_[:], in_=z_[:], func=AF.Sigmoid, scale=1.702), reads=[tz], writes=[ts])
                        S.op("dve", lambda e: e.tensor_scalar(out=u_[:], in0=u_[:], scalar1=8.0, scalar2=-6.0, op0=ALU.min, op1=ALU.max), reads=[tu], writes=[tu])
                        S.op("dve", lambda e: e.tensor_tensor(out=z_[:], in0=z_[:], in1=s_[:], op=ALU.mult), reads=[tz, ts], writes=[tz])
                        S.op("dve", lambda e: e.tensor_tensor(out=g_[:, j, :], in0=u_[:], in1=z_[:], op=ALU.mult), reads=[tu, tz], writes=[tg])
                for g in range(QT_ // 512 if dsub >= 2 else 0):
                    g_, tg = gts[g]
                    for tt in range(4 if dsub >= 3 else 0):
                        i = g * 4 + tt
                        n = q * (QT_ // 128) + i
                        for hf in range(2):
                            pY, tpY = C.psum.next()
                            for j in range(8):
                                S.op("pe", lambda e: e.matmul(pY[:], lhsT=g_[:, j, tt * 128:(tt + 1) * 128], rhs=w2[:, j, hf * 512:(hf + 1) * 512], start=(j == 0), stop=(j == 7)),
                                     reads=[tg, tw1], writes=[tpY])
                            y_, tyb = yb.next()
                            S.op("act", lambda e: e.activation(out=y_[:], in_=pY[:], func=AF.Copy, scale=C.COMB[:, n, ex:ex + 1]), reads=[tpY, C.t_comb], writes=[tyb])
                            S.op("pool", lambda e: e.tensor_tensor(out=acc[:, i, hf * 512:(hf + 1) * 512], in0=acc[:, i, hf * 512:(hf + 1) * 512], in1=y_[:], op=ALU.add),
                                 reads=[tyb, t_acc[i]], writes=[t_acc[i]])
            for i in range(QT_ // 128):
                n = q * (QT_ // 128) + i
                x_, tx = xres.next()
                S.dma("sp", xres.dkey(), x_[:], C.X1[n * 128:(n + 1) * 128, :], reads=[C.t_X1], writes=[tx])
                y = acc[:, i, :]
                S.op("dve", lambda e: e.scalar_tensor_tensor(out=y, in0=x_[:], scalar=ALPHA, in1=y, op0=ALU.mult, op1=ALU.add), reads=[tx, t_acc[i]], writes=[t_acc[i]])
                ln_tile(C, L, y, t_acc[i], g2, t_g2, b2l, t_b2l)
                S.dma("sp", dkx, dst[n * 128:(n + 1) * 128, :], y, reads=[t_acc[i]], writes=[C.t_X2], chain=True)


SMALL_INPUTS = ("ret_gn_g", "ret_gn_b", "w_out", "router_w", "router_b", "ln1_g", "ln1_b", "exp_w1", "exp_w2", "exp_b2", "ln2_g", "ln2_b")


WEIGHT_KEYS = ("w_in", "conv_w", "conv_b", "rg_bx", "rg_ba", "rg_lambda", "rg_wx", "rg_wa", "exp_b1")
_CACHE = {}


def _shared_inputs(inp):
    shared = dict(host_consts())
    shared.update(prep_weights({k: inp[k] for k in WEIGHT_KEYS}))
    for k in SMALL_INPUTS:
        shared[k] = np.ascontiguousarray(inp[k], dtype=np.float32)
    return shared


def _run(inputs, n_cores=8, trace=False, first=0):
    inp = {k: np.asarray(v) for k, v in inputs.items()}
    if "nc" not in _CACHE:
        _CACHE["nc"] = build(dict(phases="AMSRGCWD", layers=DEPTH))
    nc = _CACHE["nc"]
    shared = _shared_inputs(inp)
    x = np.ascontiguousarray(inp["x"], dtype=np.float32)
    in_maps = []
    for c in range(n_cores):
        m = dict(shared)
        m["x"] = np.ascontiguousarray(x[first + c])
        in_maps.append(m)
    res = run_bass_kernel_spmd(nc, in_maps, core_ids=list(range(n_cores)), trace=trace)
    out = np.stack([np.asarray(r["out"], dtype=np.float32) for r in res.results], axis=0)
    return out, res


LAYER_KEYS = ("w_fmr", "w_fms", "w_fmp", "w_dw", "w_tm", "rg_pc", "rg_wbd", "exp_b1p") + SMALL_INPUTS


def _run_layers(inputs, n_cores=8, trace=False):
    inp = {k: np.asarray(v) for k, v in inputs.items()}
    if "nc1" not in _CACHE:
        _CACHE["nc1"] = build(dict(phases="AMSRGCD", layers=1, decl_depth=1))
    nc = _CACHE["nc1"]
    shared = _shared_inputs(inp)
    x = np.ascontiguousarray(inp["x"], dtype=np.float32)
    results = []
    for l in range(DEPTH):
        sh = dict(shared)
        for k in LAYER_KEYS:
            sh[k] = np.ascontiguousarray(shared[k][l:l + 1])
        in_maps = []
        for c in range(n_cores):
            m = dict(sh)
            m["x"] = np.ascontiguousarray(x[c])
            in_maps.append(m)
        res = run_bass_kernel_spmd(nc, in_maps, core_ids=list(range(n_cores)), trace=trace)
        results.append(res)
        x = np.stack([np.asarray(r["out"], dtype=np.float32) for r in res.results], axis=0)
    return x, results


def kernel(**inputs):
    out, _ = _run(inputs, 8)
    return out
```
